# Optimizing a Trainium2 kernel written in Bass

```python
import math
import jax, jax.numpy as jnp
from jax import lax
import numpy as np

D_MODEL = 1024
BATCH = 2
SEQ = 8192
DEPTH = 1

N_DIFF_HEADS = 4
DIFF_HEAD_DIM = 64
DIFF_V_DIM = 2 * DIFF_HEAD_DIM
N_MOBA_HEADS = 8
MOBA_HEAD_DIM = 64
MOBA_BLOCK = 256
MOBA_TOPK = 3
Q_BLOCK = 128
N_HEADS_TOTAL = N_DIFF_HEADS + N_MOBA_HEADS
N_BUCKETS = 32
MAX_DISTANCE = 128
N_EXPERTS = 32
TOP_K = 4
D_FF = D_MODEL
SWIGLU_LIMIT = 7.0
SWIGLU_ALPHA = 1.702
RMS_EPS = 1e-5

DIFF_QK_WIDTH = N_DIFF_HEADS * 2 * DIFF_HEAD_DIM
DIFF_V_WIDTH = N_DIFF_HEADS * DIFF_V_DIM
MOBA_WIDTH = N_MOBA_HEADS * MOBA_HEAD_DIM
SPLIT_WIDTHS = (DIFF_QK_WIDTH, DIFF_QK_WIDTH, DIFF_V_WIDTH,
                MOBA_WIDTH, MOBA_WIDTH, MOBA_WIDTH, D_MODEL, D_MODEL)
IN_WIDTH = sum(SPLIT_WIDTHS)
SPLIT_POINTS = tuple(int(v) for v in np.cumsum(SPLIT_WIDTHS)[:-1])

kernel_name = "hybrid_diffattn_moba_moe_adaln"


def rmsnorm(x, g):
    xf = x.astype(jnp.float32)
    y = xf * lax.rsqrt(jnp.mean(xf * xf, axis=-1, keepdims=True) + RMS_EPS)
    return y.astype(x.dtype) * g


def t5_bucket(dist):
    n = jnp.maximum(dist, 0)
    max_exact = N_BUCKETS // 2
    nf = jnp.maximum(n, max_exact).astype(jnp.float32)
    large = max_exact + (jnp.log(nf / max_exact) / math.log(MAX_DISTANCE / max_exact)
                         * (N_BUCKETS - max_exact)).astype(jnp.int32)
    large = jnp.minimum(large, N_BUCKETS - 1)
    return jnp.where(n < max_exact, n, large)


def diff_attention(q, k, v, bias_tab, lam, lam_init, subln_g):
    B, H, _, S, d = q.shape
    dv = v.shape[-1]
    nq = S // Q_BLOCK
    scale = d ** -0.5
    k_pos = jnp.arange(S)

    def one_block(i):
        q0 = i * Q_BLOCK
        qb = lax.dynamic_slice_in_dim(q, q0, Q_BLOCK, axis=3)
        s = jnp.einsum('bhmqd,bhmkd->bhmqk', qb, k).astype(jnp.float32) * scale
        dist = (q0 + jnp.arange(Q_BLOCK))[:, None] - k_pos[None, :]
        bias = jnp.transpose(bias_tab[t5_bucket(dist)], (2, 0, 1)).astype(jnp.float32)
        s = jnp.where(dist >= 0, s + bias[None, :, None], -jnp.inf)
        p = jax.nn.softmax(s, axis=-1)
        p = p[:, :, 0] - lam * p[:, :, 1]
        return jnp.einsum('bhqk,bhkd->bhqd', p.astype(v.dtype), v)

    o = lax.map(one_block, jnp.arange(nq))
    o = jnp.transpose(o, (1, 0, 3, 2, 4)).reshape(B, S, H, dv)
    o = rmsnorm(o, subln_g) * (1.0 - lam_init)
    return o.reshape(B, S, H * dv)


def moba_attention(q, k, v, bias_tab):
    B, H, S, d = q.shape
    nb = -(-S // MOBA_BLOCK)
    pad = nb * MOBA_BLOCK - S
    k_pad = jnp.pad(k, ((0, 0), (0, 0), (0, pad), (0, 0)))
    v_pad = jnp.pad(v, ((0, 0), (0, 0), (0, pad), (0, 0)))
    k_blocks = k_pad.reshape(B, H, nb, MOBA_BLOCK, d)
    v_blocks = v_pad.reshape(B, H, nb, MOBA_BLOCK, d)
    k_mean = jnp.mean(k_blocks.astype(jnp.float32), axis=3).astype(k.dtype)
    k_sel = min(MOBA_TOPK, nb)
    scale = d ** -0.5
    bi = jnp.arange(B)[:, None, None, None]
    hi = jnp.arange(H)[None, :, None, None]
    hi5 = jnp.arange(H)[None, :, None, None, None]
    blk_ar = jnp.arange(MOBA_BLOCK)
    nq = S // Q_BLOCK

    def one_block(i):
        q0 = i * Q_BLOCK
        own = q0 // MOBA_BLOCK
        qb = lax.dynamic_slice_in_dim(q, q0, Q_BLOCK, axis=2)
        q_pos = q0 + jnp.arange(Q_BLOCK)
        gs = jnp.einsum('bhqd,bhnd->bhqn', qb, k_mean).astype(jnp.float32)
        gs = jnp.where(jnp.arange(nb) < own, gs, -jnp.inf)
        _, sel = lax.top_k(gs, k_sel)
        sel_valid = jnp.arange(k_sel) < own
        ks = k_blocks[bi, hi, sel]
        vs = v_blocks[bi, hi, sel]
        s_sel = jnp.einsum('bhqd,bhqjkd->bhqjk', qb, ks).astype(jnp.float32) * scale
        sel_pos = sel[..., None] * MOBA_BLOCK + blk_ar
        dist_sel = q_pos[:, None, None] - sel_pos
        s_sel = s_sel + bias_tab[t5_bucket(dist_sel), hi5].astype(jnp.float32)
        s_sel = jnp.where(sel_valid[:, None], s_sel, -jnp.inf)
        s_sel = s_sel.reshape(B, H, Q_BLOCK, k_sel * MOBA_BLOCK)
        k_own = lax.dynamic_slice_in_dim(k_pad, own * MOBA_BLOCK, MOBA_BLOCK, axis=2)
        v_own = lax.dynamic_slice_in_dim(v_pad, own * MOBA_BLOCK, MOBA_BLOCK, axis=2)
        s_own = jnp.einsum('bhqd,bhkd->bhqk', qb, k_own).astype(jnp.float32) * scale
        dist_own = q_pos[:, None] - (own * MOBA_BLOCK + blk_ar)[None, :]
        bias_own = jnp.transpose(bias_tab[t5_bucket(dist_own)], (2, 0, 1)).astype(jnp.float32)
        s_own = jnp.where(dist_own >= 0, s_own + bias_own[None], -jnp.inf)
        p = jax.nn.softmax(jnp.concatenate([s_sel, s_own], axis=-1), axis=-1).astype(v.dtype)
        p_sel = p[..., :k_sel * MOBA_BLOCK].reshape(B, H, Q_BLOCK, k_sel, MOBA_BLOCK)
        p_own = p[..., k_sel * MOBA_BLOCK:]
        return (jnp.einsum('bhqjk,bhqjkd->bhqd', p_sel, vs)
                + jnp.einsum('bhqk,bhkd->bhqd', p_own, v_own))

    o = lax.map(one_block, jnp.arange(nq))
    return jnp.transpose(o, (1, 0, 3, 2, 4)).reshape(B, S, H * d)


def moe_ffn(h, w_router, b_router, w_gate_up, b_gate_up, w_down, b_down):
    B, S, D = h.shape
    t = h.reshape(B * S, D)
    logits = (t @ w_router + b_router).astype(jnp.float32)
    top_val, top_idx = lax.top_k(logits, TOP_K)
    top_w = jax.nn.softmax(top_val, axis=-1)
    combine = jnp.sum(jax.nn.one_hot(top_idx, N_EXPERTS, dtype=jnp.float32)
                      * top_w[..., None], axis=1).astype(t.dtype)
    out = jnp.zeros_like(t)
    for e in range(N_EXPERTS):
        gu = t @ w_gate_up[e] + b_gate_up[e]
        gate = jnp.minimum(gu[:, :D_FF], SWIGLU_LIMIT)
        up = jnp.clip(gu[:, D_FF:], -SWIGLU_LIMIT, SWIGLU_LIMIT)
        act = (up + 1.0) * gate * jax.nn.sigmoid(SWIGLU_ALPHA * gate)
        out = out + combine[:, e:e + 1] * (act @ w_down[e] + b_down[e])
    return out.reshape(B, S, D)


def setup_inputs(seed: int = 0) -> dict:
    key = jax.random.key(seed)
    ks = jax.random.split(key, 24)
    f32 = jnp.float32
    nrm = lambda k, shape, s: jax.random.normal(k, shape, f32) * s
    L, D, E = DEPTH, D_MODEL, N_EXPERTS
    return {
        "x": nrm(ks[0], (BATCH, SEQ, D), 1.0),
        "c": nrm(ks[1], (BATCH, D), 1.0),
        "rel_bias": nrm(ks[2], (N_BUCKETS, N_HEADS_TOTAL), 0.5),
        "w_ada": nrm(ks[3], (L, D, 6 * D), 0.5 * D ** -0.5),
        "b_ada": nrm(ks[4], (L, 6 * D), 0.02),
        "g_mix": 1.0 + nrm(ks[5], (L, D), 0.02),
        "w_in": nrm(ks[6], (L, D, IN_WIDTH), D ** -0.5),
        "lambda_q1": nrm(ks[7], (L, DIFF_HEAD_DIM), 0.1),
        "lambda_k1": nrm(ks[8], (L, DIFF_HEAD_DIM), 0.1),
        "lambda_q2": nrm(ks[9], (L, DIFF_HEAD_DIM), 0.1),
        "lambda_k2": nrm(ks[10], (L, DIFF_HEAD_DIM), 0.1),
        "subln_g": 1.0 + nrm(ks[11], (L, DIFF_V_DIM), 0.02),
        "w_br_a": nrm(ks[12], (L, DIFF_V_WIDTH, D), DIFF_V_WIDTH ** -0.5),
        "w_br_b": nrm(ks[13], (L, MOBA_WIDTH, D), MOBA_WIDTH ** -0.5),
        "w_out": nrm(ks[14], (L, D, D), D ** -0.5),
        "g_ffn": 1.0 + nrm(ks[15], (L, D), 0.02),
        "w_router": nrm(ks[16], (L, D, E), D ** -0.5),
        "b_router": nrm(ks[17], (L, E), 0.01),
        "w_gate_up": nrm(ks[18], (L, E, D, 2 * D_FF), D ** -0.5),
        "b_gate_up": nrm(ks[19], (L, E, 2 * D_FF), 0.01),
        "w_down": nrm(ks[20], (L, E, D_FF, D), D_FF ** -0.5),
        "b_down": nrm(ks[21], (L, E, D), 0.01),
        "g_final": 1.0 + nrm(ks[22], (D,), 0.02),
    }


def reference(x, c, rel_bias, w_ada, b_ada, g_mix, w_in, lambda_q1, lambda_k1,
              lambda_q2, lambda_k2, subln_g, w_br_a, w_br_b, w_out, g_ffn,
              w_router, b_router, w_gate_up, b_gate_up, w_down, b_down, g_final):
    B, S, D = x.shape
    bias_a = rel_bias[:, :N_DIFF_HEADS]
    bias_b = rel_bias[:, N_DIFF_HEADS:]
    for l in range(DEPTH):
        ada = c @ w_ada[l] + b_ada[l]
        sh_m, sc_m, gt_m, sh_f, sc_f, gt_f = [a[:, None, :] for a in jnp.split(ada, 6, axis=-1)]

        h = rmsnorm(x, g_mix[l]) * (1.0 + sc_m) + sh_m
        proj = h @ w_in[l]
        q_a, k_a, v_a, q_b, k_b, v_b, gl_a, gl_b = jnp.split(proj, SPLIT_POINTS, axis=-1)
        q_a = q_a.reshape(B, S, N_DIFF_HEADS, 2, DIFF_HEAD_DIM).transpose(0, 2, 3, 1, 4)
        k_a = k_a.reshape(B, S, N_DIFF_HEADS, 2, DIFF_HEAD_DIM).transpose(0, 2, 3, 1, 4)
        v_a = v_a.reshape(B, S, N_DIFF_HEADS, DIFF_V_DIM).transpose(0, 2, 1, 3)
        q_b = q_b.reshape(B, S, N_MOBA_HEADS, MOBA_HEAD_DIM).transpose(0, 2, 1, 3)
        k_b = k_b.reshape(B, S, N_MOBA_HEADS, MOBA_HEAD_DIM).transpose(0, 2, 1, 3)
        v_b = v_b.reshape(B, S, N_MOBA_HEADS, MOBA_HEAD_DIM).transpose(0, 2, 1, 3)

        lam_init = 0.8 - 0.6 * math.exp(-0.3 * l)
        lam = (jnp.exp(jnp.sum(lambda_q1[l].astype(jnp.float32) * lambda_k1[l].astype(jnp.float32)))
               - jnp.exp(jnp.sum(lambda_q2[l].astype(jnp.float32) * lambda_k2[l].astype(jnp.float32)))
               + lam_init)
        y_a = diff_attention(q_a, k_a, v_a, bias_a, lam, lam_init, subln_g[l])
        y_b = moba_attention(q_b, k_b, v_b, bias_b)

        merged = (jax.nn.sigmoid(gl_a) * (y_a @ w_br_a[l])
                  + jax.nn.sigmoid(gl_b) * (y_b @ w_br_b[l]))
        x = x + gt_m * (merged @ w_out[l])

        h = rmsnorm(x, g_ffn[l]) * (1.0 + sc_f) + sh_f
        x = x + gt_f * moe_ffn(h, w_router[l], b_router[l], w_gate_up[l],
                               b_gate_up[l], w_down[l], b_down[l])
    return rmsnorm(x, g_final)
```

```python
import math
from contextlib import ExitStack

import numpy as np
import ml_dtypes
import concourse.bass as bass
import concourse.mybir as mybir
from concourse.bass_utils import run_bass_kernel_spmd

F32 = mybir.dt.float32
BF16 = mybir.dt.bfloat16
AF = mybir.ActivationFunctionType
ALU = mybir.AluOpType
AX = mybir.AxisListType

D = 1024
SEQ = 8192
NT = 64
NG = 16
NQB = 8
NE = 32
NEG = -30000.0
DEBUG = False
import os
STAGE = int(os.environ.get("KSTAGE", "99"))
KDEBUG = int(os.environ.get("KDEBUG", "0"))
_LAST = {}


class _Stop(Exception):
    pass

COMPUTE = ("pe", "act", "dve", "pool")


class Op:
    __slots__ = ("eng", "fn", "deps", "sem", "val", "need_inc", "is_dma")

    def __init__(self, eng, fn, is_dma=False):
        self.eng = eng
        self.fn = fn
        self.deps = []
        self.sem = None
        self.val = None
        self.need_inc = False
        self.is_dma = is_dma


class Prog:
    def __init__(self, nc, same_engine_raw=True):
        self.nc = nc
        self.ops = {e: [] for e in ("pe", "act", "dve", "pool", "sp")}
        self.last_w = {}
        self.readers = {}
        self.dma_keys = {}
        self.same_engine_raw = same_engine_raw
        self.dma_since_barrier = []

    def _add_dep(self, op, d, kind):
        if d is None or d is op:
            return
        if (not d.is_dma) and (not op.is_dma) and d.eng == op.eng:
            if d.eng == "pe":
                return
            if kind != "raw" or not self.same_engine_raw:
                return
        op.deps.append(d)
        d.need_inc = True

    def op(self, eng, fn, reads=(), writes=(), dma_key=None):
        is_dma = dma_key is not None
        o = Op(eng, fn, is_dma)
        for r in reads:
            self._add_dep(o, self.last_w.get(r), "raw")
        for w in writes:
            self._add_dep(o, self.last_w.get(w), "waw")
            for rd in self.readers.get(w, ()):
                self._add_dep(o, rd, "war")
        for r in reads:
            self.readers.setdefault(r, []).append(o)
        for w in writes:
            self.last_w[w] = o
            self.readers[w] = []
        if is_dma:
            o.sem = ("dma", dma_key)
            c = self.dma_keys.setdefault(dma_key, [0])
            c[0] += 16
            o.val = c[0]
            o.need_inc = True
            self.dma_since_barrier.append(o)
        self.ops[eng].append(o)
        return o

    def dma(self, eng, out, in_, reads=(), writes=(), key=None):
        return self.op(eng, lambda e: e.dma_start(out=out, in_=in_), reads, writes, dma_key=key)

    def barrier(self):
        lasts = [self.ops[e][-1] for e in COMPUTE if self.ops[e]]
        dmas = list(self.dma_since_barrier)
        self.dma_since_barrier = []
        for e in ("pe", "act", "dve", "pool", "sp"):
            o = Op(e, lambda eng: eng.nop())
            for d in lasts + dmas:
                if d.eng == e and not d.is_dma:
                    continue
                o.deps.append(d)
                d.need_inc = True
            self.ops[e].append(o)
        self.last_w = {}
        self.readers = {}

    def emit(self, final_wait_ops=()):
        nc = self.nc
        for e in COMPUTE:
            c = 0
            for o in self.ops[e]:
                if o.need_inc and not o.is_dma:
                    c += 1
                    o.sem = ("eng", e)
                    o.val = c
        sem_names = set()
        for e in self.ops:
            for o in self.ops[e]:
                if o.need_inc:
                    sem_names.add(o.sem)
        with ExitStack() as st:
            sems = {}
            for i, s in enumerate(sorted(sem_names, key=str)):
                sems[s] = st.enter_context(nc.semaphore("s%d" % i))
            block = st.enter_context(nc.Block())
            engmap = {"pe": "tensor", "act": "scalar", "dve": "vector", "pool": "gpsimd", "sp": "sync"}

            def make(ename):
                ops = self.ops[ename]

                def body(eng):
                    waited = {}
                    for o in ops:
                        need = {}
                        for d in o.deps:
                            if d.val > need.get(d.sem, 0):
                                need[d.sem] = d.val
                        for s, v in need.items():
                            if waited.get(s, 0) >= v:
                                continue
                            eng.wait_ge(sems[s], v)
                            waited[s] = v
                        ins = o.fn(eng)
                        if o.need_inc:
                            ins.then_inc(sems[o.sem], 16 if o.is_dma else 1)
                    if ename == "sp":
                        for o in final_wait_ops:
                            eng.wait_ge(sems[o.sem], o.val)
                return body

            for ename in self.ops:
                getattr(block, engmap[ename])(make(ename))


def _t5_bucket_np(dist):
    n = np.maximum(dist, 0)
    max_exact = 16
    nf = np.maximum(n, max_exact).astype(np.float32)
    large = max_exact + (np.log(nf / np.float32(max_exact)) / np.float32(math.log(128 / max_exact))
                         * np.float32(32 - max_exact)).astype(np.int32)
    large = np.minimum(large, 31)
    return np.where(n < max_exact, n, large)


def build_program():
    nc = bass.Bass("TRN2", target_bir_lowering=False)

    def din(name, shape, dt=F32):
        return nc.dram_tensor(name, list(shape), dt, kind="ExternalInput").ap()

    xf = din("xf", [SEQ, D])
    cbc_d = din("cbc", [128, 8 * 128])
    valid_d = din("valid", [128, NT])
    blkbias_d = din("blkbias", [128, NQB * 32])
    bt_d = din("bt", [128, 12 * 3 * 256])
    b31_d = din("b31", [128, 12])
    umask_d = din("umask", [128, 2])
    ident_d = din("ident", [128, 128], BF16)
    w_ada = din("w_ada", [D, 6 * D])
    bada_d = din("bada_bc", [128, 6 * D])
    gmix_d = din("gmix_bc", [128, D])
    gffn_d = din("gffn_bc", [128, D])
    gfin_d = din("gfin_bc", [128, D])
    gsub_d = din("gsub_bc", [128, 128])
    lam_d = din("lam_bc", [128, 4 * 64])
    w_in = din("w_in", [D, 5120])
    w_br_a = din("w_br_a", [512, D])
    w_br_b = din("w_br_b", [512, D])
    w_out = din("w_out", [D, D])
    w_router = din("w_router", [D, NE])
    brt_d = din("brouter_bc", [128, NE])
    w_gu = din("w_gate_up", [NE, D, 2 * D]) if STAGE > 20 else None
    bgu_d = din("bgu_col", [128, NE * 16])
    w_dn = din("w_down", [NE, D, D]) if STAGE > 20 else None
    bdn_d = din("b_down", [NE, D])
    out_d = nc.dram_tensor("out", [2048, D], F32, kind="ExternalOutput").ap()
    dk = "ExternalOutput" if KDEBUG else "Internal"
    hTd = nc.dram_tensor("hTd", [NG, 128, 8 * 512], BF16, kind=dk).ap()
    x1d = nc.dram_tensor("x1d", [2048, D], F32, kind=dk).ap()
    if KDEBUG:
        dbg_ada = nc.dram_tensor("dbg_ada", [128, 3072], F32, kind=dk).ap()
        dbg_ada2 = nc.dram_tensor("dbg_ada2", [128, 3072], F32, kind=dk).ap()
        dbg_yT = nc.dram_tensor("dbg_yT", [128, 16384], BF16, kind=dk).ap()
        dbg_comb = nc.dram_tensor("dbg_comb", [128, 512], F32, kind=dk).ap()
        dbg_sm = nc.dram_tensor("dbg_sm", [128, 64], F32, kind=dk).ap()
        dbg_acc = nc.dram_tensor("dbg_acc", [128, 2048], F32, kind=dk).ap()

    P = Prog(nc)

    def chk(k):
        if STAGE == k:
            raise _Stop()

    with ExitStack() as st:
      try:
          def sb(name, cols, dt):
              return st.enter_context(nc.sbuf_tensor("sb_" + name, [128, cols], dt))

          def ps(name, cols, dt):
              return st.enter_context(nc.psum_tensor("ps_" + name, [128, cols], dt))

          BIGA = sb("BIGA", 16384, BF16)
          BIGB = sb("BIGB", 16896, BF16)
          BIGC = sb("BIGC", 8192, BF16)
          BIGD = sb("BIGD", 8192, BF16)
          YT = sb("YT", 16384, BF16)
          F32A = sb("F32A", 8192, F32)
          F32B = sb("F32B", 3072, F32)
          ADA = sb("ADA", 3072, F32)
          XNX = sb("XNX", 4736, BF16)
          XN = XNX
          ident = sb("ident", 128, BF16)
          CBC = F32B[:, 2048:3072]
          valid = sb("valid", NT, F32)
          blkbias = sb("blkbias", NQB * 32, F32)
          b31 = sb("b31", 12, F32)
          UMASK = sb("umask", 2, F32)
          gsub = sb("gsub", 128, F32)
          lamt = F32A[:, 0:256]
          SM = sb("SM", 64, F32)
          COMB = sb("COMB", 16 * 32, F32)
          BGU = sb("BGU", NE * 16, F32)
          WR = sb("WR", 8 * 32, BF16)
          BRT = sb("BRT", NE, F32)
          BDN = XNX[:, 2560:3584]
          S2 = ps("S2", 1024, F32)
          PO = [ps("PO%d" % i, 512, F32) for i in range(4)]
          PM = ps("PM", 512, F32)
          PTR = ps("PTR", 1024, BF16)

          def v3(ap, inner):
              return ap.rearrange("p (a b) -> p a b", b=inner)

          EPS = SM[:, 0:1]
          NEGLAM = SM[:, 1:2]
          EPS2 = SM[:, 7:8]
          LAMF = 1.0 - (0.8 - 0.6 * math.exp(-0.3 * 0))

          P.dma("sp", ident[:], ident_d, writes=["ident"], key="c0")
          P.dma("sp", valid[:], valid_d, writes=["valid"], key="c2")
          P.dma("sp", blkbias[:], blkbias_d, writes=["blkbias"], key="c3")
          P.dma("sp", b31[:], b31_d, writes=["b31"], key="c4")
          P.dma("sp", UMASK[:], umask_d, writes=["umask"], key="c4b")
          P.dma("sp", gsub[:], gsub_d, writes=["gsub"], key="c5")
          P.dma("sp", lamt, lam_d, writes=["lamt"], key="c6")
          P.dma("sp", BGU[:], bgu_d, writes=["BGU"], key="c7")
          P.dma("sp", BRT[:], brt_d, writes=["BRT"], key="c8")
          P.dma("pool", WR[:], w_router.rearrange("(kc p) n -> p kc n", p=128), writes=["WR"], key="c9")
          P.op("dve", lambda e: e.memset(SM[:], 0.0), writes=["SM"])
          P.op("dve", lambda e: e.memset(EPS, 1e-5), writes=["SM"])
          P.op("dve", lambda e: e.memset(EPS2, 1e-5 / (LAMF * LAMF)), writes=["SM"])
          P.op("dve", lambda e: e.tensor_scalar(out=v3(BGU[:, :], 16)[:, :, 8:16], in0=v3(BGU[:, :], 16)[:, :, 8:16], scalar1=1.0,
                                                 scalar2=None, op0=ALU.add), reads=["BGU"], writes=["BGU"])
          P.op("dve", lambda e: e.tensor_tensor(out=F32B[:, 0:64], in0=lamt[:, 0:64], in1=lamt[:, 64:128], op=ALU.mult),
               reads=["lamt"], writes=["lamtmp"])
          P.op("dve", lambda e: e.reduce_sum(out=SM[:, 2:3], in_=F32B[:, 0:64], axis=AX.X), reads=["lamtmp"], writes=["lam1"])
          P.op("dve", lambda e: e.tensor_tensor(out=F32B[:, 64:128], in0=lamt[:, 128:192], in1=lamt[:, 192:256], op=ALU.mult),
               reads=["lamt"], writes=["lamtmp2"])
          P.op("dve", lambda e: e.reduce_sum(out=SM[:, 3:4], in_=F32B[:, 64:128], axis=AX.X), reads=["lamtmp2"], writes=["lam2"])
          P.op("act", lambda e: e.activation(out=SM[:, 4:6], in_=SM[:, 2:4], func=AF.Exp), reads=["lam1", "lam2", "SM"], writes=["lame"])
          P.op("dve", lambda e: e.tensor_tensor(out=SM[:, 6:7], in0=SM[:, 5:6], in1=SM[:, 4:5], op=ALU.subtract),
               reads=["lame"], writes=["lamd"])
          P.op("dve", lambda e: e.tensor_scalar(out=NEGLAM, in0=SM[:, 6:7], scalar1=-0.2, scalar2=None, op0=ALU.add),
               reads=["lamd"], writes=["neglam"])
          P.barrier()

          gtfd = nc.dram_tensor("gtfd", [128, D], F32).ap()

          def ada_seg(seg, slot, tag, load_cbc=False):
              if load_cbc:
                  P.dma("sp", CBC, cbc_d, writes=["CBC"], key="c1")
              for hh_ in range(2):
                  P.dma("sp", v3(F32A[:, 0:8192], 1024)[:, :, hh_ * 512:(hh_ + 1) * 512],
                        w_ada[:, seg * 1024 + hh_ * 512: seg * 1024 + hh_ * 512 + 512].rearrange("(kc p) n -> p kc n", p=128),
                        writes=["F32A_%d" % hh_], key="wada%d" % hh_)
              P.dma("sp", F32B[:, 0:1024], bada_d[:, seg * 1024:(seg + 1) * 1024], writes=["badaseg"], key="bada")
              for half in range(2):
                  pb = PO[half]
                  for kc in range(8):
                      P.op("pe", lambda e, kc=kc, half=half, pb=pb: e.matmul(
                          pb[:, 0:512], lhsT=CBC[:, kc * 128:(kc + 1) * 128],
                          rhs=F32A[:, kc * 1024 + half * 512: kc * 1024 + half * 512 + 512],
                          start=(kc == 0), stop=(kc == 7)), reads=["CBC", "F32A_%d" % half], writes=["PO%d" % half])
                  P.op("dve", lambda e, half=half, pb=pb: e.tensor_tensor(
                      out=ADA[:, slot * 1024 + half * 512: slot * 1024 + half * 512 + 512], in0=pb[:, 0:512],
                      in1=F32B[:, half * 512: half * 512 + 512], op=ALU.add),
                      reads=["PO%d" % half, "badaseg"], writes=[tag])

          def ada_scale(slot, tag, g_d):
              P.dma("sp", F32B[:, 1024:2048], g_d, writes=["gtmp"], key="gtmp")
              P.op("dve", lambda e: e.scalar_tensor_tensor(
                  out=ADA[:, slot * 1024:(slot + 1) * 1024], in0=ADA[:, slot * 1024:(slot + 1) * 1024], scalar=1.0,
                  in1=F32B[:, 1024:2048], op0=ALU.add, op1=ALU.mult), reads=[tag, "gtmp"], writes=[tag])

          chk(0)
          ada_seg(0, 0, "ada0", load_cbc=True)
          ada_seg(1, 1, "ada1")
          ada_scale(1, "ada1", gmix_d)
          ada_seg(2, 2, "ada2")
          P.barrier()
          if KDEBUG:
              P.dma("sp", dbg_ada, ADA[:, :], key="dbg_ada")
              P.dma("sp", dbg_sm, SM[:, :], key="dbg_sm")
              P.barrier()

          def norm_transpose(xt_ap, xt_res, aslot, atag, sslot, stag, dst3, dst_res, par):
              ssq = SM[:, 8 + par:9 + par]
              rs = SM[:, 10 + par:11 + par]
              xn = XN[:, par * 1024:(par + 1) * 1024]
              t1 = F32B[:, 2048:3072]
              P.op("dve", lambda e: e.memset(ssq, 0.0), writes=["ssq%d" % par])
              P.op("act", lambda e: e.activation(out=xn, in_=xt_ap, func=AF.Square, accum_out=ssq),
                   reads=[xt_res, "ssq%d" % par], writes=["xn%d" % par, "ssq%d" % par])
              P.op("act", lambda e: e.activation(out=rs, in_=ssq, func=AF.Sqrt, scale=1.0 / D, bias=EPS),
                   reads=["ssq%d" % par, "SM"], writes=["rs%d" % par])
              P.op("dve", lambda e: e.reciprocal(out=rs, in_=rs), reads=["rs%d" % par], writes=["rs%d" % par])
              P.op("dve", lambda e: e.scalar_tensor_tensor(out=t1, in0=xt_ap, scalar=rs, in1=ADA[:, aslot * 1024:(aslot + 1) * 1024],
                                                            op0=ALU.mult, op1=ALU.mult),
                   reads=[xt_res, "rs%d" % par, atag], writes=["t1"])
              P.op("dve", lambda e: e.tensor_tensor(out=xn, in0=t1, in1=ADA[:, sslot * 1024:(sslot + 1) * 1024], op=ALU.add),
                   reads=["t1", stag], writes=["xn%d" % par])
              for kc in range(8):
                  P.op("pe", lambda e, kc=kc: e.transpose(out=PTR[:, kc * 128:(kc + 1) * 128], in_=xn[:, kc * 128:(kc + 1) * 128],
                                                           identity=ident[:]),
                       reads=["xn%d" % par, "ident"], writes=["PTR"])
              P.op("act", lambda e: e.activation(out=dst3, in_=v3(PTR[:, 0:1024], 128), func=AF.Copy),
                   reads=["PTR"], writes=[dst_res])

          chk(1)
          def pre_a(t):
              par = t % 2
              xt = F32B[:, par * 1024:(par + 1) * 1024]
              xres = "xt%d" % par
              ssq = SM[:, 8 + par:9 + par]
              rs = SM[:, 10 + par:11 + par]
              xn = XN[:, par * 1024:(par + 1) * 1024]
              t1 = F32B[:, 2048:3072]
              P.dma("sp", xt, xf[t * 128:(t + 1) * 128, :], writes=[xres], key=xres)
              P.op("dve", lambda e: e.memset(ssq, 0.0), writes=["ssq%d" % par])
              P.op("act", lambda e: e.activation(out=xn, in_=xt, func=AF.Square, accum_out=ssq),
                   reads=[xres, "ssq%d" % par], writes=["xn%d" % par, "ssq%d" % par])
              P.op("act", lambda e: e.activation(out=rs, in_=ssq, func=AF.Sqrt, scale=1.0 / D, bias=EPS),
                   reads=["ssq%d" % par, "SM"], writes=["rs%d" % par])
              P.op("dve", lambda e: e.reciprocal(out=rs, in_=rs), reads=["rs%d" % par], writes=["rs%d" % par])
              P.op("dve", lambda e: e.scalar_tensor_tensor(out=t1, in0=xt, scalar=rs, in1=ADA[:, 1024:2048],
                                                            op0=ALU.mult, op1=ALU.mult),
                   reads=[xres, "rs%d" % par, "ada1"], writes=["t1"])
              P.op("dve", lambda e: e.tensor_tensor(out=xn, in0=t1, in1=ADA[:, 0:1024], op=ALU.add),
                   reads=["t1", "ada0"], writes=["xn%d" % par])

          def pre_b(t):
              par = t % 2
              g, tt = t // 4, t % 4
              xn = XN[:, par * 1024:(par + 1) * 1024]
              hbuf = BIGD[:, (g % 2) * 4096:(g % 2 + 1) * 4096]
              hres = "hT%d" % (g % 2)
              for kc in range(8):
                  P.op("pe", lambda e, kc=kc: e.transpose(out=PTR[:, kc * 128:(kc + 1) * 128], in_=xn[:, kc * 128:(kc + 1) * 128],
                                                           identity=ident[:]),
                       reads=["xn%d" % par, "ident"], writes=["PTR"])
              P.op("act", lambda e: e.activation(out=v3(hbuf, 512)[:, :, tt * 128:(tt + 1) * 128], in_=v3(PTR[:, 0:1024], 128), func=AF.Copy),
                   reads=["PTR"], writes=[hres])
              if tt == 3:
                  P.dma("sp", hTd[g], hbuf, reads=[hres], key="hst%d" % (g % 2))

          pre_a(0)
          for t in range(NT):
              if t + 1 < NT:
                  pre_a(t + 1)
              pre_b(t)
          P.barrier()

          chk(2)
          ada_seg(5, 0, "ada0", load_cbc=True)
          P.dma("sp", gtfd, ADA[:, 0:1024], reads=["ada0"], key="gtfst")
          ada_seg(3, 0, "ada0")
          ada_seg(4, 1, "ada1")
          ada_scale(1, "ada1", gffn_d)
          P.barrier()
          if KDEBUG:
              P.dma("sp", dbg_ada2, ADA[:, :], key="dbg_ada2")
              P.barrier()

          KT = BIGA
          V = BIGB
          WK = BIGC[:, 0:2048]
          WV = BIGC[:, 2048:4096]
          WQ = XNX[:, 2560:4608]
          KMB = XNX[:, 4608:4736]
          QT = BIGC[:, 4096:8192]
          PTring = [XNX[:, i * 512:(i + 1) * 512] for i in range(4)]
          YTOK = XNX[:, 2048:2560]
          BT = F32A[:, 0:3072]
          NTMP = [F32A[:, 3072 + i * 512: 3072 + (i + 1) * 512] for i in range(2)] + [F32A[:, 6144:6656]]
          SSLOT = [S2[:, 0:512], S2[:, 512:1024], PM[:, 0:512]]
          SELB = [F32A[:, 5400:5656], F32A[:, 7424:7680]]
          SRES = ["S0", "S1", "PM"]
          ACC = F32A[:, 4096:4096 + 1040]
          KMS = F32A[:, 5200:5200 + 64]
          GSM = F32A[:, 5300:5300 + 32]
          M8 = F32A[:, 5340:5348]
          THR = F32A[:, 5350:5351]
          SEL = F32A[:, 5400:5400 + 2 * 4 * 32]
          OTMP = F32A[:, 5700:5700 + 128]
          RL = F32A[:, 5900:5908]

          for p in range(4):
              is_diff = p < 2
              if is_diff:
                  qcol, kcol, vcol = 0 + p * 256, 512 + p * 256, 1024 + p * 256
                  W = 129
                  heads = [2 * p, 2 * p, 2 * p + 1, 2 * p + 1]
                  vcols = [(0, 129), (0, 129), (130, 129), (130, 129)]
              else:
                  pp = p - 2
                  qcol, kcol, vcol = 1536 + pp * 256, 2048 + pp * 256, 2560 + pp * 256
                  W = 65
                  heads = [4 + 4 * pp + i for i in range(4)]
                  vcols = [(i * 66, 65) for i in range(4)]
              def load_pass_weights(pn):
                  if pn < 2:
                      cols = (512 + pn * 256, 1024 + pn * 256, 0 + pn * 256)
                  else:
                      cols = (2048 + (pn - 2) * 256, 2560 + (pn - 2) * 256, 1536 + (pn - 2) * 256)
                  for (wt, col, tag) in ((WK, cols[0], "WK"), (WV, cols[1], "WV"), (WQ, cols[2], "WQ")):
                      P.dma("pool", v3(wt, 256), w_in[:, col:col + 256].rearrange("(kc p) n -> p kc n", p=128),
                            writes=[tag], key=tag)

              if p == 0:
                  load_pass_weights(0)
              for i, h in enumerate(sorted(set(heads))):
                  P.dma("sp", BT[:, i * 768:(i + 1) * 768], bt_d[:, h * 768:(h + 1) * 768], writes=["BT%d" % i], key="BT%d" % i)
                  P.op("dve", lambda e, i=i, h=h: e.tensor_scalar(out=BT[:, i * 768:(i + 1) * 768], in0=BT[:, i * 768:(i + 1) * 768],
                                                                  scalar1=b31[:, h:h + 1], scalar2=None, op0=ALU.subtract),
                       reads=["BT%d" % i, "b31"], writes=["BT%d" % i])
              hidx = {h: i for i, h in enumerate(sorted(set(heads)))}
              ncol = 2 if is_diff else 4
              stride = 130 if is_diff else 66
              for i in range(ncol):
                  P.op("dve", lambda e, i=i, stride=stride: e.tensor_copy(
                      out=v3(V[:, 0:NT * 264], 264)[:, :, i * stride + stride - 2], in_=valid[:, :]),
                      reads=["valid"], writes=["Vones"])
              for g in range(NG):
                  hbuf = BIGD[:, (g % 2) * 4096:(g % 2 + 1) * 4096]
                  hres = "hT%d" % (g % 2)
                  P.dma("sp", hbuf, hTd[g], writes=[hres], key="hld%d" % (g % 2))
                  for c in range(2):
                      pb = PO[c]
                      for kc in range(8):
                          P.op("pe", lambda e, kc=kc, c=c, pb=pb, hbuf=hbuf: e.matmul(
                              pb[:, 0:512], lhsT=WK[:, kc * 256 + c * 128: kc * 256 + c * 128 + 128],
                              rhs=hbuf[:, kc * 512:(kc + 1) * 512], start=(kc == 0), stop=(kc == 7)),
                              reads=["WK", hres], writes=["PO%d" % c])
                      P.op("act", lambda e, c=c, pb=pb, g=g: e.activation(
                          out=KT[:, c * SEQ + g * 512: c * SEQ + g * 512 + 512], in_=pb[:, 0:512], func=AF.Copy),
                          reads=["PO%d" % c], writes=["KT%d_%d" % (c, g)])
                      if not is_diff:
                          for bb in range(2):
                              P.op("dve", lambda e, c=c, pb=pb, g=g, bb=bb: e.reduce_sum(
                                  out=KMS[:, c * 32 + 2 * g + bb: c * 32 + 2 * g + bb + 1],
                                  in_=KT[:, c * SEQ + g * 512 + bb * 256: c * SEQ + g * 512 + bb * 256 + 256], axis=AX.X),
                                  reads=["KT%d_%d" % (c, g)], writes=["KMS"])
                  for tt in range(4):
                      t = 4 * g + tt
                      pb = PO[2 + tt % 2]
                      pres = "PO%d" % (2 + tt % 2)
                      for kc in range(8):
                          P.op("pe", lambda e, kc=kc, tt=tt, pb=pb, hbuf=hbuf: e.matmul(
                              pb[:, 0:256], lhsT=hbuf[:, kc * 512 + tt * 128: kc * 512 + tt * 128 + 128],
                              rhs=WV[:, kc * 256:(kc + 1) * 256], start=(kc == 0), stop=(kc == 7)),
                              reads=["WV", hres], writes=[pres])
                      if is_diff:
                          pairs = [(V[:, t * 264 + cc * 130: t * 264 + cc * 130 + 128], pb[:, cc * 128:(cc + 1) * 128]) for cc in range(2)]
                      else:
                          pairs = [(V[:, t * 264 + jj * 66: t * 264 + jj * 66 + 64], pb[:, jj * 64:(jj + 1) * 64]) for jj in range(4)]
                      for (dst, src) in pairs:
                          P.op("dve", lambda e, dst=dst, src=src, t=t: e.tensor_scalar(
                              out=dst, in0=src, scalar1=valid[:, t:t + 1], scalar2=None, op0=ALU.mult),
                              reads=[pres, "valid"], writes=["V%d" % t])
                  if g % 2 == 1:
                      s = g // 2
                      for c in range(2):
                          pb = PO[c]
                          for kc in range(8):
                              P.op("pe", lambda e, kc=kc, c=c, pb=pb, hbuf=hbuf: e.matmul(
                                  pb[:, 0:256], lhsT=WQ[:, kc * 256 + c * 128: kc * 256 + c * 128 + 128],
                                  rhs=hbuf[:, kc * 512 + 256:(kc + 1) * 512], start=(kc == 0), stop=(kc == 7)),
                                  reads=["WQ", hres], writes=["PO%d" % c])
                          P.op("act", lambda e, c=c, pb=pb, s=s: e.activation(
                              out=QT[:, c * 2048 + s * 256: c * 2048 + s * 256 + 256], in_=pb[:, 0:256], func=AF.Copy),
                              reads=["PO%d" % c], writes=["QT%d" % s])
              if not is_diff:
                  for c in range(2):
                      for u in range(2):
                          P.op("dve", lambda e, c=c, u=u: e.tensor_scalar(
                              out=KMB[:, c * 64 + u * 32: c * 64 + u * 32 + 32], in0=KMS[:, c * 32:(c + 1) * 32],
                              scalar1=UMASK[:, u:u + 1], scalar2=1.0 / 256, op0=ALU.mult, op1=ALU.mult),
                              reads=["KMS", "umask"], writes=["KMB"])

              if p + 1 < 4:
                  load_pass_weights(p + 1)
              chk(3 + p * 2)
              QBD = BIGD
              P.op("pool", lambda e: e.memset(QBD[:, 0:2048], 0.0), writes=["QBDz", "hT0"])
              step = 0
              blkc = 0
              pre_issued = False
              deferred = []
              deferred_mid = []
              for s in range(NQB):
                  F = 4 * s + 3
                  nkt = 2 * F + 2
                  ktres = ["KT%d_%d" % (c, g) for c in range(2) for g in range(NG)]
                  def gating_group(st, gi):
                      qt_, c_ = divmod(gi, 2)
                      selb = SELB[st % 2]
                      selres = "SEL%d" % (st % 2)
                      gb = PO[3][:, gi * 64:(gi + 1) * 64]
                      P.op("pe", lambda e, c_=c_, qt_=qt_, st=st, gb=gb: e.matmul(
                          gb, lhsT=QT[:, c_ * 2048 + st * 256 + qt_ * 128: c_ * 2048 + st * 256 + qt_ * 128 + 128],
                          rhs=KMB[:, c_ * 64:(c_ + 1) * 64], start=True, stop=True),
                          reads=["QT%d" % st, "KMB"], writes=["PO3"])
                      for u in range(2):
                          j = 2 * c_ + u
                          gp = gb[:, u * 32:(u + 1) * 32]
                          P.op("dve", lambda e, gp=gp, st=st: e.tensor_tensor(
                              out=GSM, in0=gp, in1=blkbias[:, st * 32:(st + 1) * 32], op=ALU.add),
                              reads=["PO3", "blkbias"], writes=["GSM"])
                          P.op("dve", lambda e: e.max(out=M8, in_=GSM), reads=["GSM"], writes=["M8"])
                          P.op("dve", lambda e: e.tensor_scalar(out=THR, in0=M8[:, 2:3], scalar1=-1e29, scalar2=None, op0=ALU.max),
                               reads=["M8"], writes=["THR"])
                          P.op("dve", lambda e, qt_=qt_, j=j, selb=selb: e.tensor_scalar(
                              out=selb[:, (qt_ * 4 + j) * 32:(qt_ * 4 + j + 1) * 32], in0=GSM, scalar1=THR, scalar2=None, op0=ALU.is_ge),
                              reads=["GSM", "THR"], writes=[selres])

                  if (not is_diff) and s == 0:
                      for gi in range(4):
                          gating_group(0, gi)
                  SELc = SELB[s % 2]
                  SELcres = "SEL%d" % (s % 2)
                  par_s = s % 2

                  def qbd_stage(st):
                      for c_ in range(2):
                          qoff_ = ((st % 2) * 2 + c_) * 512
                          for u in range(2):
                              P.op("pool", lambda e, c_=c_, u=u, st=st, qoff_=qoff_: e.tensor_copy(
                                  out=QBD[u * 64:(u + 1) * 64, qoff_ + u * 256: qoff_ + u * 256 + 256],
                                  in_=QT[u * 64:(u + 1) * 64, c_ * 2048 + st * 256: c_ * 2048 + st * 256 + 256]),
                                  reads=["QT%d" % st, "QBDz"], writes=["QBD%d_%d" % (st % 2, c_)])

                  if s == 0:
                      qbd_stage(0)
                  for c in range(2):
                    qoff = (par_s * 2 + c) * 512
                    qres = "QBD%d_%d" % (par_s, c)

                    def flags(kt):
                        if is_diff:
                            return kt == 0, kt == nkt - 1
                        return kt % 2 == 0, kt % 2 == 1

                    def emit_qk(kt, slot, c=c, qoff=qoff, qres=qres):
                        P.op("pe", lambda e, c=c, kt=kt, slot=slot, qoff=qoff: e.matmul(
                            SSLOT[slot],
                            lhsT=KT[:, c * SEQ + kt * 128: c * SEQ + kt * 128 + 128],
                            rhs=QBD[:, qoff: qoff + 512], start=True, stop=True),
                            reads=["KT%d_%d" % (c, kt // 4), qres], writes=[SRES[slot]])

                    if c == 0:
                        nxt = (1, (par_s * 2 + 1) * 512, "QBD%d_1" % par_s)
                    elif s + 1 < NQB:
                        nxt = (0, (((s + 1) % 2) * 2) * 512, "QBD%d_0" % ((s + 1) % 2))
                    else:
                        nxt = None

                    def emit_exp(kt, slot, pt, ptres, c=c, F=F):
                        r = 2 * F - kt
                        sres = SRES[slot]
                        if r <= 1:
                            ridx = 1 - r
                            nt_ = NTMP[slot]
                            for u in range(2):
                                hi = hidx[heads[2 * c + u]]
                                P.op("dve", lambda e, u=u, hi=hi, ridx=ridx, nt_=nt_, slot=slot: e.scalar_tensor_tensor(
                                    out=nt_[:, u * 256:(u + 1) * 256], in0=SSLOT[slot][:, u * 256:(u + 1) * 256],
                                    scalar=0.125, in1=BT[:, hi * 768 + ridx * 256: hi * 768 + ridx * 256 + 256],
                                    op0=ALU.mult, op1=ALU.add), reads=[sres, "BT%d" % hi], writes=["NTMP%d" % slot])
                            P.op("act", lambda e, pt=pt, nt_=nt_: e.activation(out=pt, in_=nt_, func=AF.Exp),
                                 reads=["NTMP%d" % slot], writes=[ptres])
                        else:
                            P.op("act", lambda e, pt=pt, slot=slot: e.activation(out=pt, in_=SSLOT[slot],
                                                                                 func=AF.Exp, scale=0.125),
                                 reads=[sres], writes=[ptres])

                    def acc_loc(kt, qt, u, blk_base=blkc):
                        if is_diff:
                            return PO[qt * 2 + u], 0, "PO%d" % (qt * 2 + u), True
                        b = (blk_base + kt // 2) % 3
                        return PO[b], (qt * 2 + u) * W, "PO%d" % b, (qt == 0 and u == 0)

                    def emit_pv(kt, pt, ptres, c=c):
                        gstart, gstop = flags(kt)
                        for qt in range(2):
                            for u in range(2):
                                po, col0, pores, first = acc_loc(kt, qt, u)
                                voff, vw = vcols[2 * c + u]
                                st_ = gstart and first
                                P.op("pe", lambda e, po=po, col0=col0, pt=pt, u=u, qt=qt, kt=kt, voff=voff, vw=vw, st_=st_, gstop=gstop: e.matmul(
                                    po[:, col0:col0 + vw], lhsT=pt[:, u * 256 + qt * 128: u * 256 + qt * 128 + 128],
                                    rhs=V[:, kt * 264 + voff: kt * 264 + voff + vw], start=st_, stop=gstop, skip_group_check=True),
                                    reads=[ptres, "V%d" % kt, "Vones"], writes=[pores])

                    def emit_fold(kt, first_acc, c=c, F=F):
                        n = kt // 2
                        own = n == F
                        for qt in range(2):
                            for u in range(2):
                                po, col0, pores, _first = acc_loc(kt, qt, u)
                                j = 2 * c + u
                                a = ACC[:, (qt * 4 + j) * W:(qt * 4 + j) * W + W]
                                ares = "ACC%d_%d" % (qt, j)
                                src = po[:, col0:col0 + W]
                                if is_diff or (first_acc and own):
                                    P.op("dve", lambda e, a=a, src=src: e.tensor_copy(out=a, in_=src), reads=[pores], writes=[ares])
                                elif first_acc:
                                    sc_ = SELc[:, (qt * 4 + j) * 32 + n:(qt * 4 + j) * 32 + n + 1]
                                    P.op("dve", lambda e, a=a, src=src, sc_=sc_: e.tensor_scalar(
                                        out=a, in0=src, scalar1=sc_, scalar2=None,
                                        op0=ALU.mult), reads=[pores, SELcres], writes=[ares])
                                elif own:
                                    P.op("dve", lambda e, a=a, src=src: e.tensor_tensor(out=a, in0=src, in1=a, op=ALU.add),
                                         reads=[pores, ares], writes=[ares])
                                else:
                                    sc_ = SELc[:, (qt * 4 + j) * 32 + n:(qt * 4 + j) * 32 + n + 1]
                                    P.op("dve", lambda e, a=a, src=src, sc_=sc_: e.scalar_tensor_tensor(
                                        out=a, in0=src, scalar=sc_, in1=a,
                                        op0=ALU.mult, op1=ALU.add), reads=[pores, SELcres, ares], writes=[ares])

                    slots = [(step + i) % 3 for i in range(nkt + 2)]
                    pts = [(step + i) % 4 for i in range(nkt)]
                    step += nkt
                    blkc += nkt // 2
                    first_acc = True
                    if c == 1 and s + 1 < NQB:
                        qbd_stage(s + 1)
                    if not pre_issued:
                        emit_qk(0, slots[0])
                        emit_qk(1, slots[1])
                    pre_issued = False
                    for kt in range(nkt):
                        pt = PTring[pts[kt]]
                        ptres = "PT%d" % pts[kt]
                        emit_exp(kt, slots[kt], pt, ptres)
                        if kt + 2 < nkt:
                            emit_qk(kt + 2, slots[kt + 2])
                        elif nxt is not None:
                            emit_qk(kt + 2 - nkt, slots[kt + 2], c=nxt[0], qoff=nxt[1], qres=nxt[2])
                            pre_issued = True
                        emit_pv(kt, pt, ptres)
                        if flags(kt)[1]:
                            emit_fold(kt, first_acc)
                            first_acc = False
                            if (not is_diff) and c == 1 and kt // 2 < 4 and s + 1 < NQB:
                                gating_group(s + 1, kt // 2)
                        if c == 0 and kt == 7:
                            while deferred_mid:
                                deferred_mid.pop(0)()
                        if kt == 3 and c == (1 if is_diff else 0):
                            while deferred:
                                deferred.pop(0)()
                  if is_diff:
                      OT4 = F32A[:, 6656:7168]
                      SS4 = F32A[:, 7168:7172]
                      RS4 = F32A[:, 7172:7176]
                      JK = F32A[:, 7296:7424]
                      for qt in range(2):
                          for c in range(2):
                              k = qt * 2 + c
                              rl = F32A[:, 7200 + k * 4: 7200 + k * 4 + 4]
                              ot = OT4[:, k * 128:(k + 1) * 128]
                              a1 = ACC[:, (qt * 4 + 2 * c) * W:(qt * 4 + 2 * c) * W + W]
                              a2 = ACC[:, (qt * 4 + 2 * c + 1) * W:(qt * 4 + 2 * c + 1) * W + W]
                              ar = ["ACC%d_%d" % (qt, 2 * c), "ACC%d_%d" % (qt, 2 * c + 1)]
                              P.op("dve", lambda e, a1=a1, rl=rl: e.reciprocal(out=rl[:, 0:1], in_=a1[:, 128:129]), reads=ar, writes=["RLa%d" % k])
                              P.op("dve", lambda e, a2=a2, rl=rl: e.reciprocal(out=rl[:, 1:2], in_=a2[:, 128:129]), reads=ar, writes=["RLb%d" % k])
                              P.op("dve", lambda e, rl=rl: e.tensor_tensor(out=rl[:, 2:3], in0=rl[:, 1:2], in1=NEGLAM, op=ALU.mult),
                                   reads=["RLb%d" % k, "neglam"], writes=["RLc%d" % k])
                              P.op("dve", lambda e, a1=a1, rl=rl, ot=ot: e.tensor_scalar(out=ot, in0=a1[:, 0:128], scalar1=rl[:, 0:1], scalar2=None, op0=ALU.mult),
                                   reads=ar + ["RLa%d" % k], writes=["OT%d" % k])
                              P.op("dve", lambda e, a2=a2, rl=rl, ot=ot: e.scalar_tensor_tensor(out=ot, in0=a2[:, 0:128], scalar=rl[:, 2:3], in1=ot,
                                                                                                  op0=ALU.mult, op1=ALU.add),
                                   reads=ar + ["RLc%d" % k, "OT%d" % k], writes=["OT%d" % k])
                              P.op("dve", lambda e, ot=ot: e.tensor_tensor(out=JK, in0=ot, in1=ot, op=ALU.mult), reads=["OT%d" % k], writes=["JK"])
                              P.op("dve", lambda e, k=k: e.reduce_sum(out=SS4[:, k:k + 1], in_=JK, axis=AX.X), reads=["JK"], writes=["SS4"])

                      def fin_mid(s=s):
                          P.op("act", lambda e: e.activation(out=RS4, in_=SS4, func=AF.Sqrt, scale=1.0 / (128 * LAMF * LAMF), bias=EPS2),
                               reads=["SS4", "SM"], writes=["RS4"])
                          P.op("dve", lambda e: e.reciprocal(out=RS4, in_=RS4), reads=["RS4"], writes=["RS4"])
                          for qt in range(2):
                              for c in range(2):
                                  k = qt * 2 + c
                                  P.op("dve", lambda e, c=c, qt=qt, k=k: e.scalar_tensor_tensor(
                                      out=XNX[:, 2048 + qt * 256 + c * 128: 2048 + qt * 256 + c * 128 + 128], in0=OT4[:, k * 128:(k + 1) * 128],
                                      scalar=RS4[:, k:k + 1], in1=gsub[:], op0=ALU.mult, op1=ALU.mult),
                                      reads=["OT%d" % k, "RS4", "gsub"], writes=["YTOK%d" % qt])
                      deferred_mid.append(fin_mid)
                  else:
                      for qt in range(2):
                          for j in range(4):
                              a = ACC[:, (qt * 4 + j) * W:(qt * 4 + j) * W + W]
                              ares = "ACC%d_%d" % (qt, j)
                              P.op("dve", lambda e, a=a, j=j: e.reciprocal(out=RL[:, j:j + 1], in_=a[:, 64:65]), reads=[ares], writes=["RLm%d" % j])
                              P.op("dve", lambda e, a=a, j=j, qt=qt: e.tensor_scalar(
                                  out=XNX[:, 2048 + qt * 256 + j * 64: 2048 + qt * 256 + j * 64 + 64], in0=a[:, 0:64], scalar1=RL[:, j:j + 1],
                                  scalar2=None, op0=ALU.mult),
                                   reads=[ares, "RLm%d" % j], writes=["YTOK%d" % qt])
                  for qt in range(2):
                      def fin_pe(qt=qt, s=s, ch0=2 * p):
                          for c in range(2):
                              P.op("pe", lambda e, c=c, qt=qt: e.transpose(
                                  out=PTR[:, qt * 256 + c * 128: qt * 256 + c * 128 + 128],
                                  in_=XNX[:, 2048 + qt * 256 + c * 128: 2048 + qt * 256 + c * 128 + 128],
                                  identity=ident[:]), reads=["YTOK%d" % qt, "ident"], writes=["PTR"])
                          P.op("act", lambda e, ch0=ch0, s=s, qt=qt: e.activation(
                              out=v3(YT[:, ch0 * 2048:(ch0 + 2) * 2048], 2048)[:, :, s * 256 + qt * 128: s * 256 + qt * 128 + 128],
                              in_=v3(PTR[:, qt * 256:(qt + 1) * 256], 128), func=AF.Copy), reads=["PTR"], writes=["YT%d" % s])
                      deferred.append(fin_pe)
                  if s == 0:
                      chk(50 + p)
                      if KDEBUG and p == 2:
                          P.barrier()
                          P.dma("sp", dbg_acc, F32A[:, 4096:6144], key="dbg_acc")
                          P.barrier()
              while deferred_mid:
                  deferred_mid.pop(0)()
              while deferred:
                  deferred.pop(0)()
              P.barrier()
              chk(4 + p * 2)

          if KDEBUG:
              P.dma("sp", dbg_yT, YT[:, :], key="dbg_yT")
              P.barrier()
          WGA = BIGA[:, 0:8192]
          WGB = BIGA[:, 8192:16384]
          WBA = BIGB[:, 0:4096]
          WBB = BIGB[:, 4096:8192]
          WO = BIGB[:, 8192:16384]
          P.dma("pool", v3(WGA, 1024), w_in[:, 3072:4096].rearrange("(kc p) n -> p kc n", p=128), writes=["WGA"], key="WGA")
          P.dma("pool", v3(WGB, 1024), w_in[:, 4096:5120].rearrange("(kc p) n -> p kc n", p=128), writes=["WGB"], key="WGB")
          P.dma("pool", v3(WBA, 1024), w_br_a.rearrange("(kc p) n -> p kc n", p=128), writes=["WBA"], key="WBA")
          P.dma("pool", v3(WBB, 1024), w_br_b.rearrange("(kc p) n -> p kc n", p=128), writes=["WBB"], key="WBB")
          P.dma("pool", v3(WO, 1024), w_out.rearrange("(kc p) n -> p kc n", p=128), writes=["WO"], key="WO")
          HQ = [BIGD[:, i * 2048:(i + 1) * 2048] for i in range(2)]
          MT = BIGD[:, 4096:6144]
          SG = [F32A[:, i * 256:(i + 1) * 256] for i in range(4)]
          X1 = [F32A[:, 2048 + i * 1024: 2048 + (i + 1) * 1024] for i in range(2)]
          LG = F32A[:, 4096:4096 + 32]
          EALL = F32A[:, 4160:4160 + 32]
          MTB = [BIGD[:, 4096:6144], BIGD[:, 6144:8192]]

          def mix_mloop(s):
              hq = HQ[s % 2]
              hqres = "HQ%d" % (s % 2)
              mt = MTB[s % 2]
              mtres = "MT%d" % (s % 2)
              P.dma("sp", v3(hq, 256), v3(hTd[2 * s + 1], 512)[:, :, 256:512], writes=[hqres], key=hqres)
              for m in range(8):
                  if m % 2 == 0:
                      pa, pga, pb_, pgb = S2[:, 0:256], S2[:, 256:512], S2[:, 512:768], S2[:, 768:1024]
                      ra_, rga_, rb_, rgb_ = "S2a", "S2b", "S2c", "S2d"
                  else:
                      pa, pga, pb_, pgb = PO[2][:, 0:256], PO[2][:, 256:512], PO[3][:, 0:256], PO[3][:, 256:512]
                      ra_, rga_, rb_, rgb_ = "P2a", "P2b", "P3a", "P3b"
                  for (dst, wt, nk, src, srcres, wres, ch_off, dres) in (
                          (pa, WBA, 4, YT, "YT%d" % s, "WBA", 0, ra_), (pga, WGA, 8, hq, hqres, "WGA", 0, rga_),
                          (pb_, WBB, 4, YT, "YT%d" % s, "WBB", 4, rb_), (pgb, WGB, 8, hq, hqres, "WGB", 0, rgb_)):
                      for kc in range(nk):
                          if src is YT:
                              rhs = YT[:, (ch_off + kc) * 2048 + s * 256:(ch_off + kc) * 2048 + s * 256 + 256]
                          else:
                              rhs = hq[:, kc * 256:(kc + 1) * 256]
                          P.op("pe", lambda e, dst=dst, wt=wt, kc=kc, m=m, rhs=rhs, nk=nk: e.matmul(
                              dst, lhsT=wt[:, kc * 1024 + m * 128: kc * 1024 + m * 128 + 128], rhs=rhs,
                              start=(kc == 0), stop=(kc == nk - 1)), reads=[wres, srcres], writes=[dres])
                  P.op("act", lambda e, pga=pga: e.activation(out=SG[0], in_=pga, func=AF.Sigmoid), reads=[rga_], writes=["SG0"])
                  P.op("act", lambda e, pgb=pgb: e.activation(out=SG[1], in_=pgb, func=AF.Sigmoid), reads=[rgb_], writes=["SG1"])
                  P.op("dve", lambda e, pa=pa: e.tensor_tensor(out=SG[2], in0=pa, in1=SG[0], op=ALU.mult), reads=[ra_, "SG0"], writes=["SG2"])
                  P.op("dve", lambda e, pb_=pb_: e.tensor_tensor(out=SG[3], in0=pb_, in1=SG[1], op=ALU.mult), reads=[rb_, "SG1"], writes=["SG3"])
                  P.op("dve", lambda e, m=m, mt=mt: e.tensor_tensor(out=mt[:, m * 256:(m + 1) * 256], in0=SG[2], in1=SG[3], op=ALU.add),
                       reads=["SG2", "SG3"], writes=[mtres])

          def mix_s1(s, qt):
              F = 4 * s + 3
              mt = MTB[s % 2]
              mtres = "MT%d" % (s % 2)
              tl = 2 * s + qt
              par = tl % 2
              xt = F32B[:, par * 1024:(par + 1) * 1024]
              row0 = F * 256 + qt * 128
              P.dma("sp", xt, xf[row0:row0 + 128, :], writes=["xt%d" % par], key="xt%d" % par)
              x1 = X1[par]
              x1res = "X1_%d" % par
              for half in range(2):
                  pz = PO[half]
                  for kc in range(8):
                      P.op("pe", lambda e, pz=pz, kc=kc, half=half: e.matmul(
                          pz[:, 0:512], lhsT=mt[:, kc * 256 + qt * 128: kc * 256 + qt * 128 + 128],
                          rhs=WO[:, kc * 1024 + half * 512: kc * 1024 + half * 512 + 512], start=(kc == 0), stop=(kc == 7)),
                          reads=[mtres, "WO"], writes=["PO%d" % half])
                  P.op("dve", lambda e, pz=pz, half=half: e.tensor_tensor(
                      out=x1[:, half * 512:(half + 1) * 512], in0=pz[:, 0:512], in1=ADA[:, 2048 + half * 512: 2048 + half * 512 + 512],
                      op=ALU.mult), reads=["PO%d" % half, "ada2"], writes=[x1res])
                  P.op("dve", lambda e, half=half: e.tensor_tensor(
                      out=x1[:, half * 512:(half + 1) * 512], in0=x1[:, half * 512:(half + 1) * 512], in1=xt[:, half * 512:(half + 1) * 512],
                      op=ALU.add), reads=[x1res, "xt%d" % par], writes=[x1res])
              P.dma("sp", x1d[tl * 128:(tl + 1) * 128, :], x1, reads=[x1res], key="x1st%d" % par)

          def mix_s2(s, qt):
              tl = 2 * s + qt
              par = tl % 2
              x1 = X1[par]
              x1res = "X1_%d" % par
              ssq = SM[:, 8 + par:9 + par]
              rs = SM[:, 10 + par:11 + par]
              xn = XN[:, par * 1024:(par + 1) * 1024]
              t1 = F32B[:, 2048:3072]
              P.op("dve", lambda e: e.memset(ssq, 0.0), writes=["ssq%d" % par])
              P.op("act", lambda e: e.activation(out=xn, in_=x1, func=AF.Square, accum_out=ssq),
                   reads=[x1res, "ssq%d" % par], writes=["xn%d" % par, "ssq%d" % par])
              P.op("act", lambda e: e.activation(out=rs, in_=ssq, func=AF.Sqrt, scale=1.0 / D, bias=EPS),
                   reads=["ssq%d" % par, "SM"], writes=["rs%d" % par])
              P.op("dve", lambda e: e.reciprocal(out=rs, in_=rs), reads=["rs%d" % par], writes=["rs%d" % par])
              P.op("dve", lambda e: e.scalar_tensor_tensor(out=t1, in0=x1, scalar=rs, in1=ADA[:, 1024:2048],
                                                            op0=ALU.mult, op1=ALU.mult),
                   reads=[x1res, "rs%d" % par, "ada1"], writes=["t1"])
              P.op("dve", lambda e: e.tensor_tensor(out=xn, in0=t1, in1=ADA[:, 0:1024], op=ALU.add),
                   reads=["t1", "ada0"], writes=["xn%d" % par])

          def mix_s3(s, qt):
              tl = 2 * s + qt
              par = tl % 2
              xn = XN[:, par * 1024:(par + 1) * 1024]
              for kc in range(8):
                  P.op("pe", lambda e, kc=kc: e.transpose(out=PTR[:, kc * 128:(kc + 1) * 128], in_=xn[:, kc * 128:(kc + 1) * 128],
                                                           identity=ident[:]),
                       reads=["xn%d" % par, "ident"], writes=["PTR"])
              P.op("act", lambda e: e.activation(out=v3(YT[:, 0:16384], 2048)[:, :, s * 256 + qt * 128: s * 256 + qt * 128 + 128],
                                                 in_=v3(PTR[:, 0:1024], 128), func=AF.Copy),
                   reads=["PTR"], writes=["YT%d" % s])
              for kc in range(8):
                  P.op("pe", lambda e, kc=kc: e.matmul(
                      PM[:, 0:32], lhsT=YT[:, kc * 2048 + s * 256 + qt * 128: kc * 2048 + s * 256 + qt * 128 + 128],
                      rhs=WR[:, kc * 32:(kc + 1) * 32], start=(kc == 0), stop=(kc == 7)), reads=["YT%d" % s, "WR"], writes=["PM"])
              P.op("dve", lambda e: e.tensor_tensor(out=LG, in0=PM[:, 0:32], in1=BRT[:], op=ALU.add), reads=["PM", "BRT"], writes=["LG"])
              P.op("dve", lambda e: e.max(out=M8, in_=LG), reads=["LG"], writes=["M8"])
              P.op("dve", lambda e: e.tensor_scalar(out=RL[:, 0:1], in0=M8[:, 0:1], scalar1=-1.0, scalar2=None, op0=ALU.mult),
                   reads=["M8"], writes=["RL0"])
              P.op("act", lambda e: e.activation(out=EALL, in_=LG, func=AF.Exp, bias=RL[:, 0:1]), reads=["LG", "RL0"], writes=["EALL"])
              cb = COMB[:, tl * 32:(tl + 1) * 32]
              P.op("dve", lambda e: e.scalar_tensor_tensor(out=cb, in0=LG, scalar=M8[:, 3:4], in1=EALL, op0=ALU.is_ge, op1=ALU.mult),
                   reads=["LG", "M8", "EALL"], writes=["COMB%d" % tl])
              P.op("dve", lambda e: e.reduce_sum(out=RL[:, 1:2], in_=cb, axis=AX.X), reads=["COMB%d" % tl], writes=["RL1"])
              P.op("dve", lambda e: e.reciprocal(out=RL[:, 1:2], in_=RL[:, 1:2]), reads=["RL1"], writes=["RL1"])
              P.op("dve", lambda e: e.tensor_scalar(out=cb, in0=cb, scalar1=RL[:, 1:2], scalar2=None, op0=ALU.mult),
                   reads=["COMB%d" % tl, "RL1"], writes=["COMB%d" % tl])

          mix_mloop(0)
          for s in range(NQB):
              mix_s1(s, 0)
              mix_s1(s, 1)
              mix_s2(s, 0)
              mix_s2(s, 1)
              if s + 1 < NQB:
                  mix_mloop(s + 1)
              mix_s3(s, 0)
              mix_s3(s, 1)
          P.barrier()

          if KDEBUG:
              P.dma("sp", dbg_comb, COMB[:, :], key="dbg_comb")
              P.barrier()
          chk(20)
          WGU = [BIGA, BIGB]
          WD = BIGC
          ACT_T = BIGD
          ACCM = F32A
          TG = [F32B[:, i * 512:(i + 1) * 512] for i in range(6)]
          P.dma("pool", BDN[0:32, :], bdn_d, writes=["BDN"], key="c10")
          outs = []
          it = 0
          for half in range(2):
              def actT(fc, c0, n, half=half):
                  base = fc * 1024
                  return ACT_T[:, base + c0: base + c0 + n]

              for tl in range(8):
                  gt = half * 8 + tl
                  P.op("dve", lambda e, gt=gt: e.tensor_copy(out=XNX[:, 0:32], in_=COMB[:, gt * 32:(gt + 1) * 32]),
                       reads=["COMB"], writes=["cbf"])
                  P.op("pe", lambda e: e.transpose(out=PTR[0:32, 0:128], in_=XNX[:, 0:32], identity=ident[:]),
                       reads=["cbf", "ident"], writes=["PTR"])
                  P.op("act", lambda e: e.activation(out=XNX[0:32, 128:256], in_=PTR[0:32, 0:128], func=AF.Copy),
                       reads=["PTR"], writes=["cT"])
                  for hf in range(2):
                      pz = PO[hf]
                      P.op("pe", lambda e, pz=pz, hf=hf: e.matmul(
                          pz[:, 0:512], lhsT=XNX[0:32, 128:256], rhs=BDN[0:32, hf * 512:(hf + 1) * 512],
                          start=True, stop=True), reads=["cT", "BDN"], writes=["PO%d" % hf])
                      P.op("dve", lambda e, pz=pz, tl=tl, hf=hf: e.tensor_copy(
                          out=ACCM[:, tl * 1024 + hf * 512: tl * 1024 + hf * 512 + 512], in_=pz[:, 0:512]),
                          reads=["PO%d" % hf], writes=["ACCM%d" % tl])
              def load_w(itn):
                  exn = itn % NE
                  bufn = itn % 2
                  for hh in range(2):
                      P.dma("pool", v3(WGU[bufn][:, 0:16384], 2048)[:, :, hh * 1024:(hh + 1) * 1024],
                            w_gu[exn][:, hh * 1024:(hh + 1) * 1024].rearrange("(kc p) n -> p kc n", p=128),
                            writes=["WGU%d_%d" % (bufn, hh)], key="WGU%d_%d" % (bufn, hh))

              def load_wd(itn):
                  exn = itn % NE
                  P.dma("pool", v3(WD[:, 0:8192], 1024), w_dn[exn].rearrange("(kc p) n -> p kc n", p=128),
                        writes=["WD"], key="WD")

              if half == 0:
                  load_w(0)
                  load_wd(0)
              def emit_up(ex, buf, tg, fc, half=half, actT=actT):
                  wgu = WGU[buf]
                  tok0 = half * 1024 + tg * 512
                  if fc % 2 == 0:
                      pg, pgres, pu, pures = S2[:, 0:512], "S2a", S2[:, 512:1024], "S2c"
                  else:
                      pg, pgres, pu, pures = PM[:, 0:512], "PM", PO[3][:, 0:512], "PO3"
                  for (dst, hh, dres) in ((pg, 0, pgres), (pu, 1, pures)):
                      for kc in range(8):
                          P.op("pe", lambda e, dst=dst, hh=hh, kc=kc, fc=fc, tok0=tok0, wgu=wgu: e.matmul(
                              dst, lhsT=wgu[:, kc * 2048 + hh * 1024 + fc * 128: kc * 2048 + hh * 1024 + fc * 128 + 128],
                              rhs=YT[:, kc * 2048 + tok0: kc * 2048 + tok0 + 512], start=(kc == 0), stop=(kc == 7)),
                              reads=["WGU%d_%d" % (buf, hh), "H2T"], writes=[dres])
                  k4 = (fc % 2) * 3
                  g_, sg_, u_ = TG[k4], TG[k4 + 1], TG[k4 + 2]
                  a_ = g_
                  rg, rsg, ru = ["TG%d" % (k4 + i) for i in range(3)]
                  ra = rg
                  P.op("dve", lambda e, g_=g_, pg=pg, ex=ex, fc=fc: e.tensor_scalar(
                      out=g_, in0=pg, scalar1=BGU[:, ex * 16 + fc: ex * 16 + fc + 1], scalar2=7.0, op0=ALU.add, op1=ALU.min),
                      reads=[pgres, "BGU"], writes=[rg])
                  P.op("act", lambda e, g_=g_, sg_=sg_: e.activation(out=sg_, in_=g_, func=AF.Sigmoid, scale=1.702),
                       reads=[rg], writes=[rsg])
                  P.op("dve", lambda e, u_=u_, pu=pu, ex=ex, fc=fc: e.tensor_scalar(
                      out=u_, in0=pu, scalar1=BGU[:, ex * 16 + 8 + fc: ex * 16 + 8 + fc + 1], scalar2=8.0, op0=ALU.add, op1=ALU.min),
                      reads=[pures, "BGU"], writes=[ru])
                  P.op("pool", lambda e, a_=a_, g_=g_, sg_=sg_: e.tensor_tensor(out=a_, in0=g_, in1=sg_, op=ALU.mult),
                       reads=[rg, rsg], writes=[ra])
                  dstA = actT(fc, tg * 512, 512)
                  P.op("dve", lambda e, a_=a_, u_=u_, dstA=dstA: e.scalar_tensor_tensor(
                      out=dstA, in0=u_, scalar=-6.0, in1=a_,
                      op0=ALU.max, op1=ALU.mult), reads=[ra, ru], writes=["ACT%d" % tg])

              def emit_down(ex, tg, half=half, actT=actT):
                  wd = WD
                  for tt in range(4):
                      tl = tg * 4 + tt
                      gt = half * 8 + tl
                      for hf in range(2):
                          pz = PO[(tt * 2 + hf) % 3]
                          pzres = "PO%d" % ((tt * 2 + hf) % 3)
                          for fc in range(8):
                              lA = actT(fc, tg * 512 + tt * 128, 128)
                              P.op("pe", lambda e, pz=pz, fc=fc, hf=hf, wd=wd, lA=lA: e.matmul(
                                  pz[:, 0:512], lhsT=lA,
                                  rhs=wd[:, fc * 1024 + hf * 512: fc * 1024 + hf * 512 + 512], start=(fc == 0), stop=(fc == 7)),
                                  reads=["ACT%d" % tg, "WD"], writes=[pzres])
                          acc = ACCM[:, tl * 1024 + hf * 512: tl * 1024 + hf * 512 + 512]
                          P.op("dve", lambda e, pz=pz, acc=acc, gt=gt, ex=ex: e.scalar_tensor_tensor(
                              out=acc, in0=pz[:, 0:512], scalar=COMB[:, gt * 32 + ex: gt * 32 + ex + 1], in1=acc,
                              op0=ALU.mult, op1=ALU.add), reads=[pzres, "ACCM%d" % tl], writes=["ACCM%d" % tl])

              seq = [(ex, tg) for ex in range(NE) for tg in range(2)]
              hoisted = False
              for idx, (ex, tg) in enumerate(seq):
                  itn = half * NE + ex
                  buf = itn % 2
                  if idx == 0 and itn + 1 < 2 * NE:
                      load_w(itn + 1)
                  for fc in range(8):
                      if fc == 0 and hoisted:
                          continue
                      emit_up(ex, buf, tg, fc)
                  hoisted = False
                  if idx + 1 < len(seq):
                      ex2, tg2 = seq[idx + 1]
                      itn2 = half * NE + ex2
                      if tg2 == 0 and itn2 + 1 < 2 * NE:
                          load_w(itn2 + 1)
                      emit_up(ex2, itn2 % 2, tg2, 0)
                      hoisted = True
                  emit_down(ex, tg)
                  if tg == 1 and itn + 1 < 2 * NE:
                      load_wd(itn + 1)
              P.barrier()
              if half == 0:
                  P.dma("sp", ADA[:, 2048:3072], gfin_d, writes=["ada2"], key="gfin")
                  P.dma("sp", ADA[:, 0:1024], gtfd, writes=["ada3"], key="gtfld")
              def fin_a(tl, half=half):
                  gt = half * 8 + tl
                  par = gt % 2
                  xt = F32B[:, par * 1024:(par + 1) * 1024]
                  acc = ACCM[:, tl * 1024:(tl + 1) * 1024]
                  ares = "ACCM%d" % tl
                  ssq = SM[:, 8 + par:9 + par]
                  rs = SM[:, 10 + par:11 + par]
                  P.dma("sp", xt, x1d[gt * 128:(gt + 1) * 128, :], writes=["xt%d" % par], key="xt%d" % par)
                  P.op("pool", lambda e: e.tensor_tensor(out=acc, in0=acc, in1=ADA[:, 0:1024], op=ALU.mult),
                       reads=[ares, "ada3"], writes=[ares])
                  P.op("dve", lambda e: e.tensor_tensor(out=acc, in0=acc, in1=xt, op=ALU.add),
                       reads=[ares, "xt%d" % par], writes=[ares])
                  P.op("dve", lambda e: e.memset(ssq, 0.0), writes=["ssq%d" % par])
                  P.op("act", lambda e: e.activation(out=xt, in_=acc, func=AF.Square, accum_out=ssq),
                       reads=[ares, "ssq%d" % par], writes=["xt%d" % par, "ssq%d" % par])
                  P.op("act", lambda e: e.activation(out=rs, in_=ssq, func=AF.Sqrt, scale=1.0 / D, bias=EPS),
                       reads=["ssq%d" % par, "SM"], writes=["rs%d" % par])

              def fin_b(tl, half=half):
                  gt = half * 8 + tl
                  par = gt % 2
                  acc = ACCM[:, tl * 1024:(tl + 1) * 1024]
                  ares = "ACCM%d" % tl
                  rs = SM[:, 10 + par:11 + par]
                  P.op("dve", lambda e: e.reciprocal(out=rs, in_=rs), reads=["rs%d" % par], writes=["rs%d" % par])
                  P.op("dve", lambda e: e.scalar_tensor_tensor(out=acc, in0=acc, scalar=rs, in1=ADA[:, 2048:3072],
                                                                op0=ALU.mult, op1=ALU.mult),
                       reads=[ares, "rs%d" % par, "ada2"], writes=[ares])
                  outs.append(P.dma("sp", out_d[gt * 128:(gt + 1) * 128, :], acc, reads=[ares], key="ost%d" % tl))

              fin_a(0)
              for tl in range(8):
                  if tl + 1 < 8:
                      fin_a(tl + 1)
                  fin_b(tl)
              P.barrier()
      except _Stop:
        P.barrier()
        outs = [P.dma("sp", out_d[i * 128:(i + 1) * 128, :], F32A[:, i * 1024:(i + 1) * 1024], key="dbgout%d" % i) for i in range(8)]
        if KDEBUG:
            outs.append(P.dma("sp", dbg_yT, YT[:, :], key="dbg_yT"))
      print("ops per engine", {e: len(v) for e, v in P.ops.items()})
      P.emit(final_wait_ops=outs)
    return nc


_NC_CACHE = {}


def kernel(x, c, rel_bias, w_ada, b_ada, g_mix, w_in, lambda_q1, lambda_k1, lambda_q2, lambda_k2, subln_g,
           w_br_a, w_br_b, w_out, g_ffn, w_router, b_router, w_gate_up, b_gate_up, w_down, b_down, g_final):
    f = lambda a: np.ascontiguousarray(np.asarray(a, dtype=np.float32))
    x = f(x); c = f(c); rel_bias = f(rel_bias)
    bc = lambda v: np.ascontiguousarray(np.broadcast_to(np.asarray(v, np.float32).reshape(1, -1), (128, np.asarray(v).size)))
    kk = np.arange(128)[:, None]
    qq = np.arange(256)[None, :]
    bt = np.empty((128, 12, 3, 256), np.float32)
    for ri, r in enumerate((1, 0, -1)):
        dist = r * 128 + qq - kk
        bidx = _t5_bucket_np(dist)
        tile = rel_bias[bidx]
        tile = np.where((dist >= 0)[:, :, None], tile, np.float32(NEG))
        bt[:, :, ri, :] = np.transpose(tile, (0, 2, 1))
    bt = np.ascontiguousarray(bt.reshape(128, 12 * 3 * 256))
    b31 = bc(rel_bias[31])
    ident = np.eye(128, dtype=np.float32).astype(ml_dtypes.bfloat16)
    lam_bc = bc(np.concatenate([f(lambda_q1)[0], f(lambda_k1)[0], f(lambda_q2)[0], f(lambda_k2)[0]]))
    lam_init = 0.8 - 0.6 * math.exp(-0.3 * 0)
    gsub_bc = bc(f(subln_g)[0]) * np.float32(1.0)
    shared = {
        "bt": bt, "b31": b31, "ident": ident,
        "umask": np.ascontiguousarray(np.stack([(np.arange(128) < 64), (np.arange(128) >= 64)], axis=1).astype(np.float32)),
        "w_ada": f(w_ada)[0], "bada_bc": bc(f(b_ada)[0]), "gmix_bc": bc(f(g_mix)[0]), "gffn_bc": bc(f(g_ffn)[0]),
        "gfin_bc": bc(f(g_final)), "gsub_bc": gsub_bc, "lam_bc": lam_bc,
        "w_in": f(w_in)[0], "w_br_a": f(w_br_a)[0], "w_br_b": f(w_br_b)[0], "w_out": f(w_out)[0],
        "w_router": f(w_router)[0], "brouter_bc": bc(f(b_router)[0]),
        "w_gate_up": f(w_gate_up)[0],
        "bgu_col": np.ascontiguousarray(f(b_gate_up)[0].reshape(NE, 16, 128).transpose(2, 0, 1).reshape(128, NE * 16)),
        "w_down": f(w_down)[0], "b_down": f(b_down)[0],
    }
    in_maps = []
    for core in range(8):
        b, j = core // 4, core % 4
        nd = (3 - j) * 256
        xfr = np.zeros((SEQ, D), np.float32)
        xfr[nd:] = x[b, :SEQ - nd]
        tok = np.arange(SEQ).reshape(NT, 128).T
        valid = (tok >= nd).astype(np.float32)
        blk = np.full((NQB, 32), -1e30, np.float32)
        for s in range(NQB):
            F = 4 * s + 3
            blk[s, nd // 256:F] = 0.0
        m = dict(shared)
        m.update({
            "xf": xfr,
            "cbc": np.ascontiguousarray(np.broadcast_to(c[b].reshape(8, 128).T[:, :, None], (128, 8, 128)).reshape(128, 1024)),
            "valid": np.ascontiguousarray(valid),
            "blkbias": bc(blk.reshape(-1)),
        })
        in_maps.append(m)
    if "nc" not in _NC_CACHE:
        _NC_CACHE["nc"] = build_program()
    if STAGE <= 20:
        for m in in_maps:
            m.pop("w_gate_up"); m.pop("w_down")
    res = run_bass_kernel_spmd(_NC_CACHE["nc"], in_maps, core_ids=list(range(8)))
    if KDEBUG:
        _LAST["res"] = res.results
    out = np.empty((2, SEQ, D), np.float32)
    for core in range(8):
        b, j = core // 4, core % 4
        o = res.results[core]["out"]
        for s in range(NQB):
            blk0 = (4 * s + j) * 256
            out[b, blk0:blk0 + 256] = o[s * 256:(s + 1) * 256]
    return out
```

```python
import math
from contextlib import ExitStack

import numpy as np
import ml_dtypes
import concourse.bass as bass
import concourse.mybir as mybir
from concourse.bass_utils import run_bass_kernel_spmd

F32 = mybir.dt.float32
BF16 = mybir.dt.bfloat16
AF = mybir.ActivationFunctionType
ALU = mybir.AluOpType
AX = mybir.AxisListType

D = 1024
SEQ = 8192
NT = 64
NG = 16
NQB = 8
NE = 32
NEG = -30000.0
DEBUG = False
import os
STAGE = int(os.environ.get("KSTAGE", "99"))
KDEBUG = int(os.environ.get("KDEBUG", "0"))
_LAST = {}


class _Stop(Exception):
    pass

COMPUTE = ("pe", "act", "dve", "pool")


class Op:
    __slots__ = ("eng", "fn", "deps", "sem", "val", "need_inc", "is_dma")

    def __init__(self, eng, fn, is_dma=False):
        self.eng = eng
        self.fn = fn
        self.deps = []
        self.sem = None
        self.val = None
        self.need_inc = False
        self.is_dma = is_dma


class Prog:
    def __init__(self, nc, same_engine_raw=True):
        self.nc = nc
        self.ops = {e: [] for e in ("pe", "act", "dve", "pool", "sp")}
        self.last_w = {}
        self.readers = {}
        self.dma_keys = {}
        self.same_engine_raw = same_engine_raw
        self.dma_since_barrier = []

    def _add_dep(self, op, d, kind):
        if d is None or d is op:
            return
        if (not d.is_dma) and (not op.is_dma) and d.eng == op.eng:
            if d.eng == "pe":
                return
            if kind != "raw" or not self.same_engine_raw:
                return
        op.deps.append(d)
        d.need_inc = True

    def op(self, eng, fn, reads=(), writes=(), dma_key=None):
        is_dma = dma_key is not None
        o = Op(eng, fn, is_dma)
        for r in reads:
            self._add_dep(o, self.last_w.get(r), "raw")
        for w in writes:
            self._add_dep(o, self.last_w.get(w), "waw")
            for rd in self.readers.get(w, ()):
                self._add_dep(o, rd, "war")
        for r in reads:
            self.readers.setdefault(r, []).append(o)
        for w in writes:
            self.last_w[w] = o
            self.readers[w] = []
        if is_dma:
            o.sem = ("dma", dma_key)
            c = self.dma_keys.setdefault(dma_key, [0])
            c[0] += 16
            o.val = c[0]
            o.need_inc = True
            self.dma_since_barrier.append(o)
        self.ops[eng].append(o)
        return o

    def dma(self, eng, out, in_, reads=(), writes=(), key=None):
        return self.op(eng, lambda e: e.dma_start(out=out, in_=in_), reads, writes, dma_key=key)

    def barrier(self):
        lasts = [self.ops[e][-1] for e in COMPUTE if self.ops[e]]
        dmas = list(self.dma_since_barrier)
        self.dma_since_barrier = []
        for e in ("pe", "act", "dve", "pool", "sp"):
            o = Op(e, lambda eng: eng.nop())
            for d in lasts + dmas:
                if d.eng == e and not d.is_dma:
                    continue
                o.deps.append(d)
                d.need_inc = True
            self.ops[e].append(o)
        self.last_w = {}
        self.readers = {}

    def emit(self, final_wait_ops=()):
        nc = self.nc
        for e in COMPUTE:
            c = 0
            for o in self.ops[e]:
                if o.need_inc and not o.is_dma:
                    c += 1
                    o.sem = ("eng", e)
                    o.val = c
        sem_names = set()
        for e in self.ops:
            for o in self.ops[e]:
                if o.need_inc:
                    sem_names.add(o.sem)
        with ExitStack() as st:
            sems = {}
            for i, s in enumerate(sorted(sem_names, key=str)):
                sems[s] = st.enter_context(nc.semaphore("s%d" % i))
            block = st.enter_context(nc.Block())
            engmap = {"pe": "tensor", "act": "scalar", "dve": "vector", "pool": "gpsimd", "sp": "sync"}

            def make(ename):
                ops = self.ops[ename]

                def body(eng):
                    waited = {}
                    for o in ops:
                        need = {}
                        for d in o.deps:
                            if d.val > need.get(d.sem, 0):
                                need[d.sem] = d.val
                        for s, v in need.items():
                            if waited.get(s, 0) >= v:
                                continue
                            eng.wait_ge(sems[s], v)
                            waited[s] = v
                        ins = o.fn(eng)
                        if o.need_inc:
                            ins.then_inc(sems[o.sem], 16 if o.is_dma else 1)
                    if ename == "sp":
                        for o in final_wait_ops:
                            eng.wait_ge(sems[o.sem], o.val)
                return body

            for ename in self.ops:
                getattr(block, engmap[ename])(make(ename))


def _t5_bucket_np(dist):
    n = np.maximum(dist, 0)
    max_exact = 16
    nf = np.maximum(n, max_exact).astype(np.float32)
    large = max_exact + (np.log(nf / np.float32(max_exact)) / np.float32(math.log(128 / max_exact))
                         * np.float32(32 - max_exact)).astype(np.int32)
    large = np.minimum(large, 31)
    return np.where(n < max_exact, n, large)


def build_program():
    nc = bass.Bass("TRN2", target_bir_lowering=False)

    def din(name, shape, dt=F32):
        return nc.dram_tensor(name, list(shape), dt, kind="ExternalInput").ap()

    xf = din("xf", [SEQ, D])
    cbc_d = din("cbc", [128, 8 * 128])
    valid_d = din("valid", [128, NT])
    blkbias_d = din("blkbias", [128, NQB * 32])
    bt_d = din("bt", [128, 12 * 3 * 256])
    b31_d = din("b31", [128, 12])
    umask_d = din("umask", [128, 2])
    ident_d = din("ident", [128, 128], BF16)
    w_ada = din("w_ada", [D, 6 * D])
    bada_d = din("bada_bc", [128, 6 * D])
    gmix_d = din("gmix_bc", [128, D])
    gffn_d = din("gffn_bc", [128, D])
    gfin_d = din("gfin_bc", [128, D])
    gsub_d = din("gsub_bc", [128, 128])
    lam_d = din("lam_bc", [128, 4 * 64])
    w_in = din("w_in", [D, 5120])
    w_br_a = din("w_br_a", [512, D])
    w_br_b = din("w_br_b", [512, D])
    w_out = din("w_out", [D, D])
    w_router = din("w_router", [D, NE])
    brt_d = din("brouter_bc", [128, NE])
    w_gu = din("w_gate_up", [NE, D, 2 * D]) if STAGE > 20 else None
    bgu_d = din("bgu_col", [128, NE * 16])
    w_dn = din("w_down", [NE, D, D]) if STAGE > 20 else None
    bdn_d = din("b_down", [NE, D])
    out_d = nc.dram_tensor("out", [2048, D], F32, kind="ExternalOutput").ap()
    dk = "ExternalOutput" if KDEBUG else "Internal"
    hTd = nc.dram_tensor("hTd", [NG, 128, 8 * 512], BF16, kind=dk).ap()
    x1d = nc.dram_tensor("x1d", [2048, D], F32, kind=dk).ap()
    if KDEBUG:
        dbg_ada = nc.dram_tensor("dbg_ada", [128, 3072], F32, kind=dk).ap()
        dbg_ada2 = nc.dram_tensor("dbg_ada2", [128, 3072], F32, kind=dk).ap()
        dbg_yT = nc.dram_tensor("dbg_yT", [128, 16384], BF16, kind=dk).ap()
        dbg_comb = nc.dram_tensor("dbg_comb", [128, 512], F32, kind=dk).ap()
        dbg_sm = nc.dram_tensor("dbg_sm", [128, 64], F32, kind=dk).ap()
        dbg_acc = nc.dram_tensor("dbg_acc", [128, 2048], F32, kind=dk).ap()

    P = Prog(nc)

    def chk(k):
        if STAGE == k:
            raise _Stop()

    with ExitStack() as st:
      try:
          def sb(name, cols, dt):
              return st.enter_context(nc.sbuf_tensor("sb_" + name, [128, cols], dt))

          def ps(name, cols, dt):
              return st.enter_context(nc.psum_tensor("ps_" + name, [128, cols], dt))

          BIGA = sb("BIGA", 16384, BF16)
          BIGB = sb("BIGB", 16896, BF16)
          BIGC = sb("BIGC", 8192, BF16)
          BIGD = sb("BIGD", 8192, BF16)
          YT = sb("YT", 16384, BF16)
          F32A = sb("F32A", 8192, F32)
          F32B = sb("F32B", 3072, F32)
          ADA = sb("ADA", 3072, F32)
          XNX = sb("XNX", 4736, BF16)
          XN = XNX
          ident = sb("ident", 128, BF16)
          CBC = F32B[:, 2048:3072]
          valid = sb("valid", NT, F32)
          blkbias = sb("blkbias", NQB * 32, F32)
          b31 = sb("b31", 12, F32)
          UMASK = sb("umask", 2, F32)
          gsub = sb("gsub", 128, F32)
          lamt = F32A[:, 0:256]
          SM = sb("SM", 64, F32)
          COMB = sb("COMB", 16 * 32, F32)
          BGU = sb("BGU", NE * 16, F32)
          WR = sb("WR", 8 * 32, BF16)
          BRT = sb("BRT", NE, F32)
          BDN = XNX[:, 2560:3584]
          S2 = ps("S2", 1024, F32)
          PO = [ps("PO%d" % i, 512, F32) for i in range(4)]
          PM = ps("PM", 512, F32)
          PTR = ps("PTR", 1024, BF16)

          def v3(ap, inner):
              return ap.rearrange("p (a b) -> p a b", b=inner)

          EPS = SM[:, 0:1]
          NEGLAM = SM[:, 1:2]
          EPS2 = SM[:, 7:8]
          LAMF = 1.0 - (0.8 - 0.6 * math.exp(-0.3 * 0))

          P.dma("sp", ident[:], ident_d, writes=["ident"], key="c0")
          P.dma("sp", valid[:], valid_d, writes=["valid"], key="c2")
          P.dma("sp", blkbias[:], blkbias_d, writes=["blkbias"], key="c3")
          P.dma("sp", b31[:], b31_d, writes=["b31"], key="c4")
          P.dma("sp", UMASK[:], umask_d, writes=["umask"], key="c4b")
          P.dma("sp", gsub[:], gsub_d, writes=["gsub"], key="c5")
          P.dma("sp", lamt, lam_d, writes=["lamt"], key="c6")
          P.dma("sp", BGU[:], bgu_d, writes=["BGU"], key="c7")
          P.dma("sp", BRT[:], brt_d, writes=["BRT"], key="c8")
          P.dma("pool", WR[:], w_router.rearrange("(kc p) n -> p kc n", p=128), writes=["WR"], key="c9")
          P.op("dve", lambda e: e.memset(SM[:], 0.0), writes=["SM"])
          P.op("dve", lambda e: e.memset(EPS, 1e-5), writes=["SM"])
          P.op("dve", lambda e: e.memset(EPS2, 1e-5 / (LAMF * LAMF)), writes=["SM"])
          P.op("dve", lambda e: e.tensor_scalar(out=v3(BGU[:, :], 16)[:, :, 8:16], in0=v3(BGU[:, :], 16)[:, :, 8:16], scalar1=1.0,
                                                 scalar2=None, op0=ALU.add), reads=["BGU"], writes=["BGU"])
          P.op("dve", lambda e: e.tensor_tensor(out=F32B[:, 0:64], in0=lamt[:, 0:64], in1=lamt[:, 64:128], op=ALU.mult),
               reads=["lamt"], writes=["lamtmp"])
          P.op("dve", lambda e: e.reduce_sum(out=SM[:, 2:3], in_=F32B[:, 0:64], axis=AX.X), reads=["lamtmp"], writes=["lam1"])
          P.op("dve", lambda e: e.tensor_tensor(out=F32B[:, 64:128], in0=lamt[:, 128:192], in1=lamt[:, 192:256], op=ALU.mult),
               reads=["lamt"], writes=["lamtmp2"])
          P.op("dve", lambda e: e.reduce_sum(out=SM[:, 3:4], in_=F32B[:, 64:128], axis=AX.X), reads=["lamtmp2"], writes=["lam2"])
          P.op("act", lambda e: e.activation(out=SM[:, 4:6], in_=SM[:, 2:4], func=AF.Exp), reads=["lam1", "lam2", "SM"], writes=["lame"])
          P.op("dve", lambda e: e.tensor_tensor(out=SM[:, 6:7], in0=SM[:, 5:6], in1=SM[:, 4:5], op=ALU.subtract),
               reads=["lame"], writes=["lamd"])
          P.op("dve", lambda e: e.tensor_scalar(out=NEGLAM, in0=SM[:, 6:7], scalar1=-0.2, scalar2=None, op0=ALU.add),
               reads=["lamd"], writes=["neglam"])
          P.barrier()

          gtfd = nc.dram_tensor("gtfd", [128, D], F32).ap()

          def ada_seg(seg, slot, tag, load_cbc=False):
              if load_cbc:
                  P.dma("sp", CBC, cbc_d, writes=["CBC"], key="c1")
              for hh_ in range(2):
                  P.dma("sp", v3(F32A[:, 0:8192], 1024)[:, :, hh_ * 512:(hh_ + 1) * 512],
                        w_ada[:, seg * 1024 + hh_ * 512: seg * 1024 + hh_ * 512 + 512].rearrange("(kc p) n -> p kc n", p=128),
                        writes=["F32A_%d" % hh_], key="wada%d" % hh_)
              P.dma("sp", F32B[:, 0:1024], bada_d[:, seg * 1024:(seg + 1) * 1024], writes=["badaseg"], key="bada")
              for half in range(2):
                  pb = PO[half]
                  for kc in range(8):
                      P.op("pe", lambda e, kc=kc, half=half, pb=pb: e.matmul(
                          pb[:, 0:512], lhsT=CBC[:, kc * 128:(kc + 1) * 128],
                          rhs=F32A[:, kc * 1024 + half * 512: kc * 1024 + half * 512 + 512],
                          start=(kc == 0), stop=(kc == 7)), reads=["CBC", "F32A_%d" % half], writes=["PO%d" % half])
                  P.op("dve", lambda e, half=half, pb=pb: e.tensor_tensor(
                      out=ADA[:, slot * 1024 + half * 512: slot * 1024 + half * 512 + 512], in0=pb[:, 0:512],
                      in1=F32B[:, half * 512: half * 512 + 512], op=ALU.add),
                      reads=["PO%d" % half, "badaseg"], writes=[tag])

          def ada_scale(slot, tag, g_d):
              P.dma("sp", F32B[:, 1024:2048], g_d, writes=["gtmp"], key="gtmp")
              P.op("dve", lambda e: e.scalar_tensor_tensor(
                  out=ADA[:, slot * 1024:(slot + 1) * 1024], in0=ADA[:, slot * 1024:(slot + 1) * 1024], scalar=1.0,
                  in1=F32B[:, 1024:2048], op0=ALU.add, op1=ALU.mult), reads=[tag, "gtmp"], writes=[tag])

          chk(0)
          ada_seg(0, 0, "ada0", load_cbc=True)
          ada_seg(1, 1, "ada1")
          ada_scale(1, "ada1", gmix_d)
          ada_seg(2, 2, "ada2")
          P.barrier()
          if KDEBUG:
              P.dma("sp", dbg_ada, ADA[:, :], key="dbg_ada")
              P.dma("sp", dbg_sm, SM[:, :], key="dbg_sm")
              P.barrier()

          def norm_transpose(xt_ap, xt_res, aslot, atag, sslot, stag, dst3, dst_res, par):
              ssq = SM[:, 8 + par:9 + par]
              rs = SM[:, 10 + par:11 + par]
              xn = XN[:, par * 1024:(par + 1) * 1024]
              t1 = F32B[:, 2048:3072]
              P.op("dve", lambda e: e.memset(ssq, 0.0), writes=["ssq%d" % par])
              P.op("act", lambda e: e.activation(out=xn, in_=xt_ap, func=AF.Square, accum_out=ssq),
                   reads=[xt_res, "ssq%d" % par], writes=["xn%d" % par, "ssq%d" % par])
              P.op("act", lambda e: e.activation(out=rs, in_=ssq, func=AF.Sqrt, scale=1.0 / D, bias=EPS),
                   reads=["ssq%d" % par, "SM"], writes=["rs%d" % par])
              P.op("dve", lambda e: e.reciprocal(out=rs, in_=rs), reads=["rs%d" % par], writes=["rs%d" % par])
              P.op("dve", lambda e: e.scalar_tensor_tensor(out=t1, in0=xt_ap, scalar=rs, in1=ADA[:, aslot * 1024:(aslot + 1) * 1024],
                                                            op0=ALU.mult, op1=ALU.mult),
                   reads=[xt_res, "rs%d" % par, atag], writes=["t1"])
              P.op("dve", lambda e: e.tensor_tensor(out=xn, in0=t1, in1=ADA[:, sslot * 1024:(sslot + 1) * 1024], op=ALU.add),
                   reads=["t1", stag], writes=["xn%d" % par])
              for kc in range(8):
                  P.op("pe", lambda e, kc=kc: e.transpose(out=PTR[:, kc * 128:(kc + 1) * 128], in_=xn[:, kc * 128:(kc + 1) * 128],
                                                           identity=ident[:]),
                       reads=["xn%d" % par, "ident"], writes=["PTR"])
              P.op("act", lambda e: e.activation(out=dst3, in_=v3(PTR[:, 0:1024], 128), func=AF.Copy),
                   reads=["PTR"], writes=[dst_res])

          chk(1)
          def pre_a(t):
              par = t % 2
              xt = F32B[:, par * 1024:(par + 1) * 1024]
              xres = "xt%d" % par
              ssq = SM[:, 8 + par:9 + par]
              rs = SM[:, 10 + par:11 + par]
              xn = XN[:, par * 1024:(par + 1) * 1024]
              t1 = F32B[:, 2048:3072]
              P.dma("sp", xt, xf[t * 128:(t + 1) * 128, :], writes=[xres], key=xres)
              P.op("dve", lambda e: e.memset(ssq, 0.0), writes=["ssq%d" % par])
              P.op("act", lambda e: e.activation(out=xn, in_=xt, func=AF.Square, accum_out=ssq),
                   reads=[xres, "ssq%d" % par], writes=["xn%d" % par, "ssq%d" % par])
              P.op("act", lambda e: e.activation(out=rs, in_=ssq, func=AF.Sqrt, scale=1.0 / D, bias=EPS),
                   reads=["ssq%d" % par, "SM"], writes=["rs%d" % par])
              P.op("dve", lambda e: e.reciprocal(out=rs, in_=rs), reads=["rs%d" % par], writes=["rs%d" % par])
              P.op("dve", lambda e: e.scalar_tensor_tensor(out=t1, in0=xt, scalar=rs, in1=ADA[:, 1024:2048],
                                                            op0=ALU.mult, op1=ALU.mult),
                   reads=[xres, "rs%d" % par, "ada1"], writes=["t1"])
              P.op("dve", lambda e: e.tensor_tensor(out=xn, in0=t1, in1=ADA[:, 0:1024], op=ALU.add),
                   reads=["t1", "ada0"], writes=["xn%d" % par])

          def pre_b(t):
              par = t % 2
              g, tt = t // 4, t % 4
              xn = XN[:, par * 1024:(par + 1) * 1024]
              hbuf = BIGD[:, (g % 2) * 4096:(g % 2 + 1) * 4096]
              hres = "hT%d" % (g % 2)
              for kc in range(8):
                  P.op("pe", lambda e, kc=kc: e.transpose(out=PTR[:, kc * 128:(kc + 1) * 128], in_=xn[:, kc * 128:(kc + 1) * 128],
                                                           identity=ident[:]),
                       reads=["xn%d" % par, "ident"], writes=["PTR"])
              P.op("act", lambda e: e.activation(out=v3(hbuf, 512)[:, :, tt * 128:(tt + 1) * 128], in_=v3(PTR[:, 0:1024], 128), func=AF.Copy),
                   reads=["PTR"], writes=[hres])
              if tt == 3:
                  P.dma("sp", hTd[g], hbuf, reads=[hres], key="hst%d" % (g % 2))

          pre_a(0)
          for t in range(NT):
              if t + 1 < NT:
                  pre_a(t + 1)
              pre_b(t)
          P.barrier()

          chk(2)
          ada_seg(5, 0, "ada0", load_cbc=True)
          P.dma("sp", gtfd, ADA[:, 0:1024], reads=["ada0"], key="gtfst")
          ada_seg(3, 0, "ada0")
          ada_seg(4, 1, "ada1")
          ada_scale(1, "ada1", gffn_d)
          P.barrier()
          if KDEBUG:
              P.dma("sp", dbg_ada2, ADA[:, :], key="dbg_ada2")
              P.barrier()

          KT = BIGA
          V = BIGB
          WK = BIGC[:, 0:2048]
          WV = BIGC[:, 2048:4096]
          WQ = XNX[:, 2560:4608]
          KMB = XNX[:, 4608:4736]
          QT = BIGC[:, 4096:8192]
          PTring = [XNX[:, i * 512:(i + 1) * 512] for i in range(4)]
          YTOK = XNX[:, 2048:2560]
          BT = F32A[:, 0:3072]
          NTMP = [F32A[:, 3072 + i * 512: 3072 + (i + 1) * 512] for i in range(2)] + [F32A[:, 6144:6656]]
          SSLOT = [S2[:, 0:512], S2[:, 512:1024], PM[:, 0:512]]
          SELB = [F32A[:, 5400:5656], F32A[:, 7424:7680]]
          SRES = ["S0", "S1", "PM"]
          ACC = F32A[:, 4096:4096 + 1040]
          KMS = F32A[:, 5200:5200 + 64]
          GSM = F32A[:, 5300:5300 + 32]
          M8 = F32A[:, 5340:5348]
          THR = F32A[:, 5350:5351]
          SEL = F32A[:, 5400:5400 + 2 * 4 * 32]
          OTMP = F32A[:, 5700:5700 + 128]
          RL = F32A[:, 5900:5908]

          for p in range(4):
              is_diff = p < 2
              if is_diff:
                  qcol, kcol, vcol = 0 + p * 256, 512 + p * 256, 1024 + p * 256
                  W = 129
                  heads = [2 * p, 2 * p, 2 * p + 1, 2 * p + 1]
                  vcols = [(0, 129), (0, 129), (130, 129), (130, 129)]
              else:
                  pp = p - 2
                  qcol, kcol, vcol = 1536 + pp * 256, 2048 + pp * 256, 2560 + pp * 256
                  W = 65
                  heads = [4 + 4 * pp + i for i in range(4)]
                  vcols = [(i * 66, 65) for i in range(4)]
              def load_pass_weights(pn):
                  if pn < 2:
                      cols = (512 + pn * 256, 1024 + pn * 256, 0 + pn * 256)
                  else:
                      cols = (2048 + (pn - 2) * 256, 2560 + (pn - 2) * 256, 1536 + (pn - 2) * 256)
                  for (wt, col, tag) in ((WK, cols[0], "WK"), (WV, cols[1], "WV"), (WQ, cols[2], "WQ")):
                      P.dma("pool", v3(wt, 256), w_in[:, col:col + 256].rearrange("(kc p) n -> p kc n", p=128),
                            writes=[tag], key=tag)

              if p == 0:
                  load_pass_weights(0)
              for i, h in enumerate(sorted(set(heads))):
                  P.dma("sp", BT[:, i * 768:(i + 1) * 768], bt_d[:, h * 768:(h + 1) * 768], writes=["BT%d" % i], key="BT%d" % i)
                  P.op("dve", lambda e, i=i, h=h: e.tensor_scalar(out=BT[:, i * 768:(i + 1) * 768], in0=BT[:, i * 768:(i + 1) * 768],
                                                                  scalar1=b31[:, h:h + 1], scalar2=None, op0=ALU.subtract),
                       reads=["BT%d" % i, "b31"], writes=["BT%d" % i])
              hidx = {h: i for i, h in enumerate(sorted(set(heads)))}
              ncol = 2 if is_diff else 4
              stride = 130 if is_diff else 66
              for i in range(ncol):
                  P.op("dve", lambda e, i=i, stride=stride: e.tensor_copy(
                      out=v3(V[:, 0:NT * 264], 264)[:, :, i * stride + stride - 2], in_=valid[:, :]),
                      reads=["valid"], writes=["Vones"])
              for g in range(NG):
                  hbuf = BIGD[:, (g % 2) * 4096:(g % 2 + 1) * 4096]
                  hres = "hT%d" % (g % 2)
                  P.dma("sp", hbuf, hTd[g], writes=[hres], key="hld%d" % (g % 2))
                  for c in range(2):
                      pb = PO[c]
                      for kc in range(8):
                          P.op("pe", lambda e, kc=kc, c=c, pb=pb, hbuf=hbuf: e.matmul(
                              pb[:, 0:512], lhsT=WK[:, kc * 256 + c * 128: kc * 256 + c * 128 + 128],
                              rhs=hbuf[:, kc * 512:(kc + 1) * 512], start=(kc == 0), stop=(kc == 7)),
                              reads=["WK", hres], writes=["PO%d" % c])
                      P.op("act", lambda e, c=c, pb=pb, g=g: e.activation(
                          out=KT[:, c * SEQ + g * 512: c * SEQ + g * 512 + 512], in_=pb[:, 0:512], func=AF.Copy),
                          reads=["PO%d" % c], writes=["KT%d_%d" % (c, g)])
                      if not is_diff:
                          for bb in range(2):
                              P.op("dve", lambda e, c=c, pb=pb, g=g, bb=bb: e.reduce_sum(
                                  out=KMS[:, c * 32 + 2 * g + bb: c * 32 + 2 * g + bb + 1],
                                  in_=KT[:, c * SEQ + g * 512 + bb * 256: c * SEQ + g * 512 + bb * 256 + 256], axis=AX.X),
                                  reads=["KT%d_%d" % (c, g)], writes=["KMS"])
                  for tt in range(4):
                      t = 4 * g + tt
                      pb = PO[2 + tt % 2]
                      pres = "PO%d" % (2 + tt % 2)
                      for kc in range(8):
                          P.op("pe", lambda e, kc=kc, tt=tt, pb=pb, hbuf=hbuf: e.matmul(
                              pb[:, 0:256], lhsT=hbuf[:, kc * 512 + tt * 128: kc * 512 + tt * 128 + 128],
                              rhs=WV[:, kc * 256:(kc + 1) * 256], start=(kc == 0), stop=(kc == 7)),
                              reads=["WV", hres], writes=[pres])
                      if is_diff:
                          pairs = [(V[:, t * 264 + cc * 130: t * 264 + cc * 130 + 128], pb[:, cc * 128:(cc + 1) * 128]) for cc in range(2)]
                      else:
                          pairs = [(V[:, t * 264 + jj * 66: t * 264 + jj * 66 + 64], pb[:, jj * 64:(jj + 1) * 64]) for jj in range(4)]
                      for (dst, src) in pairs:
                          P.op("dve", lambda e, dst=dst, src=src, t=t: e.tensor_scalar(
                              out=dst, in0=src, scalar1=valid[:, t:t + 1], scalar2=None, op0=ALU.mult),
                              reads=[pres, "valid"], writes=["V%d" % t])
                  if g % 2 == 1:
                      s = g // 2
                      for c in range(2):
                          pb = PO[c]
                          for kc in range(8):
                              P.op("pe", lambda e, kc=kc, c=c, pb=pb, hbuf=hbuf: e.matmul(
                                  pb[:, 0:256], lhsT=WQ[:, kc * 256 + c * 128: kc * 256 + c * 128 + 128],
                                  rhs=hbuf[:, kc * 512 + 256:(kc + 1) * 512], start=(kc == 0), stop=(kc == 7)),
                                  reads=["WQ", hres], writes=["PO%d" % c])
                          P.op("act", lambda e, c=c, pb=pb, s=s: e.activation(
                              out=QT[:, c * 2048 + s * 256: c * 2048 + s * 256 + 256], in_=pb[:, 0:256], func=AF.Copy),
                              reads=["PO%d" % c], writes=["QT%d" % s])
              if not is_diff:
                  for c in range(2):
                      for u in range(2):
                          P.op("dve", lambda e, c=c, u=u: e.tensor_scalar(
                              out=KMB[:, c * 64 + u * 32: c * 64 + u * 32 + 32], in0=KMS[:, c * 32:(c + 1) * 32],
                              scalar1=UMASK[:, u:u + 1], scalar2=1.0 / 256, op0=ALU.mult, op1=ALU.mult),
                              reads=["KMS", "umask"], writes=["KMB"])

              if p + 1 < 4:
                  load_pass_weights(p + 1)
              chk(3 + p * 2)
              QBD = BIGD
              P.op("pool", lambda e: e.memset(QBD[:, 0:2048], 0.0), writes=["QBDz", "hT0"])
              step = 0
              blkc = 0
              pre_issued = False
              deferred = []
              deferred_mid = []
              for s in range(NQB):
                  F = 4 * s + 3
                  nkt = 2 * F + 2
                  ktres = ["KT%d_%d" % (c, g) for c in range(2) for g in range(NG)]
                  def gating_group(st, gi):
                      qt_, c_ = divmod(gi, 2)
                      selb = SELB[st % 2]
                      selres = "SEL%d" % (st % 2)
                      gb = PO[3][:, gi * 64:(gi + 1) * 64]
                      P.op("pe", lambda e, c_=c_, qt_=qt_, st=st, gb=gb: e.matmul(
                          gb, lhsT=QT[:, c_ * 2048 + st * 256 + qt_ * 128: c_ * 2048 + st * 256 + qt_ * 128 + 128],
                          rhs=KMB[:, c_ * 64:(c_ + 1) * 64], start=True, stop=True),
                          reads=["QT%d" % st, "KMB"], writes=["PO3"])
                      for u in range(2):
                          j = 2 * c_ + u
                          gp = gb[:, u * 32:(u + 1) * 32]
                          P.op("dve", lambda e, gp=gp, st=st: e.tensor_tensor(
                              out=GSM, in0=gp, in1=blkbias[:, st * 32:(st + 1) * 32], op=ALU.add),
                              reads=["PO3", "blkbias"], writes=["GSM"])
                          P.op("dve", lambda e: e.max(out=M8, in_=GSM), reads=["GSM"], writes=["M8"])
                          P.op("dve", lambda e: e.tensor_scalar(out=THR, in0=M8[:, 2:3], scalar1=-1e29, scalar2=None, op0=ALU.max),
                               reads=["M8"], writes=["THR"])
                          P.op("dve", lambda e, qt_=qt_, j=j, selb=selb: e.tensor_scalar(
                              out=selb[:, (qt_ * 4 + j) * 32:(qt_ * 4 + j + 1) * 32], in0=GSM, scalar1=THR, scalar2=None, op0=ALU.is_ge),
                              reads=["GSM", "THR"], writes=[selres])

                  if (not is_diff) and s == 0:
                      for gi in range(4):
                          gating_group(0, gi)
                  SELc = SELB[s % 2]
                  SELcres = "SEL%d" % (s % 2)
                  par_s = s % 2

                  def qbd_stage(st):
                      for c_ in range(2):
                          qoff_ = ((st % 2) * 2 + c_) * 512
                          for u in range(2):
                              P.op("pool", lambda e, c_=c_, u=u, st=st, qoff_=qoff_: e.tensor_copy(
                                  out=QBD[u * 64:(u + 1) * 64, qoff_ + u * 256: qoff_ + u * 256 + 256],
                                  in_=QT[u * 64:(u + 1) * 64, c_ * 2048 + st * 256: c_ * 2048 + st * 256 + 256]),
                                  reads=["QT%d" % st, "QBDz"], writes=["QBD%d_%d" % (st % 2, c_)])

                  if s == 0:
                      qbd_stage(0)
                  for c in range(2):
                    qoff = (par_s * 2 + c) * 512
                    qres = "QBD%d_%d" % (par_s, c)

                    def flags(kt):
                        if is_diff:
                            return kt == 0, kt == nkt - 1
                        return kt % 2 == 0, kt % 2 == 1

                    def emit_qk(kt, slot, c=c, qoff=qoff, qres=qres):
                        P.op("pe", lambda e, c=c, kt=kt, slot=slot, qoff=qoff: e.matmul(
                            SSLOT[slot],
                            lhsT=KT[:, c * SEQ + kt * 128: c * SEQ + kt * 128 + 128],
                            rhs=QBD[:, qoff: qoff + 512], start=True, stop=True),
                            reads=["KT%d_%d" % (c, kt // 4), qres], writes=[SRES[slot]])

                    if c == 0:
                        nxt = (1, (par_s * 2 + 1) * 512, "QBD%d_1" % par_s)
                    elif s + 1 < NQB:
                        nxt = (0, (((s + 1) % 2) * 2) * 512, "QBD%d_0" % ((s + 1) % 2))
                    else:
                        nxt = None

                    def emit_exp(kt, slot, pt, ptres, c=c, F=F):
                        r = 2 * F - kt
                        sres = SRES[slot]
                        if r <= 1:
                            ridx = 1 - r
                            nt_ = NTMP[slot]
                            for u in range(2):
                                hi = hidx[heads[2 * c + u]]
                                P.op("dve", lambda e, u=u, hi=hi, ridx=ridx, nt_=nt_, slot=slot: e.scalar_tensor_tensor(
                                    out=nt_[:, u * 256:(u + 1) * 256], in0=SSLOT[slot][:, u * 256:(u + 1) * 256],
                                    scalar=0.125, in1=BT[:, hi * 768 + ridx * 256: hi * 768 + ridx * 256 + 256],
                                    op0=ALU.mult, op1=ALU.add), reads=[sres, "BT%d" % hi], writes=["NTMP%d" % slot])
                            P.op("act", lambda e, pt=pt, nt_=nt_: e.activation(out=pt, in_=nt_, func=AF.Exp),
                                 reads=["NTMP%d" % slot], writes=[ptres])
                        else:
                            P.op("act", lambda e, pt=pt, slot=slot: e.activation(out=pt, in_=SSLOT[slot],
                                                                                 func=AF.Exp, scale=0.125),
                                 reads=[sres], writes=[ptres])

                    def acc_loc(kt, qt, u, blk_base=blkc):
                        if is_diff:
                            return PO[qt * 2 + u], 0, "PO%d" % (qt * 2 + u), True
                        b = (blk_base + kt // 2) % 3
                        return PO[b], (qt * 2 + u) * W, "PO%d" % b, (qt == 0 and u == 0)

                    def emit_pv(kt, pt, ptres, c=c):
                        gstart, gstop = flags(kt)
                        for qt in range(2):
                            for u in range(2):
                                po, col0, pores, first = acc_loc(kt, qt, u)
                                voff, vw = vcols[2 * c + u]
                                st_ = gstart and first
                                P.op("pe", lambda e, po=po, col0=col0, pt=pt, u=u, qt=qt, kt=kt, voff=voff, vw=vw, st_=st_, gstop=gstop: e.matmul(
                                    po[:, col0:col0 + vw], lhsT=pt[:, u * 256 + qt * 128: u * 256 + qt * 128 + 128],
                                    rhs=V[:, kt * 264 + voff: kt * 264 + voff + vw], start=st_, stop=gstop, skip_group_check=True),
                                    reads=[ptres, "V%d" % kt, "Vones"], writes=[pores])

                    def emit_fold(kt, first_acc, c=c, F=F):
                        n = kt // 2
                        own = n == F
                        for qt in range(2):
                            for u in range(2):
                                po, col0, pores, _first = acc_loc(kt, qt, u)
                                j = 2 * c + u
                                a = ACC[:, (qt * 4 + j) * W:(qt * 4 + j) * W + W]
                                ares = "ACC%d_%d" % (qt, j)
                                src = po[:, col0:col0 + W]
                                if is_diff or (first_acc and own):
                                    P.op("dve", lambda e, a=a, src=src: e.tensor_copy(out=a, in_=src), reads=[pores], writes=[ares])
                                elif first_acc:
                                    sc_ = SELc[:, (qt * 4 + j) * 32 + n:(qt * 4 + j) * 32 + n + 1]
                                    P.op("dve", lambda e, a=a, src=src, sc_=sc_: e.tensor_scalar(
                                        out=a, in0=src, scalar1=sc_, scalar2=None,
                                        op0=ALU.mult), reads=[pores, SELcres], writes=[ares])
                                elif own:
                                    P.op("dve", lambda e, a=a, src=src: e.tensor_tensor(out=a, in0=src, in1=a, op=ALU.add),
                                         reads=[pores, ares], writes=[ares])
                                else:
                                    sc_ = SELc[:, (qt * 4 + j) * 32 + n:(qt * 4 + j) * 32 + n + 1]
                                    P.op("dve", lambda e, a=a, src=src, sc_=sc_: e.scalar_tensor_tensor(
                                        out=a, in0=src, scalar=sc_, in1=a,
                                        op0=ALU.mult, op1=ALU.add), reads=[pores, SELcres, ares], writes=[ares])

                    slots = [(step + i) % 3 for i in range(nkt + 2)]
                    pts = [(step + i) % 4 for i in range(nkt)]
                    step += nkt
                    blkc += nkt // 2
                    first_acc = True
                    if c == 1 and s + 1 < NQB:
                        qbd_stage(s + 1)
                    if not pre_issued:
                        emit_qk(0, slots[0])
                        emit_qk(1, slots[1])
                    pre_issued = False
                    for kt in range(nkt):
                        pt = PTring[pts[kt]]
                        ptres = "PT%d" % pts[kt]
                        emit_exp(kt, slots[kt], pt, ptres)
                        if kt + 2 < nkt:
                            emit_qk(kt + 2, slots[kt + 2])
                        elif nxt is not None:
                            emit_qk(kt + 2 - nkt, slots[kt + 2], c=nxt[0], qoff=nxt[1], qres=nxt[2])
                            pre_issued = True
                        emit_pv(kt, pt, ptres)
                        if flags(kt)[1]:
                            emit_fold(kt, first_acc)
                            first_acc = False
                            if (not is_diff) and c == 1 and kt // 2 < 4 and s + 1 < NQB:
                                gating_group(s + 1, kt // 2)
                        if c == 0 and kt == 7:
                            while deferred_mid:
                                deferred_mid.pop(0)()
                        if kt == 3 and c == (1 if is_diff else 0):
                            while deferred:
                                deferred.pop(0)()
                  if is_diff:
                      OT4 = F32A[:, 6656:7168]
                      SS4 = F32A[:, 7168:7172]
                      RS4 = F32A[:, 7172:7176]
                      JK = F32A[:, 7296:7424]
                      for qt in range(2):
                          for c in range(2):
                              k = qt * 2 + c
                              rl = F32A[:, 7200 + k * 4: 7200 + k * 4 + 4]
                              ot = OT4[:, k * 128:(k + 1) * 128]
                              a1 = ACC[:, (qt * 4 + 2 * c) * W:(qt * 4 + 2 * c) * W + W]
                              a2 = ACC[:, (qt * 4 + 2 * c + 1) * W:(qt * 4 + 2 * c + 1) * W + W]
                              ar = ["ACC%d_%d" % (qt, 2 * c), "ACC%d_%d" % (qt, 2 * c + 1)]
                              P.op("dve", lambda e, a1=a1, rl=rl: e.reciprocal(out=rl[:, 0:1], in_=a1[:, 128:129]), reads=ar, writes=["RLa%d" % k])
                              P.op("dve", lambda e, a2=a2, rl=rl: e.reciprocal(out=rl[:, 1:2], in_=a2[:, 128:129]), reads=ar, writes=["RLb%d" % k])
                              P.op("dve", lambda e, rl=rl: e.tensor_tensor(out=rl[:, 2:3], in0=rl[:, 1:2], in1=NEGLAM, op=ALU.mult),
                                   reads=["RLb%d" % k, "neglam"], writes=["RLc%d" % k])
                              P.op("dve", lambda e, a1=a1, rl=rl, ot=ot: e.tensor_scalar(out=ot, in0=a1[:, 0:128], scalar1=rl[:, 0:1], scalar2=None, op0=ALU.mult),
                                   reads=ar + ["RLa%d" % k], writes=["OT%d" % k])
                              P.op("dve", lambda e, a2=a2, rl=rl, ot=ot: e.scalar_tensor_tensor(out=ot, in0=a2[:, 0:128], scalar=rl[:, 2:3], in1=ot,
                                                                                                  op0=ALU.mult, op1=ALU.add),
                                   reads=ar + ["RLc%d" % k, "OT%d" % k], writes=["OT%d" % k])
                              P.op("dve", lambda e, ot=ot: e.tensor_tensor(out=JK, in0=ot, in1=ot, op=ALU.mult), reads=["OT%d" % k], writes=["JK"])
                              P.op("dve", lambda e, k=k: e.reduce_sum(out=SS4[:, k:k + 1], in_=JK, axis=AX.X), reads=["JK"], writes=["SS4"])

                      def fin_mid(s=s):
                          P.op("act", lambda e: e.activation(out=RS4, in_=SS4, func=AF.Sqrt, scale=1.0 / (128 * LAMF * LAMF), bias=EPS2),
                               reads=["SS4", "SM"], writes=["RS4"])
                          P.op("dve", lambda e: e.reciprocal(out=RS4, in_=RS4), reads=["RS4"], writes=["RS4"])
                          for qt in range(2):
                              for c in range(2):
                                  k = qt * 2 + c
                                  P.op("dve", lambda e, c=c, qt=qt, k=k: e.scalar_tensor_tensor(
                                      out=XNX[:, 2048 + qt * 256 + c * 128: 2048 + qt * 256 + c * 128 + 128], in0=OT4[:, k * 128:(k + 1) * 128],
                                      scalar=RS4[:, k:k + 1], in1=gsub[:], op0=ALU.mult, op1=ALU.mult),
                                      reads=["OT%d" % k, "RS4", "gsub"], writes=["YTOK%d" % qt])
                      deferred_mid.append(fin_mid)
                  else:
                      for qt in range(2):
                          for j in range(4):
                              a = ACC[:, (qt * 4 + j) * W:(qt * 4 + j) * W + W]
                              ares = "ACC%d_%d" % (qt, j)
                              P.op("dve", lambda e, a=a, j=j: e.reciprocal(out=RL[:, j:j + 1], in_=a[:, 64:65]), reads=[ares], writes=["RLm%d" % j])
                              P.op("dve", lambda e, a=a, j=j, qt=qt: e.tensor_scalar(
                                  out=XNX[:, 2048 + qt * 256 + j * 64: 2048 + qt * 256 + j * 64 + 64], in0=a[:, 0:64], scalar1=RL[:, j:j + 1],
                                  scalar2=None, op0=ALU.mult),
                                   reads=[ares, "RLm%d" % j], writes=["YTOK%d" % qt])
                  for qt in range(2):
                      def fin_pe(qt=qt, s=s, ch0=2 * p):
                          for c in range(2):
                              P.op("pe", lambda e, c=c, qt=qt: e.transpose(
                                  out=PTR[:, qt * 256 + c * 128: qt * 256 + c * 128 + 128],
                                  in_=XNX[:, 2048 + qt * 256 + c * 128: 2048 + qt * 256 + c * 128 + 128],
                                  identity=ident[:]), reads=["YTOK%d" % qt, "ident"], writes=["PTR"])
                          P.op("act", lambda e, ch0=ch0, s=s, qt=qt: e.activation(
                              out=v3(YT[:, ch0 * 2048:(ch0 + 2) * 2048], 2048)[:, :, s * 256 + qt * 128: s * 256 + qt * 128 + 128],
                              in_=v3(PTR[:, qt * 256:(qt + 1) * 256], 128), func=AF.Copy), reads=["PTR"], writes=["YT%d" % s])
                      deferred.append(fin_pe)
                  if s == 0:
                      chk(50 + p)
                      if KDEBUG and p == 2:
                          P.barrier()
                          P.dma("sp", dbg_acc, F32A[:, 4096:6144], key="dbg_acc")
                          P.barrier()
              while deferred_mid:
                  deferred_mid.pop(0)()
              while deferred:
                  deferred.pop(0)()
              P.barrier()
              chk(4 + p * 2)

          if KDEBUG:
              P.dma("sp", dbg_yT, YT[:, :], key="dbg_yT")
              P.barrier()
          WGA = BIGA[:, 0:8192]
          WGB = BIGA[:, 8192:16384]
          WBA = BIGB[:, 0:4096]
          WBB = BIGB[:, 4096:8192]
          WO = BIGB[:, 8192:16384]
          P.dma("pool", v3(WGA, 1024), w_in[:, 3072:4096].rearrange("(kc p) n -> p kc n", p=128), writes=["WGA"], key="WGA")
          P.dma("pool", v3(WGB, 1024), w_in[:, 4096:5120].rearrange("(kc p) n -> p kc n", p=128), writes=["WGB"], key="WGB")
          P.dma("pool", v3(WBA, 1024), w_br_a.rearrange("(kc p) n -> p kc n", p=128), writes=["WBA"], key="WBA")
          P.dma("pool", v3(WBB, 1024), w_br_b.rearrange("(kc p) n -> p kc n", p=128), writes=["WBB"], key="WBB")
          P.dma("pool", v3(WO, 1024), w_out.rearrange("(kc p) n -> p kc n", p=128), writes=["WO"], key="WO")
          HQ = [BIGD[:, i * 2048:(i + 1) * 2048] for i in range(2)]
          MT = BIGD[:, 4096:6144]
          SG = [F32A[:, i * 256:(i + 1) * 256] for i in range(4)]
          X1 = [F32A[:, 2048 + i * 1024: 2048 + (i + 1) * 1024] for i in range(2)]
          LG = F32A[:, 4096:4096 + 32]
          EALL = F32A[:, 4160:4160 + 32]
          MTB = [BIGD[:, 4096:6144], BIGD[:, 6144:8192]]

          def mix_mloop(s):
              hq = HQ[s % 2]
              hqres = "HQ%d" % (s % 2)
              mt = MTB[s % 2]
              mtres = "MT%d" % (s % 2)
              P.dma("sp", v3(hq, 256), v3(hTd[2 * s + 1], 512)[:, :, 256:512], writes=[hqres], key=hqres)
              for m in range(8):
                  if m % 2 == 0:
                      pa, pga, pb_, pgb = S2[:, 0:256], S2[:, 256:512], S2[:, 512:768], S2[:, 768:1024]
                      ra_, rga_, rb_, rgb_ = "S2a", "S2b", "S2c", "S2d"
                  else:
                      pa, pga, pb_, pgb = PO[2][:, 0:256], PO[2][:, 256:512], PO[3][:, 0:256], PO[3][:, 256:512]
                      ra_, rga_, rb_, rgb_ = "P2a", "P2b", "P3a", "P3b"
                  for (dst, wt, nk, src, srcres, wres, ch_off, dres) in (
                          (pa, WBA, 4, YT, "YT%d" % s, "WBA", 0, ra_), (pga, WGA, 8, hq, hqres, "WGA", 0, rga_),
                          (pb_, WBB, 4, YT, "YT%d" % s, "WBB", 4, rb_), (pgb, WGB, 8, hq, hqres, "WGB", 0, rgb_)):
                      for kc in range(nk):
                          if src is YT:
                              rhs = YT[:, (ch_off + kc) * 2048 + s * 256:(ch_off + kc) * 2048 + s * 256 + 256]
                          else:
                              rhs = hq[:, kc * 256:(kc + 1) * 256]
                          P.op("pe", lambda e, dst=dst, wt=wt, kc=kc, m=m, rhs=rhs, nk=nk: e.matmul(
                              dst, lhsT=wt[:, kc * 1024 + m * 128: kc * 1024 + m * 128 + 128], rhs=rhs,
                              start=(kc == 0), stop=(kc == nk - 1)), reads=[wres, srcres], writes=[dres])
                  P.op("act", lambda e, pga=pga: e.activation(out=SG[0], in_=pga, func=AF.Sigmoid), reads=[rga_], writes=["SG0"])
                  P.op("act", lambda e, pgb=pgb: e.activation(out=SG[1], in_=pgb, func=AF.Sigmoid), reads=[rgb_], writes=["SG1"])
                  P.op("dve", lambda e, pa=pa: e.tensor_tensor(out=SG[2], in0=pa, in1=SG[0], op=ALU.mult), reads=[ra_, "SG0"], writes=["SG2"])
                  P.op("dve", lambda e, pb_=pb_: e.tensor_tensor(out=SG[3], in0=pb_, in1=SG[1], op=ALU.mult), reads=[rb_, "SG1"], writes=["SG3"])
                  P.op("dve", lambda e, m=m, mt=mt: e.tensor_tensor(out=mt[:, m * 256:(m + 1) * 256], in0=SG[2], in1=SG[3], op=ALU.add),
                       reads=["SG2", "SG3"], writes=[mtres])

          def mix_s1(s, qt):
              F = 4 * s + 3
              mt = MTB[s % 2]
              mtres = "MT%d" % (s % 2)
              tl = 2 * s + qt
              par = tl % 2
              xt = F32B[:, par * 1024:(par + 1) * 1024]
              row0 = F * 256 + qt * 128
              P.dma("sp", xt, xf[row0:row0 + 128, :], writes=["xt%d" % par], key="xt%d" % par)
              x1 = X1[par]
              x1res = "X1_%d" % par
              for half in range(2):
                  pz = PO[half]
                  for kc in range(8):
                      P.op("pe", lambda e, pz=pz, kc=kc, half=half: e.matmul(
                          pz[:, 0:512], lhsT=mt[:, kc * 256 + qt * 128: kc * 256 + qt * 128 + 128],
                          rhs=WO[:, kc * 1024 + half * 512: kc * 1024 + half * 512 + 512], start=(kc == 0), stop=(kc == 7)),
                          reads=[mtres, "WO"], writes=["PO%d" % half])
                  P.op("dve", lambda e, pz=pz, half=half: e.tensor_tensor(
                      out=x1[:, half * 512:(half + 1) * 512], in0=pz[:, 0:512], in1=ADA[:, 2048 + half * 512: 2048 + half * 512 + 512],
                      op=ALU.mult), reads=["PO%d" % half, "ada2"], writes=[x1res])
                  P.op("dve", lambda e, half=half: e.tensor_tensor(
                      out=x1[:, half * 512:(half + 1) * 512], in0=x1[:, half * 512:(half + 1) * 512], in1=xt[:, half * 512:(half + 1) * 512],
                      op=ALU.add), reads=[x1res, "xt%d" % par], writes=[x1res])
              P.dma("sp", x1d[tl * 128:(tl + 1) * 128, :], x1, reads=[x1res], key="x1st%d" % par)

          def mix_s2(s, qt):
              tl = 2 * s + qt
              par = tl % 2
              x1 = X1[par]
              x1res = "X1_%d" % par
              ssq = SM[:, 8 + par:9 + par]
              rs = SM[:, 10 + par:11 + par]
              xn = XN[:, par * 1024:(par + 1) * 1024]
              t1 = F32B[:, 2048:3072]
              P.op("dve", lambda e: e.memset(ssq, 0.0), writes=["ssq%d" % par])
              P.op("act", lambda e: e.activation(out=xn, in_=x1, func=AF.Square, accum_out=ssq),
                   reads=[x1res, "ssq%d" % par], writes=["xn%d" % par, "ssq%d" % par])
              P.op("act", lambda e: e.activation(out=rs, in_=ssq, func=AF.Sqrt, scale=1.0 / D, bias=EPS),
                   reads=["ssq%d" % par, "SM"], writes=["rs%d" % par])
              P.op("dve", lambda e: e.reciprocal(out=rs, in_=rs), reads=["rs%d" % par], writes=["rs%d" % par])
              P.op("dve", lambda e: e.scalar_tensor_tensor(out=t1, in0=x1, scalar=rs, in1=ADA[:, 1024:2048],
                                                            op0=ALU.mult, op1=ALU.mult),
                   reads=[x1res, "rs%d" % par, "ada1"], writes=["t1"])
              P.op("dve", lambda e: e.tensor_tensor(out=xn, in0=t1, in1=ADA[:, 0:1024], op=ALU.add),
                   reads=["t1", "ada0"], writes=["xn%d" % par])

          def mix_s3(s, qt):
              tl = 2 * s + qt
              par = tl % 2
              xn = XN[:, par * 1024:(par + 1) * 1024]
              for kc in range(8):
                  P.op("pe", lambda e, kc=kc: e.transpose(out=PTR[:, kc * 128:(kc + 1) * 128], in_=xn[:, kc * 128:(kc + 1) * 128],
                                                           identity=ident[:]),
                       reads=["xn%d" % par, "ident"], writes=["PTR"])
              P.op("act", lambda e: e.activation(out=v3(YT[:, 0:16384], 2048)[:, :, s * 256 + qt * 128: s * 256 + qt * 128 + 128],
                                                 in_=v3(PTR[:, 0:1024], 128), func=AF.Copy),
                   reads=["PTR"], writes=["YT%d" % s])
              for kc in range(8):
                  P.op("pe", lambda e, kc=kc: e.matmul(
                      PM[:, 0:32], lhsT=YT[:, kc * 2048 + s * 256 + qt * 128: kc * 2048 + s * 256 + qt * 128 + 128],
                      rhs=WR[:, kc * 32:(kc + 1) * 32], start=(kc == 0), stop=(kc == 7)), reads=["YT%d" % s, "WR"], writes=["PM"])
              P.op("dve", lambda e: e.tensor_tensor(out=LG, in0=PM[:, 0:32], in1=BRT[:], op=ALU.add), reads=["PM", "BRT"], writes=["LG"])
              P.op("dve", lambda e: e.max(out=M8, in_=LG), reads=["LG"], writes=["M8"])
              P.op("dve", lambda e: e.tensor_scalar(out=RL[:, 0:1], in0=M8[:, 0:1], scalar1=-1.0, scalar2=None, op0=ALU.mult),
                   reads=["M8"], writes=["RL0"])
              P.op("act", lambda e: e.activation(out=EALL, in_=LG, func=AF.Exp, bias=RL[:, 0:1]), reads=["LG", "RL0"], writes=["EALL"])
              cb = COMB[:, tl * 32:(tl + 1) * 32]
              P.op("dve", lambda e: e.scalar_tensor_tensor(out=cb, in0=LG, scalar=M8[:, 3:4], in1=EALL, op0=ALU.is_ge, op1=ALU.mult),
                   reads=["LG", "M8", "EALL"], writes=["COMB%d" % tl])
              P.op("dve", lambda e: e.reduce_sum(out=RL[:, 1:2], in_=cb, axis=AX.X), reads=["COMB%d" % tl], writes=["RL1"])
              P.op("dve", lambda e: e.reciprocal(out=RL[:, 1:2], in_=RL[:, 1:2]), reads=["RL1"], writes=["RL1"])
              P.op("dve", lambda e: e.tensor_scalar(out=cb, in0=cb, scalar1=RL[:, 1:2], scalar2=None, op0=ALU.mult),
                   reads=["COMB%d" % tl, "RL1"], writes=["COMB%d" % tl])

          mix_mloop(0)
          for s in range(NQB):
              mix_s1(s, 0)
              mix_s1(s, 1)
              mix_s2(s, 0)
              mix_s2(s, 1)
              if s + 1 < NQB:
                  mix_mloop(s + 1)
              mix_s3(s, 0)
              mix_s3(s, 1)
          P.barrier()

          if KDEBUG:
              P.dma("sp", dbg_comb, COMB[:, :], key="dbg_comb")
              P.barrier()
          chk(20)
          WGU = [BIGA, BIGB]
          WD = BIGC
          ACT_T = BIGD
          ACCM = F32A
          TG = [F32B[:, i * 512:(i + 1) * 512] for i in range(6)]
          P.dma("pool", BDN[0:32, :], bdn_d, writes=["BDN"], key="c10")
          outs = []
          it = 0
          for half in range(2):
              def actT(fc, c0, n, half=half):
                  base = fc * 1024
                  return ACT_T[:, base + c0: base + c0 + n]

              for tl in range(8):
                  gt = half * 8 + tl
                  P.op("dve", lambda e, gt=gt: e.tensor_copy(out=XNX[:, 0:32], in_=COMB[:, gt * 32:(gt + 1) * 32]),
                       reads=["COMB"], writes=["cbf"])
                  P.op("pe", lambda e: e.transpose(out=PTR[0:32, 0:128], in_=XNX[:, 0:32], identity=ident[:]),
                       reads=["cbf", "ident"], writes=["PTR"])
                  P.op("act", lambda e: e.activation(out=XNX[0:32, 128:256], in_=PTR[0:32, 0:128], func=AF.Copy),
                       reads=["PTR"], writes=["cT"])
                  for hf in range(2):
                      pz = PO[hf]
                      P.op("pe", lambda e, pz=pz, hf=hf: e.matmul(
                          pz[:, 0:512], lhsT=XNX[0:32, 128:256], rhs=BDN[0:32, hf * 512:(hf + 1) * 512],
                          start=True, stop=True), reads=["cT", "BDN"], writes=["PO%d" % hf])
                      P.op("dve", lambda e, pz=pz, tl=tl, hf=hf: e.tensor_copy(
                          out=ACCM[:, tl * 1024 + hf * 512: tl * 1024 + hf * 512 + 512], in_=pz[:, 0:512]),
                          reads=["PO%d" % hf], writes=["ACCM%d" % tl])
              def load_w(itn):
                  exn = itn % NE
                  bufn = itn % 2
                  for hh in range(2):
                      P.dma("pool", v3(WGU[bufn][:, 0:16384], 2048)[:, :, hh * 1024:(hh + 1) * 1024],
                            w_gu[exn][:, hh * 1024:(hh + 1) * 1024].rearrange("(kc p) n -> p kc n", p=128),
                            writes=["WGU%d_%d" % (bufn, hh)], key="WGU%d_%d" % (bufn, hh))

              def load_wd(itn):
                  exn = itn % NE
                  P.dma("pool", v3(WD[:, 0:8192], 1024), w_dn[exn].rearrange("(kc p) n -> p kc n", p=128),
                        writes=["WD"], key="WD")

              if half == 0:
                  load_w(0)
                  load_wd(0)
              def emit_up(ex, buf, tg, fc, half=half, actT=actT):
                  wgu = WGU[buf]
                  tok0 = half * 1024 + tg * 512
                  if fc % 2 == 0:
                      pg, pgres, pu, pures = S2[:, 0:512], "S2a", S2[:, 512:1024], "S2c"
                  else:
                      pg, pgres, pu, pures = PM[:, 0:512], "PM", PO[3][:, 0:512], "PO3"
                  for (dst, hh, dres) in ((pg, 0, pgres), (pu, 1, pures)):
                      for kc in range(8):
                          P.op("pe", lambda e, dst=dst, hh=hh, kc=kc, fc=fc, tok0=tok0, wgu=wgu: e.matmul(
                              dst, lhsT=wgu[:, kc * 2048 + hh * 1024 + fc * 128: kc * 2048 + hh * 1024 + fc * 128 + 128],
                              rhs=YT[:, kc * 2048 + tok0: kc * 2048 + tok0 + 512], start=(kc == 0), stop=(kc == 7)),
                              reads=["WGU%d_%d" % (buf, hh), "H2T"], writes=[dres])
                  k4 = (fc % 2) * 3
                  g_, sg_, u_ = TG[k4], TG[k4 + 1], TG[k4 + 2]
                  a_ = g_
                  rg, rsg, ru = ["TG%d" % (k4 + i) for i in range(3)]
                  ra = rg
                  P.op("dve", lambda e, g_=g_, pg=pg, ex=ex, fc=fc: e.tensor_scalar(
                      out=g_, in0=pg, scalar1=BGU[:, ex * 16 + fc: ex * 16 + fc + 1], scalar2=7.0, op0=ALU.add, op1=ALU.min),
                      reads=[pgres, "BGU"], writes=[rg])
                  P.op("act", lambda e, g_=g_, sg_=sg_: e.activation(out=sg_, in_=g_, func=AF.Sigmoid, scale=1.702),
                       reads=[rg], writes=[rsg])
                  P.op("dve", lambda e, u_=u_, pu=pu, ex=ex, fc=fc: e.tensor_scalar(
                      out=u_, in0=pu, scalar1=BGU[:, ex * 16 + 8 + fc: ex * 16 + 8 + fc + 1], scalar2=8.0, op0=ALU.add, op1=ALU.min),
                      reads=[pures, "BGU"], writes=[ru])
                  P.op("pool", lambda e, a_=a_, g_=g_, sg_=sg_: e.tensor_tensor(out=a_, in0=g_, in1=sg_, op=ALU.mult),
                       reads=[rg, rsg], writes=[ra])
                  dstA = actT(fc, tg * 512, 512)
                  P.op("dve", lambda e, a_=a_, u_=u_, dstA=dstA: e.scalar_tensor_tensor(
                      out=dstA, in0=u_, scalar=-6.0, in1=a_,
                      op0=ALU.max, op1=ALU.mult), reads=[ra, ru], writes=["ACT%d" % tg])

              def emit_down(ex, tg, half=half, actT=actT):
                  wd = WD
                  for tt in range(4):
                      tl = tg * 4 + tt
                      gt = half * 8 + tl
                      for hf in range(2):
                          pz = PO[(tt * 2 + hf) % 3]
                          pzres = "PO%d" % ((tt * 2 + hf) % 3)
                          for fc in range(8):
                              lA = actT(fc, tg * 512 + tt * 128, 128)
                              P.op("pe", lambda e, pz=pz, fc=fc, hf=hf, wd=wd, lA=lA: e.matmul(
                                  pz[:, 0:512], lhsT=lA,
                                  rhs=wd[:, fc * 1024 + hf * 512: fc * 1024 + hf * 512 + 512], start=(fc == 0), stop=(fc == 7)),
                                  reads=["ACT%d" % tg, "WD"], writes=[pzres])
                          acc = ACCM[:, tl * 1024 + hf * 512: tl * 1024 + hf * 512 + 512]
                          P.op("dve", lambda e, pz=pz, acc=acc, gt=gt, ex=ex: e.scalar_tensor_tensor(
                              out=acc, in0=pz[:, 0:512], scalar=COMB[:, gt * 32 + ex: gt * 32 + ex + 1], in1=acc,
                              op0=ALU.mult, op1=ALU.add), reads=[pzres, "ACCM%d" % tl], writes=["ACCM%d" % tl])

              seq = [(ex, tg) for ex in range(NE) for tg in range(2)]
              hoisted = False
              for idx, (ex, tg) in enumerate(seq):
                  itn = half * NE + ex
                  buf = itn % 2
                  if idx == 0 and itn + 1 < 2 * NE:
                      load_w(itn + 1)
                  for fc in range(8):
                      if fc < 2 and hoisted:
                          continue
                      emit_up(ex, buf, tg, fc)
                  hoisted = False
                  if idx + 1 < len(seq):
                      ex2, tg2 = seq[idx + 1]
                      itn2 = half * NE + ex2
                      if tg2 == 0 and itn2 + 1 < 2 * NE:
                          load_w(itn2 + 1)
                      emit_up(ex2, itn2 % 2, tg2, 0)
                      emit_up(ex2, itn2 % 2, tg2, 1)
                      hoisted = True
                  emit_down(ex, tg)
                  if tg == 1 and itn + 1 < 2 * NE:
                      load_wd(itn + 1)
              P.barrier()
              if half == 0:
                  P.dma("sp", ADA[:, 2048:3072], gfin_d, writes=["ada2"], key="gfin")
                  P.dma("sp", ADA[:, 0:1024], gtfd, writes=["ada3"], key="gtfld")
              def fin_a(tl, half=half):
                  gt = half * 8 + tl
                  par = gt % 2
                  xt = F32B[:, par * 1024:(par + 1) * 1024]
                  acc = ACCM[:, tl * 1024:(tl + 1) * 1024]
                  ares = "ACCM%d" % tl
                  ssq = SM[:, 8 + par:9 + par]
                  rs = SM[:, 10 + par:11 + par]
                  P.dma("sp", xt, x1d[gt * 128:(gt + 1) * 128, :], writes=["xt%d" % par], key="xt%d" % par)
                  P.op("pool", lambda e: e.tensor_tensor(out=acc, in0=acc, in1=ADA[:, 0:1024], op=ALU.mult),
                       reads=[ares, "ada3"], writes=[ares])
                  P.op("dve", lambda e: e.tensor_tensor(out=acc, in0=acc, in1=xt, op=ALU.add),
                       reads=[ares, "xt%d" % par], writes=[ares])
                  P.op("dve", lambda e: e.memset(ssq, 0.0), writes=["ssq%d" % par])
                  P.op("act", lambda e: e.activation(out=xt, in_=acc, func=AF.Square, accum_out=ssq),
                       reads=[ares, "ssq%d" % par], writes=["xt%d" % par, "ssq%d" % par])
                  P.op("act", lambda e: e.activation(out=rs, in_=ssq, func=AF.Sqrt, scale=1.0 / D, bias=EPS),
                       reads=["ssq%d" % par, "SM"], writes=["rs%d" % par])

              def fin_b(tl, half=half):
                  gt = half * 8 + tl
                  par = gt % 2
                  acc = ACCM[:, tl * 1024:(tl + 1) * 1024]
                  ares = "ACCM%d" % tl
                  rs = SM[:, 10 + par:11 + par]
                  P.op("dve", lambda e: e.reciprocal(out=rs, in_=rs), reads=["rs%d" % par], writes=["rs%d" % par])
                  P.op("dve", lambda e: e.scalar_tensor_tensor(out=acc, in0=acc, scalar=rs, in1=ADA[:, 2048:3072],
                                                                op0=ALU.mult, op1=ALU.mult),
                       reads=[ares, "rs%d" % par, "ada2"], writes=[ares])
                  outs.append(P.dma("sp", out_d[gt * 128:(gt + 1) * 128, :], acc, reads=[ares], key="ost%d" % tl))

              fin_a(0)
              for tl in range(8):
                  if tl + 1 < 8:
                      fin_a(tl + 1)
                  fin_b(tl)
              P.barrier()
      except _Stop:
        P.barrier()
        outs = [P.dma("sp", out_d[i * 128:(i + 1) * 128, :], F32A[:, i * 1024:(i + 1) * 1024], key="dbgout%d" % i) for i in range(8)]
        if KDEBUG:
            outs.append(P.dma("sp", dbg_yT, YT[:, :], key="dbg_yT"))
      print("ops per engine", {e: len(v) for e, v in P.ops.items()})
      P.emit(final_wait_ops=outs)
    return nc


_NC_CACHE = {}


def kernel(x, c, rel_bias, w_ada, b_ada, g_mix, w_in, lambda_q1, lambda_k1, lambda_q2, lambda_k2, subln_g,
           w_br_a, w_br_b, w_out, g_ffn, w_router, b_router, w_gate_up, b_gate_up, w_down, b_down, g_final):
    f = lambda a: np.ascontiguousarray(np.asarray(a, dtype=np.float32))
    x = f(x); c = f(c); rel_bias = f(rel_bias)
    bc = lambda v: np.ascontiguousarray(np.broadcast_to(np.asarray(v, np.float32).reshape(1, -1), (128, np.asarray(v).size)))
    kk = np.arange(128)[:, None]
    qq = np.arange(256)[None, :]
    bt = np.empty((128, 12, 3, 256), np.float32)
    for ri, r in enumerate((1, 0, -1)):
        dist = r * 128 + qq - kk
        bidx = _t5_bucket_np(dist)
        tile = rel_bias[bidx]
        tile = np.where((dist >= 0)[:, :, None], tile, np.float32(NEG))
        bt[:, :, ri, :] = np.transpose(tile, (0, 2, 1))
    bt = np.ascontiguousarray(bt.reshape(128, 12 * 3 * 256))
    b31 = bc(rel_bias[31])
    ident = np.eye(128, dtype=np.float32).astype(ml_dtypes.bfloat16)
    lam_bc = bc(np.concatenate([f(lambda_q1)[0], f(lambda_k1)[0], f(lambda_q2)[0], f(lambda_k2)[0]]))
    lam_init = 0.8 - 0.6 * math.exp(-0.3 * 0)
    gsub_bc = bc(f(subln_g)[0]) * np.float32(1.0)
    shared = {
        "bt": bt, "b31": b31, "ident": ident,
        "umask": np.ascontiguousarray(np.stack([(np.arange(128) < 64), (np.arange(128) >= 64)], axis=1).astype(np.float32)),
        "w_ada": f(w_ada)[0], "bada_bc": bc(f(b_ada)[0]), "gmix_bc": bc(f(g_mix)[0]), "gffn_bc": bc(f(g_ffn)[0]),
        "gfin_bc": bc(f(g_final)), "gsub_bc": gsub_bc, "lam_bc": lam_bc,
        "w_in": f(w_in)[0], "w_br_a": f(w_br_a)[0], "w_br_b": f(w_br_b)[0], "w_out": f(w_out)[0],
        "w_router": f(w_router)[0], "brouter_bc": bc(f(b_router)[0]),
        "w_gate_up": f(w_gate_up)[0],
        "bgu_col": np.ascontiguousarray(f(b_gate_up)[0].reshape(NE, 16, 128).transpose(2, 0, 1).reshape(128, NE * 16)),
        "w_down": f(w_down)[0], "b_down": f(b_down)[0],
    }
    in_maps = []
    for core in range(8):
        b, j = core // 4, core % 4
        nd = (3 - j) * 256
        xfr = np.zeros((SEQ, D), np.float32)
        xfr[nd:] = x[b, :SEQ - nd]
        tok = np.arange(SEQ).reshape(NT, 128).T
        valid = (tok >= nd).astype(np.float32)
        blk = np.full((NQB, 32), -1e30, np.float32)
        for s in range(NQB):
            F = 4 * s + 3
            blk[s, nd // 256:F] = 0.0
        m = dict(shared)
        m.update({
            "xf": xfr,
            "cbc": np.ascontiguousarray(np.broadcast_to(c[b].reshape(8, 128).T[:, :, None], (128, 8, 128)).reshape(128, 1024)),
            "valid": np.ascontiguousarray(valid),
            "blkbias": bc(blk.reshape(-1)),
        })
        in_maps.append(m)
    if "nc" not in _NC_CACHE:
        _NC_CACHE["nc"] = build_program()
    if STAGE <= 20:
        for m in in_maps:
            m.pop("w_gate_up"); m.pop("w_down")
    res = run_bass_kernel_spmd(_NC_CACHE["nc"], in_maps, core_ids=list(range(8)))
    if KDEBUG:
        _LAST["res"] = res.results
    out = np.empty((2, SEQ, D), np.float32)
    for core in range(8):
        b, j = core // 4, core % 4
        o = res.results[core]["out"]
        for s in range(NQB):
            blk0 = (4 * s + j) * 256
            out[b, blk0:blk0 + 256] = o[s * 256:(s + 1) * 256]
    return out
```

```python
import math
from contextlib import ExitStack

import numpy as np
import ml_dtypes
import concourse.bass as bass
import concourse.mybir as mybir
from concourse.bass_utils import run_bass_kernel_spmd

F32 = mybir.dt.float32
BF16 = mybir.dt.bfloat16
AF = mybir.ActivationFunctionType
ALU = mybir.AluOpType
AX = mybir.AxisListType

D = 1024
SEQ = 8192
NT = 64
NG = 16
NQB = 8
NE = 32
NEG = -30000.0
DEBUG = False
import os
STAGE = int(os.environ.get("KSTAGE", "99"))
KDEBUG = int(os.environ.get("KDEBUG", "0"))
_LAST = {}


class _Stop(Exception):
    pass

COMPUTE = ("pe", "act", "dve", "pool")


class Op:
    __slots__ = ("eng", "fn", "deps", "sem", "val", "need_inc", "is_dma")

    def __init__(self, eng, fn, is_dma=False):
        self.eng = eng
        self.fn = fn
        self.deps = []
        self.sem = None
        self.val = None
        self.need_inc = False
        self.is_dma = is_dma


class Prog:
    def __init__(self, nc, same_engine_raw=True):
        self.nc = nc
        self.ops = {e: [] for e in ("pe", "act", "dve", "pool", "sp")}
        self.last_w = {}
        self.readers = {}
        self.dma_keys = {}
        self.same_engine_raw = same_engine_raw
        self.dma_since_barrier = []

    def _add_dep(self, op, d, kind):
        if d is None or d is op:
            return
        if (not d.is_dma) and (not op.is_dma) and d.eng == op.eng:
            if d.eng == "pe":
                return
            if kind != "raw" or not self.same_engine_raw:
                return
        op.deps.append(d)
        d.need_inc = True

    def op(self, eng, fn, reads=(), writes=(), dma_key=None):
        is_dma = dma_key is not None
        o = Op(eng, fn, is_dma)
        for r in reads:
            self._add_dep(o, self.last_w.get(r), "raw")
        for w in writes:
            self._add_dep(o, self.last_w.get(w), "waw")
            for rd in self.readers.get(w, ()):
                self._add_dep(o, rd, "war")
        for r in reads:
            self.readers.setdefault(r, []).append(o)
        for w in writes:
            self.last_w[w] = o
            self.readers[w] = []
        if is_dma:
            o.sem = ("dma", dma_key)
            c = self.dma_keys.setdefault(dma_key, [0])
            c[0] += 16
            o.val = c[0]
            o.need_inc = True
            self.dma_since_barrier.append(o)
        self.ops[eng].append(o)
        return o

    def dma(self, eng, out, in_, reads=(), writes=(), key=None):
        return self.op(eng, lambda e: e.dma_start(out=out, in_=in_), reads, writes, dma_key=key)

    def barrier(self):
        lasts = [self.ops[e][-1] for e in COMPUTE if self.ops[e]]
        dmas = list(self.dma_since_barrier)
        self.dma_since_barrier = []
        for e in ("pe", "act", "dve", "pool", "sp"):
            o = Op(e, lambda eng: eng.nop())
            for d in lasts + dmas:
                if d.eng == e and not d.is_dma:
                    continue
                o.deps.append(d)
                d.need_inc = True
            self.ops[e].append(o)
        self.last_w = {}
        self.readers = {}

    def emit(self, final_wait_ops=()):
        nc = self.nc
        for e in COMPUTE:
            c = 0
            for o in self.ops[e]:
                if o.need_inc and not o.is_dma:
                    c += 1
                    o.sem = ("eng", e)
                    o.val = c
        sem_names = set()
        for e in self.ops:
            for o in self.ops[e]:
                if o.need_inc:
                    sem_names.add(o.sem)
        with ExitStack() as st:
            sems = {}
            for i, s in enumerate(sorted(sem_names, key=str)):
                sems[s] = st.enter_context(nc.semaphore("s%d" % i))
            block = st.enter_context(nc.Block())
            engmap = {"pe": "tensor", "act": "scalar", "dve": "vector", "pool": "gpsimd", "sp": "sync"}

            def make(ename):
                ops = self.ops[ename]

                def body(eng):
                    waited = {}
                    for o in ops:
                        need = {}
                        for d in o.deps:
                            if d.val > need.get(d.sem, 0):
                                need[d.sem] = d.val
                        for s, v in need.items():
                            if waited.get(s, 0) >= v:
                                continue
                            eng.wait_ge(sems[s], v)
                            waited[s] = v
                        ins = o.fn(eng)
                        if o.need_inc:
                            ins.then_inc(sems[o.sem], 16 if o.is_dma else 1)
                    if ename == "sp":
                        for o in final_wait_ops:
                            eng.wait_ge(sems[o.sem], o.val)
                return body

            for ename in self.ops:
                getattr(block, engmap[ename])(make(ename))


def _t5_bucket_np(dist):
    n = np.maximum(dist, 0)
    max_exact = 16
    nf = np.maximum(n, max_exact).astype(np.float32)
    large = max_exact + (np.log(nf / np.float32(max_exact)) / np.float32(math.log(128 / max_exact))
                         * np.float32(32 - max_exact)).astype(np.int32)
    large = np.minimum(large, 31)
    return np.where(n < max_exact, n, large)


def build_program():
    nc = bass.Bass("TRN2", target_bir_lowering=False)

    def din(name, shape, dt=F32):
        return nc.dram_tensor(name, list(shape), dt, kind="ExternalInput").ap()

    xf = din("xf", [SEQ, D])
    cbc_d = din("cbc", [128, 8 * 128])
    valid_d = din("valid", [128, NT])
    blkbias_d = din("blkbias", [128, NQB * 32])
    bt_d = din("bt", [128, 12 * 3 * 256])
    b31_d = din("b31", [128, 12])
    umask_d = din("umask", [128, 2])
    ident_d = din("ident", [128, 128], BF16)
    w_ada = din("w_ada", [D, 6 * D])
    bada_d = din("bada_bc", [128, 6 * D])
    gmix_d = din("gmix_bc", [128, D])
    gffn_d = din("gffn_bc", [128, D])
    gfin_d = din("gfin_bc", [128, D])
    gsub_d = din("gsub_bc", [128, 128])
    lam_d = din("lam_bc", [128, 4 * 64])
    w_in = din("w_in", [D, 5120])
    w_br_a = din("w_br_a", [512, D])
    w_br_b = din("w_br_b", [512, D])
    w_out = din("w_out", [D, D])
    w_router = din("w_router", [D, NE])
    brt_d = din("brouter_bc", [128, NE])
    w_gu = din("w_gate_up", [NE, D, 2 * D]) if STAGE > 20 else None
    bgu_d = din("bgu_col", [128, NE * 16])
    w_dn = din("w_down", [NE, D, D]) if STAGE > 20 else None
    bdn_d = din("b_down", [NE, D])
    out_d = nc.dram_tensor("out", [2048, D], F32, kind="ExternalOutput").ap()
    dk = "ExternalOutput" if KDEBUG else "Internal"
    hTd = nc.dram_tensor("hTd", [NG, 128, 8 * 512], BF16, kind=dk).ap()
    x1d = nc.dram_tensor("x1d", [2048, D], F32, kind=dk).ap()
    if KDEBUG:
        dbg_ada = nc.dram_tensor("dbg_ada", [128, 3072], F32, kind=dk).ap()
        dbg_ada2 = nc.dram_tensor("dbg_ada2", [128, 3072], F32, kind=dk).ap()
        dbg_yT = nc.dram_tensor("dbg_yT", [128, 16384], BF16, kind=dk).ap()
        dbg_comb = nc.dram_tensor("dbg_comb", [128, 512], F32, kind=dk).ap()
        dbg_sm = nc.dram_tensor("dbg_sm", [128, 64], F32, kind=dk).ap()
        dbg_acc = nc.dram_tensor("dbg_acc", [128, 2048], F32, kind=dk).ap()

    P = Prog(nc)

    def chk(k):
        if STAGE == k:
            raise _Stop()

    with ExitStack() as st:
      try:
          def sb(name, cols, dt):
              return st.enter_context(nc.sbuf_tensor("sb_" + name, [128, cols], dt))

          def ps(name, cols, dt):
              return st.enter_context(nc.psum_tensor("ps_" + name, [128, cols], dt))

          BIGA = sb("BIGA", 16384, BF16)
          BIGB = sb("BIGB", 16896, BF16)
          BIGC = sb("BIGC", 8192, BF16)
          BIGD = sb("BIGD", 8192, BF16)
          YT = sb("YT", 16384, BF16)
          F32A = sb("F32A", 8192, F32)
          F32B = sb("F32B", 3072, F32)
          ADA = sb("ADA", 3072, F32)
          XNX = sb("XNX", 4736, BF16)
          XN = XNX
          ident = sb("ident", 128, BF16)
          CBC = F32B[:, 2048:3072]
          valid = sb("valid", NT, F32)
          blkbias = sb("blkbias", NQB * 32, F32)
          b31 = sb("b31", 12, F32)
          UMASK = sb("umask", 2, F32)
          gsub = sb("gsub", 128, F32)
          lamt = F32A[:, 0:256]
          SM = sb("SM", 64, F32)
          COMB = sb("COMB", 16 * 32, F32)
          BGU = sb("BGU", NE * 16, F32)
          WR = sb("WR", 8 * 32, BF16)
          BRT = sb("BRT", NE, F32)
          BDN = XNX[:, 2560:3584]
          S2 = ps("S2", 1024, F32)
          PO = [ps("PO%d" % i, 512, F32) for i in range(4)]
          PM = ps("PM", 512, F32)
          PTR = ps("PTR", 1024, BF16)

          def v3(ap, inner):
              return ap.rearrange("p (a b) -> p a b", b=inner)

          EPS = SM[:, 0:1]
          NEGLAM = SM[:, 1:2]
          EPS2 = SM[:, 7:8]
          LAMF = 1.0 - (0.8 - 0.6 * math.exp(-0.3 * 0))

          P.dma("sp", ident[:], ident_d, writes=["ident"], key="c0")
          P.dma("sp", valid[:], valid_d, writes=["valid"], key="c2")
          P.dma("sp", blkbias[:], blkbias_d, writes=["blkbias"], key="c3")
          P.dma("sp", b31[:], b31_d, writes=["b31"], key="c4")
          P.dma("sp", UMASK[:], umask_d, writes=["umask"], key="c4b")
          P.dma("sp", gsub[:], gsub_d, writes=["gsub"], key="c5")
          P.dma("sp", lamt, lam_d, writes=["lamt"], key="c6")
          P.dma("sp", BGU[:], bgu_d, writes=["BGU"], key="c7")
          P.dma("sp", BRT[:], brt_d, writes=["BRT"], key="c8")
          P.dma("pool", WR[:], w_router.rearrange("(kc p) n -> p kc n", p=128), writes=["WR"], key="c9")
          P.op("dve", lambda e: e.memset(SM[:], 0.0), writes=["SM"])
          P.op("dve", lambda e: e.memset(EPS, 1e-5), writes=["SM"])
          P.op("dve", lambda e: e.memset(EPS2, 1e-5 / (LAMF * LAMF)), writes=["SM"])
          P.op("dve", lambda e: e.tensor_scalar(out=v3(BGU[:, :], 16)[:, :, 8:16], in0=v3(BGU[:, :], 16)[:, :, 8:16], scalar1=1.0,
                                                 scalar2=None, op0=ALU.add), reads=["BGU"], writes=["BGU"])
          P.op("dve", lambda e: e.tensor_tensor(out=F32B[:, 0:64], in0=lamt[:, 0:64], in1=lamt[:, 64:128], op=ALU.mult),
               reads=["lamt"], writes=["lamtmp"])
          P.op("dve", lambda e: e.reduce_sum(out=SM[:, 2:3], in_=F32B[:, 0:64], axis=AX.X), reads=["lamtmp"], writes=["lam1"])
          P.op("dve", lambda e: e.tensor_tensor(out=F32B[:, 64:128], in0=lamt[:, 128:192], in1=lamt[:, 192:256], op=ALU.mult),
               reads=["lamt"], writes=["lamtmp2"])
          P.op("dve", lambda e: e.reduce_sum(out=SM[:, 3:4], in_=F32B[:, 64:128], axis=AX.X), reads=["lamtmp2"], writes=["lam2"])
          P.op("act", lambda e: e.activation(out=SM[:, 4:6], in_=SM[:, 2:4], func=AF.Exp), reads=["lam1", "lam2", "SM"], writes=["lame"])
          P.op("dve", lambda e: e.tensor_tensor(out=SM[:, 6:7], in0=SM[:, 5:6], in1=SM[:, 4:5], op=ALU.subtract),
               reads=["lame"], writes=["lamd"])
          P.op("dve", lambda e: e.tensor_scalar(out=NEGLAM, in0=SM[:, 6:7], scalar1=-0.2, scalar2=None, op0=ALU.add),
               reads=["lamd"], writes=["neglam"])
          P.barrier()

          gtfd = nc.dram_tensor("gtfd", [128, D], F32).ap()

          def ada_seg(seg, slot, tag, load_cbc=False):
              if load_cbc:
                  P.dma("sp", CBC, cbc_d, writes=["CBC"], key="c1")
              for hh_ in range(2):
                  P.dma("sp", v3(F32A[:, 0:8192], 1024)[:, :, hh_ * 512:(hh_ + 1) * 512],
                        w_ada[:, seg * 1024 + hh_ * 512: seg * 1024 + hh_ * 512 + 512].rearrange("(kc p) n -> p kc n", p=128),
                        writes=["F32A_%d" % hh_], key="wada%d" % hh_)
              P.dma("sp", F32B[:, 0:1024], bada_d[:, seg * 1024:(seg + 1) * 1024], writes=["badaseg"], key="bada")
              for half in range(2):
                  pb = PO[half]
                  for kc in range(8):
                      P.op("pe", lambda e, kc=kc, half=half, pb=pb: e.matmul(
                          pb[:, 0:512], lhsT=CBC[:, kc * 128:(kc + 1) * 128],
                          rhs=F32A[:, kc * 1024 + half * 512: kc * 1024 + half * 512 + 512],
                          start=(kc == 0), stop=(kc == 7)), reads=["CBC", "F32A_%d" % half], writes=["PO%d" % half])
                  P.op("dve", lambda e, half=half, pb=pb: e.tensor_tensor(
                      out=ADA[:, slot * 1024 + half * 512: slot * 1024 + half * 512 + 512], in0=pb[:, 0:512],
                      in1=F32B[:, half * 512: half * 512 + 512], op=ALU.add),
                      reads=["PO%d" % half, "badaseg"], writes=[tag])

          def ada_scale(slot, tag, g_d):
              P.dma("sp", F32B[:, 1024:2048], g_d, writes=["gtmp"], key="gtmp")
              P.op("dve", lambda e: e.scalar_tensor_tensor(
                  out=ADA[:, slot * 1024:(slot + 1) * 1024], in0=ADA[:, slot * 1024:(slot + 1) * 1024], scalar=1.0,
                  in1=F32B[:, 1024:2048], op0=ALU.add, op1=ALU.mult), reads=[tag, "gtmp"], writes=[tag])

          chk(0)
          ada_seg(0, 0, "ada0", load_cbc=True)
          ada_seg(1, 1, "ada1")
          ada_scale(1, "ada1", gmix_d)
          ada_seg(2, 2, "ada2")
          P.barrier()
          if KDEBUG:
              P.dma("sp", dbg_ada, ADA[:, :], key="dbg_ada")
              P.dma("sp", dbg_sm, SM[:, :], key="dbg_sm")
              P.barrier()

          def norm_transpose(xt_ap, xt_res, aslot, atag, sslot, stag, dst3, dst_res, par):
              ssq = SM[:, 8 + par:9 + par]
              rs = SM[:, 10 + par:11 + par]
              xn = XN[:, par * 1024:(par + 1) * 1024]
              t1 = F32B[:, 2048:3072]
              P.op("dve", lambda e: e.memset(ssq, 0.0), writes=["ssq%d" % par])
              P.op("act", lambda e: e.activation(out=xn, in_=xt_ap, func=AF.Square, accum_out=ssq),
                   reads=[xt_res, "ssq%d" % par], writes=["xn%d" % par, "ssq%d" % par])
              P.op("act", lambda e: e.activation(out=rs, in_=ssq, func=AF.Sqrt, scale=1.0 / D, bias=EPS),
                   reads=["ssq%d" % par, "SM"], writes=["rs%d" % par])
              P.op("dve", lambda e: e.reciprocal(out=rs, in_=rs), reads=["rs%d" % par], writes=["rs%d" % par])
              P.op("dve", lambda e: e.scalar_tensor_tensor(out=t1, in0=xt_ap, scalar=rs, in1=ADA[:, aslot * 1024:(aslot + 1) * 1024],
                                                            op0=ALU.mult, op1=ALU.mult),
                   reads=[xt_res, "rs%d" % par, atag], writes=["t1"])
              P.op("dve", lambda e: e.tensor_tensor(out=xn, in0=t1, in1=ADA[:, sslot * 1024:(sslot + 1) * 1024], op=ALU.add),
                   reads=["t1", stag], writes=["xn%d" % par])
              for kc in range(8):
                  P.op("pe", lambda e, kc=kc: e.transpose(out=PTR[:, kc * 128:(kc + 1) * 128], in_=xn[:, kc * 128:(kc + 1) * 128],
                                                           identity=ident[:]),
                       reads=["xn%d" % par, "ident"], writes=["PTR"])
              P.op("act", lambda e: e.activation(out=dst3, in_=v3(PTR[:, 0:1024], 128), func=AF.Copy),
                   reads=["PTR"], writes=[dst_res])

          chk(1)
          def pre_a(t):
              par = t % 2
              xt = F32B[:, par * 1024:(par + 1) * 1024]
              xres = "xt%d" % par
              ssq = SM[:, 8 + par:9 + par]
              rs = SM[:, 10 + par:11 + par]
              xn = XN[:, par * 1024:(par + 1) * 1024]
              t1 = F32B[:, 2048:3072]
              P.dma("sp", xt, xf[t * 128:(t + 1) * 128, :], writes=[xres], key=xres)
              P.op("dve", lambda e: e.memset(ssq, 0.0), writes=["ssq%d" % par])
              P.op("act", lambda e: e.activation(out=xn, in_=xt, func=AF.Square, accum_out=ssq),
                   reads=[xres, "ssq%d" % par], writes=["xn%d" % par, "ssq%d" % par])
              P.op("act", lambda e: e.activation(out=rs, in_=ssq, func=AF.Sqrt, scale=1.0 / D, bias=EPS),
                   reads=["ssq%d" % par, "SM"], writes=["rs%d" % par])
              P.op("dve", lambda e: e.reciprocal(out=rs, in_=rs), reads=["rs%d" % par], writes=["rs%d" % par])
              P.op("dve", lambda e: e.scalar_tensor_tensor(out=t1, in0=xt, scalar=rs, in1=ADA[:, 1024:2048],
                                                            op0=ALU.mult, op1=ALU.mult),
                   reads=[xres, "rs%d" % par, "ada1"], writes=["t1"])
              P.op("pool", lambda e: e.tensor_tensor(out=xn, in0=t1, in1=ADA[:, 0:1024], op=ALU.add),
                   reads=["t1", "ada0"], writes=["xn%d" % par])

          def pre_b(t):
              par = t % 2
              g, tt = t // 4, t % 4
              xn = XN[:, par * 1024:(par + 1) * 1024]
              hbuf = BIGD[:, (g % 2) * 4096:(g % 2 + 1) * 4096]
              hres = "hT%d" % (g % 2)
              for kc in range(8):
                  P.op("pe", lambda e, kc=kc: e.transpose(out=PTR[:, kc * 128:(kc + 1) * 128], in_=xn[:, kc * 128:(kc + 1) * 128],
                                                           identity=ident[:]),
                       reads=["xn%d" % par, "ident"], writes=["PTR"])
              P.op("act", lambda e: e.activation(out=v3(hbuf, 512)[:, :, tt * 128:(tt + 1) * 128], in_=v3(PTR[:, 0:1024], 128), func=AF.Copy),
                   reads=["PTR"], writes=[hres])
              if tt == 3:
                  P.dma("sp", hTd[g], hbuf, reads=[hres], key="hst%d" % (g % 2))

          pre_a(0)
          for t in range(NT):
              if t + 1 < NT:
                  pre_a(t + 1)
              pre_b(t)
          P.barrier()

          chk(2)
          ada_seg(5, 0, "ada0", load_cbc=True)
          P.dma("sp", gtfd, ADA[:, 0:1024], reads=["ada0"], key="gtfst")
          ada_seg(3, 0, "ada0")
          ada_seg(4, 1, "ada1")
          ada_scale(1, "ada1", gffn_d)
          P.barrier()
          if KDEBUG:
              P.dma("sp", dbg_ada2, ADA[:, :], key="dbg_ada2")
              P.barrier()

          KT = BIGA
          V = BIGB
          WK = BIGC[:, 0:2048]
          WV = BIGC[:, 2048:4096]
          WQ = XNX[:, 2560:4608]
          KMB = XNX[:, 4608:4736]
          QT = BIGC[:, 4096:8192]
          PTring = [XNX[:, i * 512:(i + 1) * 512] for i in range(4)]
          YTOK = XNX[:, 2048:2560]
          BT = F32A[:, 0:3072]
          NTMP = [F32A[:, 3072 + i * 512: 3072 + (i + 1) * 512] for i in range(2)] + [F32A[:, 6144:6656]]
          SSLOT = [S2[:, 0:512], S2[:, 512:1024], PM[:, 0:512]]
          SELB = [F32A[:, 5400:5656], F32A[:, 7424:7680]]
          SRES = ["S0", "S1", "PM"]
          ACC = F32A[:, 4096:4096 + 1040]
          KMS = F32A[:, 5200:5200 + 64]
          GSM = F32A[:, 5300:5300 + 32]
          M8 = F32A[:, 5340:5348]
          THR = F32A[:, 5350:5351]
          SEL = F32A[:, 5400:5400 + 2 * 4 * 32]
          OTMP = F32A[:, 5700:5700 + 128]
          RL = F32A[:, 5900:5908]

          for p in range(4):
              is_diff = p < 2
              if is_diff:
                  qcol, kcol, vcol = 0 + p * 256, 512 + p * 256, 1024 + p * 256
                  W = 129
                  heads = [2 * p, 2 * p, 2 * p + 1, 2 * p + 1]
                  vcols = [(0, 129), (0, 129), (130, 129), (130, 129)]
              else:
                  pp = p - 2
                  qcol, kcol, vcol = 1536 + pp * 256, 2048 + pp * 256, 2560 + pp * 256
                  W = 65
                  heads = [4 + 4 * pp + i for i in range(4)]
                  vcols = [(i * 66, 65) for i in range(4)]
              def load_pass_weights(pn):
                  if pn < 2:
                      cols = (512 + pn * 256, 1024 + pn * 256, 0 + pn * 256)
                  else:
                      cols = (2048 + (pn - 2) * 256, 2560 + (pn - 2) * 256, 1536 + (pn - 2) * 256)
                  for (wt, col, tag) in ((WK, cols[0], "WK"), (WV, cols[1], "WV"), (WQ, cols[2], "WQ")):
                      P.dma("pool", v3(wt, 256), w_in[:, col:col + 256].rearrange("(kc p) n -> p kc n", p=128),
                            writes=[tag], key=tag)

              if p == 0:
                  load_pass_weights(0)
              for i, h in enumerate(sorted(set(heads))):
                  P.dma("sp", BT[:, i * 768:(i + 1) * 768], bt_d[:, h * 768:(h + 1) * 768], writes=["BT%d" % i], key="BT%d" % i)
                  P.op("dve", lambda e, i=i, h=h: e.tensor_scalar(out=BT[:, i * 768:(i + 1) * 768], in0=BT[:, i * 768:(i + 1) * 768],
                                                                  scalar1=b31[:, h:h + 1], scalar2=None, op0=ALU.subtract),
                       reads=["BT%d" % i, "b31"], writes=["BT%d" % i])
              hidx = {h: i for i, h in enumerate(sorted(set(heads)))}
              ncol = 2 if is_diff else 4
              stride = 130 if is_diff else 66
              for i in range(ncol):
                  P.op("dve", lambda e, i=i, stride=stride: e.tensor_copy(
                      out=v3(V[:, 0:NT * 264], 264)[:, :, i * stride + stride - 2], in_=valid[:, :]),
                      reads=["valid"], writes=["Vones"])
              for g in range(NG):
                  hbuf = BIGD[:, (g % 2) * 4096:(g % 2 + 1) * 4096]
                  hres = "hT%d" % (g % 2)
                  P.dma("sp", hbuf, hTd[g], writes=[hres], key="hld%d" % (g % 2))
                  for c in range(2):
                      pb = PO[c]
                      for kc in range(8):
                          P.op("pe", lambda e, kc=kc, c=c, pb=pb, hbuf=hbuf: e.matmul(
                              pb[:, 0:512], lhsT=WK[:, kc * 256 + c * 128: kc * 256 + c * 128 + 128],
                              rhs=hbuf[:, kc * 512:(kc + 1) * 512], start=(kc == 0), stop=(kc == 7)),
                              reads=["WK", hres], writes=["PO%d" % c])
                      P.op("act", lambda e, c=c, pb=pb, g=g: e.activation(
                          out=KT[:, c * SEQ + g * 512: c * SEQ + g * 512 + 512], in_=pb[:, 0:512], func=AF.Copy),
                          reads=["PO%d" % c], writes=["KT%d_%d" % (c, g)])
                      if not is_diff:
                          for bb in range(2):
                              P.op("dve", lambda e, c=c, pb=pb, g=g, bb=bb: e.reduce_sum(
                                  out=KMS[:, c * 32 + 2 * g + bb: c * 32 + 2 * g + bb + 1],
                                  in_=KT[:, c * SEQ + g * 512 + bb * 256: c * SEQ + g * 512 + bb * 256 + 256], axis=AX.X),
                                  reads=["KT%d_%d" % (c, g)], writes=["KMS"])
                  for tt in range(4):
                      t = 4 * g + tt
                      pb = PO[2 + tt % 2]
                      pres = "PO%d" % (2 + tt % 2)
                      for kc in range(8):
                          P.op("pe", lambda e, kc=kc, tt=tt, pb=pb, hbuf=hbuf: e.matmul(
                              pb[:, 0:256], lhsT=hbuf[:, kc * 512 + tt * 128: kc * 512 + tt * 128 + 128],
                              rhs=WV[:, kc * 256:(kc + 1) * 256], start=(kc == 0), stop=(kc == 7)),
                              reads=["WV", hres], writes=[pres])
                      if is_diff:
                          pairs = [(V[:, t * 264 + cc * 130: t * 264 + cc * 130 + 128], pb[:, cc * 128:(cc + 1) * 128]) for cc in range(2)]
                      else:
                          pairs = [(V[:, t * 264 + jj * 66: t * 264 + jj * 66 + 64], pb[:, jj * 64:(jj + 1) * 64]) for jj in range(4)]
                      for (dst, src) in pairs:
                          P.op("dve", lambda e, dst=dst, src=src, t=t: e.tensor_scalar(
                              out=dst, in0=src, scalar1=valid[:, t:t + 1], scalar2=None, op0=ALU.mult),
                              reads=[pres, "valid"], writes=["V%d" % t])
                  if g % 2 == 1:
                      s = g // 2
                      for c in range(2):
                          pb = PO[c]
                          for kc in range(8):
                              P.op("pe", lambda e, kc=kc, c=c, pb=pb, hbuf=hbuf: e.matmul(
                                  pb[:, 0:256], lhsT=WQ[:, kc * 256 + c * 128: kc * 256 + c * 128 + 128],
                                  rhs=hbuf[:, kc * 512 + 256:(kc + 1) * 512], start=(kc == 0), stop=(kc == 7)),
                                  reads=["WQ", hres], writes=["PO%d" % c])
                          P.op("act", lambda e, c=c, pb=pb, s=s: e.activation(
                              out=QT[:, c * 2048 + s * 256: c * 2048 + s * 256 + 256], in_=pb[:, 0:256], func=AF.Copy),
                              reads=["PO%d" % c], writes=["QT%d" % s])
              if not is_diff:
                  for c in range(2):
                      for u in range(2):
                          P.op("dve", lambda e, c=c, u=u: e.tensor_scalar(
                              out=KMB[:, c * 64 + u * 32: c * 64 + u * 32 + 32], in0=KMS[:, c * 32:(c + 1) * 32],
                              scalar1=UMASK[:, u:u + 1], scalar2=1.0 / 256, op0=ALU.mult, op1=ALU.mult),
                              reads=["KMS", "umask"], writes=["KMB"])

              if p + 1 < 4:
                  load_pass_weights(p + 1)
              chk(3 + p * 2)
              QBD = BIGD
              P.op("pool", lambda e: e.memset(QBD[:, 0:2048], 0.0), writes=["QBDz", "hT0"])
              step = 0
              blkc = 0
              pre_issued = False
              deferred = []
              deferred_mid = []
              for s in range(NQB):
                  F = 4 * s + 3
                  nkt = 2 * F + 2
                  ktres = ["KT%d_%d" % (c, g) for c in range(2) for g in range(NG)]
                  def gating_group(st, gi):
                      qt_, c_ = divmod(gi, 2)
                      selb = SELB[st % 2]
                      selres = "SEL%d" % (st % 2)
                      gb = PO[3][:, gi * 64:(gi + 1) * 64]
                      P.op("pe", lambda e, c_=c_, qt_=qt_, st=st, gb=gb: e.matmul(
                          gb, lhsT=QT[:, c_ * 2048 + st * 256 + qt_ * 128: c_ * 2048 + st * 256 + qt_ * 128 + 128],
                          rhs=KMB[:, c_ * 64:(c_ + 1) * 64], start=True, stop=True),
                          reads=["QT%d" % st, "KMB"], writes=["PO3"])
                      for u in range(2):
                          j = 2 * c_ + u
                          gp = gb[:, u * 32:(u + 1) * 32]
                          P.op("dve", lambda e, gp=gp, st=st: e.tensor_tensor(
                              out=GSM, in0=gp, in1=blkbias[:, st * 32:(st + 1) * 32], op=ALU.add),
                              reads=["PO3", "blkbias"], writes=["GSM"])
                          P.op("dve", lambda e: e.max(out=M8, in_=GSM), reads=["GSM"], writes=["M8"])
                          P.op("dve", lambda e: e.tensor_scalar(out=THR, in0=M8[:, 2:3], scalar1=-1e29, scalar2=None, op0=ALU.max),
                               reads=["M8"], writes=["THR"])
                          P.op("dve", lambda e, qt_=qt_, j=j, selb=selb: e.tensor_scalar(
                              out=selb[:, (qt_ * 4 + j) * 32:(qt_ * 4 + j + 1) * 32], in0=GSM, scalar1=THR, scalar2=None, op0=ALU.is_ge),
                              reads=["GSM", "THR"], writes=[selres])

                  if (not is_diff) and s == 0:
                      for gi in range(4):
                          gating_group(0, gi)
                  SELc = SELB[s % 2]
                  SELcres = "SEL%d" % (s % 2)
                  par_s = s % 2

                  def qbd_stage(st):
                      for c_ in range(2):
                          qoff_ = ((st % 2) * 2 + c_) * 512
                          for u in range(2):
                              P.op("pool", lambda e, c_=c_, u=u, st=st, qoff_=qoff_: e.tensor_copy(
                                  out=QBD[u * 64:(u + 1) * 64, qoff_ + u * 256: qoff_ + u * 256 + 256],
                                  in_=QT[u * 64:(u + 1) * 64, c_ * 2048 + st * 256: c_ * 2048 + st * 256 + 256]),
                                  reads=["QT%d" % st, "QBDz"], writes=["QBD%d_%d" % (st % 2, c_)])

                  if s == 0:
                      qbd_stage(0)
                  for c in range(2):
                    qoff = (par_s * 2 + c) * 512
                    qres = "QBD%d_%d" % (par_s, c)

                    def flags(kt):
                        if is_diff:
                            return kt == 0, kt == nkt - 1
                        return kt % 2 == 0, kt % 2 == 1

                    def emit_qk(kt, slot, c=c, qoff=qoff, qres=qres):
                        P.op("pe", lambda e, c=c, kt=kt, slot=slot, qoff=qoff: e.matmul(
                            SSLOT[slot],
                            lhsT=KT[:, c * SEQ + kt * 128: c * SEQ + kt * 128 + 128],
                            rhs=QBD[:, qoff: qoff + 512], start=True, stop=True),
                            reads=["KT%d_%d" % (c, kt // 4), qres], writes=[SRES[slot]])

                    if c == 0:
                        nxt = (1, (par_s * 2 + 1) * 512, "QBD%d_1" % par_s)
                    elif s + 1 < NQB:
                        nxt = (0, (((s + 1) % 2) * 2) * 512, "QBD%d_0" % ((s + 1) % 2))
                    else:
                        nxt = None

                    def emit_exp(kt, slot, pt, ptres, c=c, F=F):
                        r = 2 * F - kt
                        sres = SRES[slot]
                        if r <= 1:
                            ridx = 1 - r
                            nt_ = NTMP[slot]
                            for u in range(2):
                                hi = hidx[heads[2 * c + u]]
                                P.op("dve", lambda e, u=u, hi=hi, ridx=ridx, nt_=nt_, slot=slot: e.scalar_tensor_tensor(
                                    out=nt_[:, u * 256:(u + 1) * 256], in0=SSLOT[slot][:, u * 256:(u + 1) * 256],
                                    scalar=0.125, in1=BT[:, hi * 768 + ridx * 256: hi * 768 + ridx * 256 + 256],
                                    op0=ALU.mult, op1=ALU.add), reads=[sres, "BT%d" % hi], writes=["NTMP%d" % slot])
                            P.op("act", lambda e, pt=pt, nt_=nt_: e.activation(out=pt, in_=nt_, func=AF.Exp),
                                 reads=["NTMP%d" % slot], writes=[ptres])
                        else:
                            P.op("act", lambda e, pt=pt, slot=slot: e.activation(out=pt, in_=SSLOT[slot],
                                                                                 func=AF.Exp, scale=0.125),
                                 reads=[sres], writes=[ptres])

                    def acc_loc(kt, qt, u, blk_base=blkc):
                        if is_diff:
                            return PO[qt * 2 + u], 0, "PO%d" % (qt * 2 + u), True
                        b = (blk_base + kt // 2) % 3
                        return PO[b], (qt * 2 + u) * W, "PO%d" % b, (qt == 0 and u == 0)

                    def emit_pv(kt, pt, ptres, c=c):
                        gstart, gstop = flags(kt)
                        for qt in range(2):
                            for u in range(2):
                                po, col0, pores, first = acc_loc(kt, qt, u)
                                voff, vw = vcols[2 * c + u]
                                st_ = gstart and first
                                P.op("pe", lambda e, po=po, col0=col0, pt=pt, u=u, qt=qt, kt=kt, voff=voff, vw=vw, st_=st_, gstop=gstop: e.matmul(
                                    po[:, col0:col0 + vw], lhsT=pt[:, u * 256 + qt * 128: u * 256 + qt * 128 + 128],
                                    rhs=V[:, kt * 264 + voff: kt * 264 + voff + vw], start=st_, stop=gstop, skip_group_check=True),
                                    reads=[ptres, "V%d" % kt, "Vones"], writes=[pores])

                    def emit_fold(kt, first_acc, c=c, F=F):
                        n = kt // 2
                        own = n == F
                        for qt in range(2):
                            for u in range(2):
                                po, col0, pores, _first = acc_loc(kt, qt, u)
                                j = 2 * c + u
                                a = ACC[:, (qt * 4 + j) * W:(qt * 4 + j) * W + W]
                                ares = "ACC%d_%d" % (qt, j)
                                src = po[:, col0:col0 + W]
                                if is_diff or (first_acc and own):
                                    P.op("dve", lambda e, a=a, src=src: e.tensor_copy(out=a, in_=src), reads=[pores], writes=[ares])
                                elif first_acc:
                                    sc_ = SELc[:, (qt * 4 + j) * 32 + n:(qt * 4 + j) * 32 + n + 1]
                                    P.op("dve", lambda e, a=a, src=src, sc_=sc_: e.tensor_scalar(
                                        out=a, in0=src, scalar1=sc_, scalar2=None,
                                        op0=ALU.mult), reads=[pores, SELcres], writes=[ares])
                                elif own:
                                    P.op("dve", lambda e, a=a, src=src: e.tensor_tensor(out=a, in0=src, in1=a, op=ALU.add),
                                         reads=[pores, ares], writes=[ares])
                                else:
                                    sc_ = SELc[:, (qt * 4 + j) * 32 + n:(qt * 4 + j) * 32 + n + 1]
                                    P.op("dve", lambda e, a=a, src=src, sc_=sc_: e.scalar_tensor_tensor(
                                        out=a, in0=src, scalar=sc_, in1=a,
                                        op0=ALU.mult, op1=ALU.add), reads=[pores, SELcres, ares], writes=[ares])

                    slots = [(step + i) % 3 for i in range(nkt + 2)]
                    pts = [(step + i) % 4 for i in range(nkt)]
                    step += nkt
                    blkc += nkt // 2
                    first_acc = True
                    if c == 1 and s + 1 < NQB:
                        qbd_stage(s + 1)
                    if not pre_issued:
                        emit_qk(0, slots[0])
                        emit_qk(1, slots[1])
                    pre_issued = False
                    for kt in range(nkt):
                        pt = PTring[pts[kt]]
                        ptres = "PT%d" % pts[kt]
                        emit_exp(kt, slots[kt], pt, ptres)
                        if kt + 2 < nkt:
                            emit_qk(kt + 2, slots[kt + 2])
                        elif nxt is not None:
                            emit_qk(kt + 2 - nkt, slots[kt + 2], c=nxt[0], qoff=nxt[1], qres=nxt[2])
                            pre_issued = True
                        emit_pv(kt, pt, ptres)
                        if flags(kt)[1]:
                            emit_fold(kt, first_acc)
                            first_acc = False
                            if (not is_diff) and c == 1 and kt // 2 < 4 and s + 1 < NQB:
                                gating_group(s + 1, kt // 2)
                        if c == 0 and kt == 7:
                            while deferred_mid:
                                deferred_mid.pop(0)()
                        if kt == 3 and c == (1 if is_diff else 0):
                            while deferred:
                                deferred.pop(0)()
                  if is_diff:
                      OT4 = F32A[:, 6656:7168]
                      SS4 = F32A[:, 7168:7172]
                      RS4 = F32A[:, 7172:7176]
                      JK = F32A[:, 7296:7424]
                      for qt in range(2):
                          for c in range(2):
                              k = qt * 2 + c
                              rl = F32A[:, 7200 + k * 4: 7200 + k * 4 + 4]
                              ot = OT4[:, k * 128:(k + 1) * 128]
                              a1 = ACC[:, (qt * 4 + 2 * c) * W:(qt * 4 + 2 * c) * W + W]
                              a2 = ACC[:, (qt * 4 + 2 * c + 1) * W:(qt * 4 + 2 * c + 1) * W + W]
                              ar = ["ACC%d_%d" % (qt, 2 * c), "ACC%d_%d" % (qt, 2 * c + 1)]
                              P.op("dve", lambda e, a1=a1, rl=rl: e.reciprocal(out=rl[:, 0:1], in_=a1[:, 128:129]), reads=ar, writes=["RLa%d" % k])
                              P.op("dve", lambda e, a2=a2, rl=rl: e.reciprocal(out=rl[:, 1:2], in_=a2[:, 128:129]), reads=ar, writes=["RLb%d" % k])
                              P.op("dve", lambda e, rl=rl: e.tensor_tensor(out=rl[:, 2:3], in0=rl[:, 1:2], in1=NEGLAM, op=ALU.mult),
                                   reads=["RLb%d" % k, "neglam"], writes=["RLc%d" % k])
                              P.op("dve", lambda e, a1=a1, rl=rl, ot=ot: e.tensor_scalar(out=ot, in0=a1[:, 0:128], scalar1=rl[:, 0:1], scalar2=None, op0=ALU.mult),
                                   reads=ar + ["RLa%d" % k], writes=["OT%d" % k])
                              P.op("dve", lambda e, a2=a2, rl=rl, ot=ot: e.scalar_tensor_tensor(out=ot, in0=a2[:, 0:128], scalar=rl[:, 2:3], in1=ot,
                                                                                                  op0=ALU.mult, op1=ALU.add),
                                   reads=ar + ["RLc%d" % k, "OT%d" % k], writes=["OT%d" % k])
                              P.op("dve", lambda e, ot=ot: e.tensor_tensor(out=JK, in0=ot, in1=ot, op=ALU.mult), reads=["OT%d" % k], writes=["JK"])
                              P.op("dve", lambda e, k=k: e.reduce_sum(out=SS4[:, k:k + 1], in_=JK, axis=AX.X), reads=["JK"], writes=["SS4"])

                      def fin_mid(s=s):
                          P.op("act", lambda e: e.activation(out=RS4, in_=SS4, func=AF.Sqrt, scale=1.0 / (128 * LAMF * LAMF), bias=EPS2),
                               reads=["SS4", "SM"], writes=["RS4"])
                          P.op("dve", lambda e: e.reciprocal(out=RS4, in_=RS4), reads=["RS4"], writes=["RS4"])
                          for qt in range(2):
                              for c in range(2):
                                  k = qt * 2 + c
                                  P.op("dve", lambda e, c=c, qt=qt, k=k: e.scalar_tensor_tensor(
                                      out=XNX[:, 2048 + qt * 256 + c * 128: 2048 + qt * 256 + c * 128 + 128], in0=OT4[:, k * 128:(k + 1) * 128],
                                      scalar=RS4[:, k:k + 1], in1=gsub[:], op0=ALU.mult, op1=ALU.mult),
                                      reads=["OT%d" % k, "RS4", "gsub"], writes=["YTOK%d" % qt])
                      deferred_mid.append(fin_mid)
                  else:
                      for qt in range(2):
                          for j in range(4):
                              a = ACC[:, (qt * 4 + j) * W:(qt * 4 + j) * W + W]
                              ares = "ACC%d_%d" % (qt, j)
                              P.op("dve", lambda e, a=a, j=j: e.reciprocal(out=RL[:, j:j + 1], in_=a[:, 64:65]), reads=[ares], writes=["RLm%d" % j])
                              P.op("dve", lambda e, a=a, j=j, qt=qt: e.tensor_scalar(
                                  out=XNX[:, 2048 + qt * 256 + j * 64: 2048 + qt * 256 + j * 64 + 64], in0=a[:, 0:64], scalar1=RL[:, j:j + 1],
                                  scalar2=None, op0=ALU.mult),
                                   reads=[ares, "RLm%d" % j], writes=["YTOK%d" % qt])
                  for qt in range(2):
                      def fin_pe(qt=qt, s=s, ch0=2 * p):
                          for c in range(2):
                              P.op("pe", lambda e, c=c, qt=qt: e.transpose(
                                  out=PTR[:, qt * 256 + c * 128: qt * 256 + c * 128 + 128],
                                  in_=XNX[:, 2048 + qt * 256 + c * 128: 2048 + qt * 256 + c * 128 + 128],
                                  identity=ident[:]), reads=["YTOK%d" % qt, "ident"], writes=["PTR"])
                          P.op("act", lambda e, ch0=ch0, s=s, qt=qt: e.activation(
                              out=v3(YT[:, ch0 * 2048:(ch0 + 2) * 2048], 2048)[:, :, s * 256 + qt * 128: s * 256 + qt * 128 + 128],
                              in_=v3(PTR[:, qt * 256:(qt + 1) * 256], 128), func=AF.Copy), reads=["PTR"], writes=["YT%d" % s])
                      deferred.append(fin_pe)
                  if s == 0:
                      chk(50 + p)
                      if KDEBUG and p == 2:
                          P.barrier()
                          P.dma("sp", dbg_acc, F32A[:, 4096:6144], key="dbg_acc")
                          P.barrier()
              while deferred_mid:
                  deferred_mid.pop(0)()
              while deferred:
                  deferred.pop(0)()
              P.barrier()
              chk(4 + p * 2)

          if KDEBUG:
              P.dma("sp", dbg_yT, YT[:, :], key="dbg_yT")
              P.barrier()
          WGA = BIGA[:, 0:8192]
          WGB = BIGA[:, 8192:16384]
          WBA = BIGB[:, 0:4096]
          WBB = BIGB[:, 4096:8192]
          WO = BIGB[:, 8192:16384]
          P.dma("pool", v3(WGA, 1024), w_in[:, 3072:4096].rearrange("(kc p) n -> p kc n", p=128), writes=["WGA"], key="WGA")
          P.dma("pool", v3(WGB, 1024), w_in[:, 4096:5120].rearrange("(kc p) n -> p kc n", p=128), writes=["WGB"], key="WGB")
          P.dma("pool", v3(WBA, 1024), w_br_a.rearrange("(kc p) n -> p kc n", p=128), writes=["WBA"], key="WBA")
          P.dma("pool", v3(WBB, 1024), w_br_b.rearrange("(kc p) n -> p kc n", p=128), writes=["WBB"], key="WBB")
          P.dma("pool", v3(WO, 1024), w_out.rearrange("(kc p) n -> p kc n", p=128), writes=["WO"], key="WO")
          HQ = [BIGD[:, i * 2048:(i + 1) * 2048] for i in range(2)]
          MT = BIGD[:, 4096:6144]
          SG = [F32A[:, i * 256:(i + 1) * 256] for i in range(4)]
          X1 = [F32A[:, 2048 + i * 1024: 2048 + (i + 1) * 1024] for i in range(2)]
          LG = F32A[:, 4096:4096 + 32]
          EALL = F32A[:, 4160:4160 + 32]
          MTB = [BIGD[:, 4096:6144], BIGD[:, 6144:8192]]

          def mix_mloop(s):
              hq = HQ[s % 2]
              hqres = "HQ%d" % (s % 2)
              mt = MTB[s % 2]
              mtres = "MT%d" % (s % 2)
              P.dma("sp", v3(hq, 256), v3(hTd[2 * s + 1], 512)[:, :, 256:512], writes=[hqres], key=hqres)
              for m in range(8):
                  if m % 2 == 0:
                      pa, pga, pb_, pgb = S2[:, 0:256], S2[:, 256:512], S2[:, 512:768], S2[:, 768:1024]
                      ra_, rga_, rb_, rgb_ = "S2a", "S2b", "S2c", "S2d"
                  else:
                      pa, pga, pb_, pgb = PO[2][:, 0:256], PO[2][:, 256:512], PO[3][:, 0:256], PO[3][:, 256:512]
                      ra_, rga_, rb_, rgb_ = "P2a", "P2b", "P3a", "P3b"
                  for (dst, wt, nk, src, srcres, wres, ch_off, dres) in (
                          (pa, WBA, 4, YT, "YT%d" % s, "WBA", 0, ra_), (pga, WGA, 8, hq, hqres, "WGA", 0, rga_),
                          (pb_, WBB, 4, YT, "YT%d" % s, "WBB", 4, rb_), (pgb, WGB, 8, hq, hqres, "WGB", 0, rgb_)):
                      for kc in range(nk):
                          if src is YT:
                              rhs = YT[:, (ch_off + kc) * 2048 + s * 256:(ch_off + kc) * 2048 + s * 256 + 256]
                          else:
                              rhs = hq[:, kc * 256:(kc + 1) * 256]
                          P.op("pe", lambda e, dst=dst, wt=wt, kc=kc, m=m, rhs=rhs, nk=nk: e.matmul(
                              dst, lhsT=wt[:, kc * 1024 + m * 128: kc * 1024 + m * 128 + 128], rhs=rhs,
                              start=(kc == 0), stop=(kc == nk - 1)), reads=[wres, srcres], writes=[dres])
                  P.op("act", lambda e, pga=pga: e.activation(out=SG[0], in_=pga, func=AF.Sigmoid), reads=[rga_], writes=["SG0"])
                  P.op("act", lambda e, pgb=pgb: e.activation(out=SG[1], in_=pgb, func=AF.Sigmoid), reads=[rgb_], writes=["SG1"])
                  P.op("dve", lambda e, pa=pa: e.tensor_tensor(out=SG[2], in0=pa, in1=SG[0], op=ALU.mult), reads=[ra_, "SG0"], writes=["SG2"])
                  P.op("dve", lambda e, pb_=pb_: e.tensor_tensor(out=SG[3], in0=pb_, in1=SG[1], op=ALU.mult), reads=[rb_, "SG1"], writes=["SG3"])
                  P.op("dve", lambda e, m=m, mt=mt: e.tensor_tensor(out=mt[:, m * 256:(m + 1) * 256], in0=SG[2], in1=SG[3], op=ALU.add),
                       reads=["SG2", "SG3"], writes=[mtres])

          def mix_s1(s, qt):
              F = 4 * s + 3
              mt = MTB[s % 2]
              mtres = "MT%d" % (s % 2)
              tl = 2 * s + qt
              par = tl % 2
              xt = F32B[:, par * 1024:(par + 1) * 1024]
              row0 = F * 256 + qt * 128
              P.dma("sp", xt, xf[row0:row0 + 128, :], writes=["xt%d" % par], key="xt%d" % par)
              x1 = X1[par]
              x1res = "X1_%d" % par
              for half in range(2):
                  pz = PO[half]
                  for kc in range(8):
                      P.op("pe", lambda e, pz=pz, kc=kc, half=half: e.matmul(
                          pz[:, 0:512], lhsT=mt[:, kc * 256 + qt * 128: kc * 256 + qt * 128 + 128],
                          rhs=WO[:, kc * 1024 + half * 512: kc * 1024 + half * 512 + 512], start=(kc == 0), stop=(kc == 7)),
                          reads=[mtres, "WO"], writes=["PO%d" % half])
                  P.op("dve", lambda e, pz=pz, half=half: e.tensor_tensor(
                      out=x1[:, half * 512:(half + 1) * 512], in0=pz[:, 0:512], in1=ADA[:, 2048 + half * 512: 2048 + half * 512 + 512],
                      op=ALU.mult), reads=["PO%d" % half, "ada2"], writes=[x1res])
                  P.op("dve", lambda e, half=half: e.tensor_tensor(
                      out=x1[:, half * 512:(half + 1) * 512], in0=x1[:, half * 512:(half + 1) * 512], in1=xt[:, half * 512:(half + 1) * 512],
                      op=ALU.add), reads=[x1res, "xt%d" % par], writes=[x1res])
              P.dma("sp", x1d[tl * 128:(tl + 1) * 128, :], x1, reads=[x1res], key="x1st%d" % par)

          def mix_s2(s, qt):
              tl = 2 * s + qt
              par = tl % 2
              x1 = X1[par]
              x1res = "X1_%d" % par
              ssq = SM[:, 8 + par:9 + par]
              rs = SM[:, 10 + par:11 + par]
              xn = XN[:, par * 1024:(par + 1) * 1024]
              t1 = F32B[:, 2048:3072]
              P.op("dve", lambda e: e.memset(ssq, 0.0), writes=["ssq%d" % par])
              P.op("act", lambda e: e.activation(out=xn, in_=x1, func=AF.Square, accum_out=ssq),
                   reads=[x1res, "ssq%d" % par], writes=["xn%d" % par, "ssq%d" % par])
              P.op("act", lambda e: e.activation(out=rs, in_=ssq, func=AF.Sqrt, scale=1.0 / D, bias=EPS),
                   reads=["ssq%d" % par, "SM"], writes=["rs%d" % par])
              P.op("dve", lambda e: e.reciprocal(out=rs, in_=rs), reads=["rs%d" % par], writes=["rs%d" % par])
              P.op("dve", lambda e: e.scalar_tensor_tensor(out=t1, in0=x1, scalar=rs, in1=ADA[:, 1024:2048],
                                                            op0=ALU.mult, op1=ALU.mult),
                   reads=[x1res, "rs%d" % par, "ada1"], writes=["t1"])
              P.op("pool", lambda e: e.tensor_tensor(out=xn, in0=t1, in1=ADA[:, 0:1024], op=ALU.add),
                   reads=["t1", "ada0"], writes=["xn%d" % par])

          def mix_s3(s, qt):
              tl = 2 * s + qt
              par = tl % 2
              xn = XN[:, par * 1024:(par + 1) * 1024]
              for kc in range(8):
                  P.op("pe", lambda e, kc=kc: e.transpose(out=PTR[:, kc * 128:(kc + 1) * 128], in_=xn[:, kc * 128:(kc + 1) * 128],
                                                           identity=ident[:]),
                       reads=["xn%d" % par, "ident"], writes=["PTR"])
              P.op("act", lambda e: e.activation(out=v3(YT[:, 0:16384], 2048)[:, :, s * 256 + qt * 128: s * 256 + qt * 128 + 128],
                                                 in_=v3(PTR[:, 0:1024], 128), func=AF.Copy),
                   reads=["PTR"], writes=["YT%d" % s])
              for kc in range(8):
                  P.op("pe", lambda e, kc=kc: e.matmul(
                      PM[:, 0:32], lhsT=YT[:, kc * 2048 + s * 256 + qt * 128: kc * 2048 + s * 256 + qt * 128 + 128],
                      rhs=WR[:, kc * 32:(kc + 1) * 32], start=(kc == 0), stop=(kc == 7)), reads=["YT%d" % s, "WR"], writes=["PM"])
              P.op("dve", lambda e: e.tensor_tensor(out=LG, in0=PM[:, 0:32], in1=BRT[:], op=ALU.add), reads=["PM", "BRT"], writes=["LG"])
              P.op("dve", lambda e: e.max(out=M8, in_=LG), reads=["LG"], writes=["M8"])
              P.op("dve", lambda e: e.tensor_scalar(out=RL[:, 0:1], in0=M8[:, 0:1], scalar1=-1.0, scalar2=None, op0=ALU.mult),
                   reads=["M8"], writes=["RL0"])
              P.op("act", lambda e: e.activation(out=EALL, in_=LG, func=AF.Exp, bias=RL[:, 0:1]), reads=["LG", "RL0"], writes=["EALL"])
              cb = COMB[:, tl * 32:(tl + 1) * 32]
              P.op("dve", lambda e: e.scalar_tensor_tensor(out=cb, in0=LG, scalar=M8[:, 3:4], in1=EALL, op0=ALU.is_ge, op1=ALU.mult),
                   reads=["LG", "M8", "EALL"], writes=["COMB%d" % tl])
              P.op("dve", lambda e: e.reduce_sum(out=RL[:, 1:2], in_=cb, axis=AX.X), reads=["COMB%d" % tl], writes=["RL1"])
              P.op("dve", lambda e: e.reciprocal(out=RL[:, 1:2], in_=RL[:, 1:2]), reads=["RL1"], writes=["RL1"])
              P.op("dve", lambda e: e.tensor_scalar(out=cb, in0=cb, scalar1=RL[:, 1:2], scalar2=None, op0=ALU.mult),
                   reads=["COMB%d" % tl, "RL1"], writes=["COMB%d" % tl])

          mix_mloop(0)
          for s in range(NQB):
              mix_s1(s, 0)
              mix_s1(s, 1)
              mix_s2(s, 0)
              mix_s2(s, 1)
              if s + 1 < NQB:
                  mix_mloop(s + 1)
              mix_s3(s, 0)
              mix_s3(s, 1)
          P.barrier()

          if KDEBUG:
              P.dma("sp", dbg_comb, COMB[:, :], key="dbg_comb")
              P.barrier()
          chk(20)
          WGU = [BIGA, BIGB]
          WD = BIGC
          ACT_T = BIGD
          ACCM = F32A
          TG = [F32B[:, i * 512:(i + 1) * 512] for i in range(6)]
          P.dma("pool", BDN[0:32, :], bdn_d, writes=["BDN"], key="c10")
          outs = []
          it = 0
          for half in range(2):
              def actT(fc, c0, n, half=half):
                  base = fc * 1024
                  return ACT_T[:, base + c0: base + c0 + n]

              for tl in range(8):
                  gt = half * 8 + tl
                  P.op("dve", lambda e, gt=gt: e.tensor_copy(out=XNX[:, 0:32], in_=COMB[:, gt * 32:(gt + 1) * 32]),
                       reads=["COMB"], writes=["cbf"])
                  P.op("pe", lambda e: e.transpose(out=PTR[0:32, 0:128], in_=XNX[:, 0:32], identity=ident[:]),
                       reads=["cbf", "ident"], writes=["PTR"])
                  P.op("act", lambda e: e.activation(out=XNX[0:32, 128:256], in_=PTR[0:32, 0:128], func=AF.Copy),
                       reads=["PTR"], writes=["cT"])
                  for hf in range(2):
                      pz = PO[hf]
                      P.op("pe", lambda e, pz=pz, hf=hf: e.matmul(
                          pz[:, 0:512], lhsT=XNX[0:32, 128:256], rhs=BDN[0:32, hf * 512:(hf + 1) * 512],
                          start=True, stop=True), reads=["cT", "BDN"], writes=["PO%d" % hf])
                      P.op("dve", lambda e, pz=pz, tl=tl, hf=hf: e.tensor_copy(
                          out=ACCM[:, tl * 1024 + hf * 512: tl * 1024 + hf * 512 + 512], in_=pz[:, 0:512]),
                          reads=["PO%d" % hf], writes=["ACCM%d" % tl])
              def load_w(itn):
                  exn = itn % NE
                  bufn = itn % 2
                  for hh in range(2):
                      P.dma("pool", v3(WGU[bufn][:, 0:16384], 2048)[:, :, hh * 1024:(hh + 1) * 1024],
                            w_gu[exn][:, hh * 1024:(hh + 1) * 1024].rearrange("(kc p) n -> p kc n", p=128),
                            writes=["WGU%d_%d" % (bufn, hh)], key="WGU%d_%d" % (bufn, hh))

              def load_wd(itn):
                  exn = itn % NE
                  P.dma("pool", v3(WD[:, 0:8192], 1024), w_dn[exn].rearrange("(kc p) n -> p kc n", p=128),
                        writes=["WD"], key="WD")

              if half == 0:
                  load_w(0)
                  load_wd(0)
              def emit_up(ex, buf, tg, fc, half=half, actT=actT):
                  wgu = WGU[buf]
                  tok0 = half * 1024 + tg * 512
                  if fc % 2 == 0:
                      pg, pgres, pu, pures = S2[:, 0:512], "S2a", S2[:, 512:1024], "S2c"
                  else:
                      pg, pgres, pu, pures = PM[:, 0:512], "PM", PO[3][:, 0:512], "PO3"
                  for (dst, hh, dres) in ((pg, 0, pgres), (pu, 1, pures)):
                      for kc in range(8):
                          P.op("pe", lambda e, dst=dst, hh=hh, kc=kc, fc=fc, tok0=tok0, wgu=wgu: e.matmul(
                              dst, lhsT=wgu[:, kc * 2048 + hh * 1024 + fc * 128: kc * 2048 + hh * 1024 + fc * 128 + 128],
                              rhs=YT[:, kc * 2048 + tok0: kc * 2048 + tok0 + 512], start=(kc == 0), stop=(kc == 7)),
                              reads=["WGU%d_%d" % (buf, hh), "H2T"], writes=[dres])
                  k4 = (fc % 2) * 3
                  g_, sg_, u_ = TG[k4], TG[k4 + 1], TG[k4 + 2]
                  a_ = g_
                  rg, rsg, ru = ["TG%d" % (k4 + i) for i in range(3)]
                  ra = rg
                  P.op("dve", lambda e, g_=g_, pg=pg, ex=ex, fc=fc: e.tensor_scalar(
                      out=g_, in0=pg, scalar1=BGU[:, ex * 16 + fc: ex * 16 + fc + 1], scalar2=7.0, op0=ALU.add, op1=ALU.min),
                      reads=[pgres, "BGU"], writes=[rg])
                  P.op("act", lambda e, g_=g_, sg_=sg_: e.activation(out=sg_, in_=g_, func=AF.Sigmoid, scale=1.702),
                       reads=[rg], writes=[rsg])
                  P.op("dve", lambda e, u_=u_, pu=pu, ex=ex, fc=fc: e.tensor_scalar(
                      out=u_, in0=pu, scalar1=BGU[:, ex * 16 + 8 + fc: ex * 16 + 8 + fc + 1], scalar2=8.0, op0=ALU.add, op1=ALU.min),
                      reads=[pures, "BGU"], writes=[ru])
                  P.op("pool", lambda e, a_=a_, g_=g_, sg_=sg_: e.tensor_tensor(out=a_, in0=g_, in1=sg_, op=ALU.mult),
                       reads=[rg, rsg], writes=[ra])
                  dstA = actT(fc, tg * 512, 512)
                  P.op("dve", lambda e, a_=a_, u_=u_, dstA=dstA: e.scalar_tensor_tensor(
                      out=dstA, in0=u_, scalar=-6.0, in1=a_,
                      op0=ALU.max, op1=ALU.mult), reads=[ra, ru], writes=["ACT%d" % tg])

              def emit_down(ex, tg, half=half, actT=actT):
                  wd = WD
                  for tt in range(4):
                      tl = tg * 4 + tt
                      gt = half * 8 + tl
                      for hf in range(2):
                          pz = PO[(tt * 2 + hf) % 3]
                          pzres = "PO%d" % ((tt * 2 + hf) % 3)
                          for fc in range(8):
                              lA = actT(fc, tg * 512 + tt * 128, 128)
                              P.op("pe", lambda e, pz=pz, fc=fc, hf=hf, wd=wd, lA=lA: e.matmul(
                                  pz[:, 0:512], lhsT=lA,
                                  rhs=wd[:, fc * 1024 + hf * 512: fc * 1024 + hf * 512 + 512], start=(fc == 0), stop=(fc == 7)),
                                  reads=["ACT%d" % tg, "WD"], writes=[pzres])
                          acc = ACCM[:, tl * 1024 + hf * 512: tl * 1024 + hf * 512 + 512]
                          P.op("dve", lambda e, pz=pz, acc=acc, gt=gt, ex=ex: e.scalar_tensor_tensor(
                              out=acc, in0=pz[:, 0:512], scalar=COMB[:, gt * 32 + ex: gt * 32 + ex + 1], in1=acc,
                              op0=ALU.mult, op1=ALU.add), reads=[pzres, "ACCM%d" % tl], writes=["ACCM%d" % tl])

              seq = [(ex, tg) for ex in range(NE) for tg in range(2)]
              hoisted = False
              for idx, (ex, tg) in enumerate(seq):
                  itn = half * NE + ex
                  buf = itn % 2
                  if idx == 0 and itn + 1 < 2 * NE:
                      load_w(itn + 1)
                  for fc in range(8):
                      if fc < 2 and hoisted:
                          continue
                      emit_up(ex, buf, tg, fc)
                  hoisted = False
                  if idx + 1 < len(seq):
                      ex2, tg2 = seq[idx + 1]
                      itn2 = half * NE + ex2
                      if tg2 == 0 and itn2 + 1 < 2 * NE:
                          load_w(itn2 + 1)
                      emit_up(ex2, itn2 % 2, tg2, 0)
                      emit_up(ex2, itn2 % 2, tg2, 1)
                      hoisted = True
                  emit_down(ex, tg)
                  if tg == 1 and itn + 1 < 2 * NE:
                      load_wd(itn + 1)
              P.barrier()
              if half == 0:
                  P.dma("sp", ADA[:, 2048:3072], gfin_d, writes=["ada2"], key="gfin")
                  P.dma("sp", ADA[:, 0:1024], gtfd, writes=["ada3"], key="gtfld")
              def fin_a(tl, half=half):
                  gt = half * 8 + tl
                  par = gt % 2
                  xt = F32B[:, par * 1024:(par + 1) * 1024]
                  acc = ACCM[:, tl * 1024:(tl + 1) * 1024]
                  ares = "ACCM%d" % tl
                  ssq = SM[:, 8 + par:9 + par]
                  rs = SM[:, 10 + par:11 + par]
                  P.dma("sp", xt, x1d[gt * 128:(gt + 1) * 128, :], writes=["xt%d" % par], key="xt%d" % par)
                  P.op("pool", lambda e: e.tensor_tensor(out=acc, in0=acc, in1=ADA[:, 0:1024], op=ALU.mult),
                       reads=[ares, "ada3"], writes=[ares])
                  P.op("dve", lambda e: e.tensor_tensor(out=acc, in0=acc, in1=xt, op=ALU.add),
                       reads=[ares, "xt%d" % par], writes=[ares])
                  P.op("dve", lambda e: e.memset(ssq, 0.0), writes=["ssq%d" % par])
                  P.op("act", lambda e: e.activation(out=xt, in_=acc, func=AF.Square, accum_out=ssq),
                       reads=[ares, "ssq%d" % par], writes=["xt%d" % par, "ssq%d" % par])
                  P.op("act", lambda e: e.activation(out=rs, in_=ssq, func=AF.Sqrt, scale=1.0 / D, bias=EPS),
                       reads=["ssq%d" % par, "SM"], writes=["rs%d" % par])

              def fin_b(tl, half=half):
                  gt = half * 8 + tl
                  par = gt % 2
                  acc = ACCM[:, tl * 1024:(tl + 1) * 1024]
                  ares = "ACCM%d" % tl
                  rs = SM[:, 10 + par:11 + par]
                  P.op("dve", lambda e: e.reciprocal(out=rs, in_=rs), reads=["rs%d" % par], writes=["rs%d" % par])
                  P.op("dve", lambda e: e.scalar_tensor_tensor(out=acc, in0=acc, scalar=rs, in1=ADA[:, 2048:3072],
                                                                op0=ALU.mult, op1=ALU.mult),
                       reads=[ares, "rs%d" % par, "ada2"], writes=[ares])
                  outs.append(P.dma("sp", out_d[gt * 128:(gt + 1) * 128, :], acc, reads=[ares], key="ost%d" % tl))

              fin_a(0)
              for tl in range(8):
                  if tl + 1 < 8:
                      fin_a(tl + 1)
                  fin_b(tl)
              P.barrier()
      except _Stop:
        P.barrier()
        outs = [P.dma("sp", out_d[i * 128:(i + 1) * 128, :], F32A[:, i * 1024:(i + 1) * 1024], key="dbgout%d" % i) for i in range(8)]
        if KDEBUG:
            outs.append(P.dma("sp", dbg_yT, YT[:, :], key="dbg_yT"))
      print("ops per engine", {e: len(v) for e, v in P.ops.items()})
      P.emit(final_wait_ops=outs)
    return nc


_NC_CACHE = {}


def kernel(x, c, rel_bias, w_ada, b_ada, g_mix, w_in, lambda_q1, lambda_k1, lambda_q2, lambda_k2, subln_g,
           w_br_a, w_br_b, w_out, g_ffn, w_router, b_router, w_gate_up, b_gate_up, w_down, b_down, g_final):
    f = lambda a: np.ascontiguousarray(np.asarray(a, dtype=np.float32))
    x = f(x); c = f(c); rel_bias = f(rel_bias)
    bc = lambda v: np.ascontiguousarray(np.broadcast_to(np.asarray(v, np.float32).reshape(1, -1), (128, np.asarray(v).size)))
    kk = np.arange(128)[:, None]
    qq = np.arange(256)[None, :]
    bt = np.empty((128, 12, 3, 256), np.float32)
    for ri, r in enumerate((1, 0, -1)):
        dist = r * 128 + qq - kk
        bidx = _t5_bucket_np(dist)
        tile = rel_bias[bidx]
        tile = np.where((dist >= 0)[:, :, None], tile, np.float32(NEG))
        bt[:, :, ri, :] = np.transpose(tile, (0, 2, 1))
    bt = np.ascontiguousarray(bt.reshape(128, 12 * 3 * 256))
    b31 = bc(rel_bias[31])
    ident = np.eye(128, dtype=np.float32).astype(ml_dtypes.bfloat16)
    lam_bc = bc(np.concatenate([f(lambda_q1)[0], f(lambda_k1)[0], f(lambda_q2)[0], f(lambda_k2)[0]]))
    lam_init = 0.8 - 0.6 * math.exp(-0.3 * 0)
    gsub_bc = bc(f(subln_g)[0]) * np.float32(1.0)
    shared = {
        "bt": bt, "b31": b31, "ident": ident,
        "umask": np.ascontiguousarray(np.stack([(np.arange(128) < 64), (np.arange(128) >= 64)], axis=1).astype(np.float32)),
        "w_ada": f(w_ada)[0], "bada_bc": bc(f(b_ada)[0]), "gmix_bc": bc(f(g_mix)[0]), "gffn_bc": bc(f(g_ffn)[0]),
        "gfin_bc": bc(f(g_final)), "gsub_bc": gsub_bc, "lam_bc": lam_bc,
        "w_in": f(w_in)[0], "w_br_a": f(w_br_a)[0], "w_br_b": f(w_br_b)[0], "w_out": f(w_out)[0],
        "w_router": f(w_router)[0], "brouter_bc": bc(f(b_router)[0]),
        "w_gate_up": f(w_gate_up)[0],
        "bgu_col": np.ascontiguousarray(f(b_gate_up)[0].reshape(NE, 16, 128).transpose(2, 0, 1).reshape(128, NE * 16)),
        "w_down": f(w_down)[0], "b_down": f(b_down)[0],
    }
    in_maps = []
    for core in range(8):
        b, j = core // 4, core % 4
        nd = (3 - j) * 256
        xfr = np.zeros((SEQ, D), np.float32)
        xfr[nd:] = x[b, :SEQ - nd]
        tok = np.arange(SEQ).reshape(NT, 128).T
        valid = (tok >= nd).astype(np.float32)
        blk = np.full((NQB, 32), -1e30, np.float32)
        for s in range(NQB):
            F = 4 * s + 3
            blk[s, nd // 256:F] = 0.0
        m = dict(shared)
        m.update({
            "xf": xfr,
            "cbc": np.ascontiguousarray(np.broadcast_to(c[b].reshape(8, 128).T[:, :, None], (128, 8, 128)).reshape(128, 1024)),
            "valid": np.ascontiguousarray(valid),
            "blkbias": bc(blk.reshape(-1)),
        })
        in_maps.append(m)
    if "nc" not in _NC_CACHE:
        _NC_CACHE["nc"] = build_program()
    if STAGE <= 20:
        for m in in_maps:
            m.pop("w_gate_up"); m.pop("w_down")
    res = run_bass_kernel_spmd(_NC_CACHE["nc"], in_maps, core_ids=list(range(8)))
    if KDEBUG:
        _LAST["res"] = res.results
    out = np.empty((2, SEQ, D), np.float32)
    for core in range(8):
        b, j = core // 4, core % 4
        o = res.results[core]["out"]
        for s in range(NQB):
            blk0 = (4 * s + j) * 256
            out[b, blk0:blk0 + 256] = o[s * 256:(s + 1) * 256]
    return out
```
